# Optimizing a Trainium2 kernel written in Bass

```python
import math
import jax
import jax.numpy as jnp
from jax import lax
import numpy as np

D_MODEL = 1024
BATCH = 8
SEQ = 2048
DEPTH = 2

GRID_W = 64
CTX_LEN = 256
NORM_EPS = 1e-6
NA_HEADS = 8
NA_HEAD_DIM = 64
NA_WIDTH = NA_HEADS * NA_HEAD_DIM
WIN_ROWS = 8
WIN_COLS = 16
CONV_CH = D_MODEL // 2
CONV_K = 31
MIX_IN = 3 * NA_WIDTH + 2 * CONV_CH
MIX_OUT = NA_WIDTH + CONV_CH
SSM_WIDTH = D_MODEL
SSM_GROUP = 16
SSM_GROUPS = SSM_WIDTH // SSM_GROUP
SSM_STATE = 64
FFN_DIM = 2816
N_EXPERTS = 8
TOP_K = 2
EXPERT_DIM = 3584

kernel_name = 'hybrid_natten_conformer_s5_moe_dit'


def _f32(t):
    return t.astype(jnp.float32)


def rmsnorm(x, g):
    xf = _f32(x)
    y = xf * lax.rsqrt(jnp.mean(xf * xf, axis=-1, keepdims=True) + NORM_EPS)
    return (y * _f32(g)).astype(x.dtype)


def layernorm(x, g, b):
    xf = _f32(x)
    mu = jnp.mean(xf, axis=-1, keepdims=True)
    var = jnp.mean(jnp.square(xf - mu), axis=-1, keepdims=True)
    return ((xf - mu) * lax.rsqrt(var + NORM_EPS) * _f32(g) + _f32(b)).astype(x.dtype)


def adaln(cond, w, b):
    return jnp.split(jax.nn.silu(cond) @ w + b, 6, axis=-1)


def modulate(h, shift, scale):
    return h * (1 + scale[:, None, :]) + shift[:, None, :]


def swiglu(h, w1, w3, w2):
    return (jax.nn.silu(h @ w1) * (h @ w3)) @ w2


def neighbourhood_attention(q, k, v, k_ctx, v_ctx, rpb):
    B, L, H, Dh = q.shape
    rows = L // GRID_W
    kr = min(WIN_ROWS, rows)
    qg = q.reshape(B, rows, GRID_W, H, Dh)
    kg = k.reshape(B, rows, GRID_W, H, Dh)
    vg = v.reshape(B, rows, GRID_W, H, Dh)
    r = jnp.arange(rows)
    row_idx = jnp.clip(r - kr // 2, 0, rows - kr)[:, None] + jnp.arange(kr)
    k_band = kg[:, row_idx]
    v_band = vg[:, row_idx]
    col = jnp.arange(GRID_W)
    col_start = jnp.clip(col - WIN_COLS // 2, 0, GRID_W - WIN_COLS)
    col_ok = (col[None, :] >= col_start[:, None]) & (col[None, :] < col_start[:, None] + WIN_COLS)
    dr = row_idx - r[:, None]
    dc = col[None, :] - col[:, None]
    ri = (dr + WIN_ROWS - 1)[:, None, :, None]
    ci = jnp.clip(dc + WIN_COLS - 1, 0, 2 * WIN_COLS - 2)[None, :, None, :]
    bias = _f32(rpb[:, ri, ci])
    scale = NA_HEAD_DIM ** -0.5
    s_band = _f32(jnp.einsum('brqhd,brkchd->bhrqkc', qg, k_band)) * scale + bias
    s_band = jnp.where(col_ok[:, None, :], s_band, -jnp.inf)
    s_ctx = _f32(jnp.einsum('brqhd,bnhd->bhrqn', qg, k_ctx)) * scale
    nb = kr * GRID_W
    s = jnp.concatenate([s_band.reshape(B, H, rows, GRID_W, nb), s_ctx], axis=-1)
    p = jax.nn.softmax(s, axis=-1).astype(v.dtype)
    p_band = p[..., :nb].reshape(B, H, rows, GRID_W, kr, GRID_W)
    o = jnp.einsum('bhrqkc,brkchd->brqhd', p_band, v_band) + jnp.einsum('bhrqn,bnhd->brqhd', p[..., nb:], v_ctx)
    return o.reshape(B, L, H * Dh)


def context_attention(q, k, v):
    s = _f32(jnp.einsum('bnhd,bmhd->bhnm', q, k)) * (NA_HEAD_DIM ** -0.5)
    p = jax.nn.softmax(s, axis=-1).astype(v.dtype)
    o = jnp.einsum('bhnm,bmhd->bnhd', p, v)
    return o.reshape(o.shape[0], o.shape[1], NA_WIDTH)


def conv_module(a, g, dw_w, dw_b, ln_g, ln_b):
    u = a * jax.nn.sigmoid(g)
    u = lax.conv_general_dilated(u, dw_w[:, None, :], window_strides=(1,), padding=[(CONV_K // 2, CONV_K // 2)], dimension_numbers=('NWC', 'WIO', 'NWC'), feature_group_count=CONV_CH) + dw_b
    return jax.nn.silu(layernorm(u, ln_g, ln_b))


def na_conv_mixer(h_lat, h_ctx, w_in, q_g, k_g, rpb, dw_w, dw_b, ln_g, ln_b, w_out, with_ctx):
    def project(h):
        q, k, v, a, g = jnp.split(h @ w_in, [NA_WIDTH, 2 * NA_WIDTH, 3 * NA_WIDTH, 3 * NA_WIDTH + CONV_CH], axis=-1)
        heads = lambda t: t.reshape(t.shape[0], t.shape[1], NA_HEADS, NA_HEAD_DIM)
        return rmsnorm(heads(q), q_g), rmsnorm(heads(k), k_g), heads(v), a, g
    ql, kl, vl, al, gl = project(h_lat)
    qc, kc, vc, ac, gc = project(h_ctx)
    y_lat = jnp.concatenate([neighbourhood_attention(ql, kl, vl, kc, vc, rpb), conv_module(al, gl, dw_w, dw_b, ln_g, ln_b)], axis=-1) @ w_out
    if not with_ctx:
        return y_lat, None
    y_ctx = jnp.concatenate([context_attention(qc, kc, vc), conv_module(ac, gc, dw_w, dw_b, ln_g, ln_b)], axis=-1) @ w_out
    return y_lat, y_ctx


def s5_discretise(a_re, a_im, log_dt, b_re, b_im):
    lam = lax.complex(_f32(a_re), _f32(a_im))
    dt = jnp.exp(_f32(log_dt))[:, None]
    lam_bar = jnp.exp(lam * dt)
    b = lax.complex(_f32(b_re), _f32(b_im))
    return lam_bar, ((lam_bar - 1) / lam)[..., None] * b


def diag_scan(bu, lam_bar, reverse):
    a = jnp.broadcast_to(lam_bar, (1,) + bu.shape[1:])
    def combine(e1, e2):
        a1, b1 = e1
        a2, b2 = e2
        return a1 * a2, a2 * b1 + b2
    return lax.associative_scan(combine, (a, bu), reverse=reverse, axis=1)


def s5_mixer(h_lat, h_ctx, w_in, a_re, a_im, log_dt, b_re, b_im, c_re, c_im, d_skip, w_out, with_ctx):
    u_lat = _f32(h_lat @ w_in)
    u_ctx = _f32(h_ctx @ w_in)
    ul = u_lat.reshape(u_lat.shape[0], u_lat.shape[1], SSM_GROUPS, SSM_GROUP)
    uc = u_ctx.reshape(u_ctx.shape[0], u_ctx.shape[1], SSM_GROUPS, SSM_GROUP)
    lat_dirs, ctx_dirs = [], []
    for direction, reverse in ((0, False), (1, True)):
        lam_bar, b_bar = s5_discretise(a_re[direction], a_im[direction], log_dt[direction], b_re[direction], b_im[direction])
        c_mat = lax.complex(_f32(c_re[direction]), _f32(c_im[direction]))
        _, st_ctx = diag_scan(jnp.einsum('bngq,gpq->bngp', uc, b_bar), lam_bar, reverse)
        s0 = st_ctx[:, 0] if reverse else st_ctx[:, -1]
        powers, st_lat = diag_scan(jnp.einsum('blgq,gpq->blgp', ul, b_bar), lam_bar, reverse)
        st_lat = st_lat + powers * s0[:, None]
        lat_dirs.append(jnp.einsum('blgp,gqp->blgq', st_lat, c_mat).real)
        if with_ctx:
            ctx_dirs.append(jnp.einsum('bngp,gqp->bngq', st_ctx, c_mat).real)
    d = _f32(d_skip).reshape(SSM_GROUPS, SSM_GROUP)
    def readout(ys, u, dtype):
        y = jax.nn.gelu(ys[0] + ys[1] + d * u)
        y = y.reshape(y.shape[0], y.shape[1], SSM_WIDTH).astype(dtype)
        z = y @ w_out
        return z[..., :D_MODEL] * jax.nn.sigmoid(z[..., D_MODEL:])
    y_lat = readout(lat_dirs, ul, h_lat.dtype)
    if not with_ctx:
        return y_lat, None
    return y_lat, readout(ctx_dirs, uc, h_ctx.dtype)


def moe_swiglu(h, w_router, w1, w3, w2):
    shp = h.shape
    t = h.reshape(-1, shp[-1])
    logits = _f32(t @ w_router)
    top_val, top_idx = lax.top_k(logits, TOP_K)
    top_w = jax.nn.softmax(top_val, axis=-1)
    gate = jnp.einsum('tk,tke->te', top_w, jax.nn.one_hot(top_idx, N_EXPERTS, dtype=jnp.float32)).astype(h.dtype)
    out = gate[:, 0:1] * swiglu(t, w1[0], w3[0], w2[0])
    for e in range(1, N_EXPERTS):
        out = out + gate[:, e:e + 1] * swiglu(t, w1[e], w3[e], w2[e])
    return out.reshape(shp)


def setup_inputs(seed: int = 0) -> dict:
    key = jax.random.key(seed)
    ks = iter(jax.random.split(key, 48))
    def nrm(shape, std):
        return jax.random.normal(next(ks), shape, jnp.float32) * std
    D = D_MODEL
    ne, no = (DEPTH + 1) // 2, DEPTH // 2
    G, P, Q = SSM_GROUPS, SSM_STATE, SSM_GROUP
    x = nrm((BATCH, SEQ, D), 1.0)
    c = nrm((BATCH, D), 1.0)
    ctx = nrm((BATCH, CTX_LEN, D), 1.0)
    c_ctx = nrm((D,), 1.0)
    ada_w = nrm((DEPTH, D, 6 * D), 0.5 * D ** -0.5)
    ada_b = nrm((DEPTH, 6 * D), 0.01)
    norm1_g = 1.0 + nrm((DEPTH, D), 0.01)
    norm2_g = 1.0 + nrm((DEPTH, D), 0.01)
    mix_w_in = nrm((ne, D, MIX_IN), D ** -0.5)
    q_norm_g = 1.0 + nrm((ne, NA_HEAD_DIM), 0.01)
    k_norm_g = 1.0 + nrm((ne, NA_HEAD_DIM), 0.01)
    na_rpb = nrm((ne, NA_HEADS, 2 * WIN_ROWS - 1, 2 * WIN_COLS - 1), 0.02)
    conv_dw_w = nrm((ne, CONV_K, CONV_CH), CONV_K ** -0.5)
    conv_dw_b = nrm((ne, CONV_CH), 0.01)
    conv_ln_g = 1.0 + nrm((ne, CONV_CH), 0.01)
    conv_ln_b = nrm((ne, CONV_CH), 0.01)
    mix_w_out = nrm((ne, MIX_OUT, D), MIX_OUT ** -0.5)
    ffn_w1 = nrm((ne, D, FFN_DIM), D ** -0.5)
    ffn_w3 = nrm((ne, D, FFN_DIM), D ** -0.5)
    ffn_w2 = nrm((ne, FFN_DIM, D), FFN_DIM ** -0.5)
    ssm_w_in = nrm((no, D, SSM_WIDTH), D ** -0.5)
    ssm_a_re = -0.5 + nrm((no, 2, G, P), 0.01)
    ssm_a_im = math.pi * jnp.arange(P, dtype=jnp.float32) + nrm((no, 2, G, P), 0.01)
    ssm_log_dt = jax.random.uniform(next(ks), (no, 2, G), jnp.float32, math.log(1e-3), math.log(1e-1))
    ssm_b_re = nrm((no, 2, G, P, Q), (2 * Q) ** -0.5)
    ssm_b_im = nrm((no, 2, G, P, Q), (2 * Q) ** -0.5)
    ssm_c_re = nrm((no, 2, G, Q, P), (2 * P) ** -0.5)
    ssm_c_im = nrm((no, 2, G, Q, P), (2 * P) ** -0.5)
    ssm_d = nrm((no, SSM_WIDTH), 1.0)
    ssm_w_out = nrm((no, SSM_WIDTH, 2 * D), SSM_WIDTH ** -0.5)
    moe_router = nrm((no, D, N_EXPERTS), D ** -0.5)
    moe_w1 = nrm((no, N_EXPERTS, D, EXPERT_DIM), D ** -0.5)
    moe_w3 = nrm((no, N_EXPERTS, D, EXPERT_DIM), D ** -0.5)
    moe_w2 = nrm((no, N_EXPERTS, EXPERT_DIM, D), EXPERT_DIM ** -0.5)
    return {'x': x, 'c': c, 'ctx': ctx, 'c_ctx': c_ctx, 'ada_w': ada_w, 'ada_b': ada_b, 'norm1_g': norm1_g, 'norm2_g': norm2_g, 'mix_w_in': mix_w_in, 'q_norm_g': q_norm_g, 'k_norm_g': k_norm_g, 'na_rpb': na_rpb, 'conv_dw_w': conv_dw_w, 'conv_dw_b': conv_dw_b, 'conv_ln_g': conv_ln_g, 'conv_ln_b': conv_ln_b, 'mix_w_out': mix_w_out, 'ffn_w1': ffn_w1, 'ffn_w3': ffn_w3, 'ffn_w2': ffn_w2, 'ssm_w_in': ssm_w_in, 'ssm_a_re': ssm_a_re, 'ssm_a_im': ssm_a_im, 'ssm_log_dt': ssm_log_dt, 'ssm_b_re': ssm_b_re, 'ssm_b_im': ssm_b_im, 'ssm_c_re': ssm_c_re, 'ssm_c_im': ssm_c_im, 'ssm_d': ssm_d, 'ssm_w_out': ssm_w_out, 'moe_router': moe_router, 'moe_w1': moe_w1, 'moe_w3': moe_w3, 'moe_w2': moe_w2}


def reference(x, c, ctx, c_ctx, ada_w, ada_b, norm1_g, norm2_g, mix_w_in, q_norm_g, k_norm_g, na_rpb, conv_dw_w, conv_dw_b, conv_ln_g, conv_ln_b, mix_w_out, ffn_w1, ffn_w3, ffn_w2, ssm_w_in, ssm_a_re, ssm_a_im, ssm_log_dt, ssm_b_re, ssm_b_im, ssm_c_re, ssm_c_im, ssm_d, ssm_w_out, moe_router, moe_w1, moe_w3, moe_w2):
    x_lat, x_ctx = x, ctx
    for l in range(DEPTH):
        i = l // 2
        with_ctx = l < DEPTH - 1
        sh1, sc1, g1, sh2, sc2, g2 = adaln(c, ada_w[l], ada_b[l])
        csh1, csc1, cg1, csh2, csc2, cg2 = adaln(c_ctx[None, :], ada_w[l], ada_b[l])
        h_lat = modulate(rmsnorm(x_lat, norm1_g[l]), sh1, sc1)
        h_ctx = modulate(rmsnorm(x_ctx, norm1_g[l]), csh1, csc1)
        if l % 2 == 0:
            y_lat, y_ctx = na_conv_mixer(h_lat, h_ctx, mix_w_in[i], q_norm_g[i], k_norm_g[i], na_rpb[i], conv_dw_w[i], conv_dw_b[i], conv_ln_g[i], conv_ln_b[i], mix_w_out[i], with_ctx)
            def ffn(h):
                return swiglu(h, ffn_w1[i], ffn_w3[i], ffn_w2[i])
        else:
            y_lat, y_ctx = s5_mixer(h_lat, h_ctx, ssm_w_in[i], ssm_a_re[i], ssm_a_im[i], ssm_log_dt[i], ssm_b_re[i], ssm_b_im[i], ssm_c_re[i], ssm_c_im[i], ssm_d[i], ssm_w_out[i], with_ctx)
            def ffn(h):
                return moe_swiglu(h, moe_router[i], moe_w1[i], moe_w3[i], moe_w2[i])
        x_lat = x_lat + g1[:, None, :] * y_lat
        x_lat = x_lat + g2[:, None, :] * ffn(modulate(rmsnorm(x_lat, norm2_g[l]), sh2, sc2))
        if with_ctx:
            x_ctx = x_ctx + cg1[:, None, :] * y_ctx
            x_ctx = x_ctx + cg2[:, None, :] * ffn(modulate(rmsnorm(x_ctx, norm2_g[l]), csh2, csc2))
    return x_lat
```

```python
import contextlib
import math
import numpy as np
import concourse.bass as bass
import concourse.mybir as mybir
from concourse.bass_utils import run_bass_kernel_spmd

F32 = mybir.dt.float32
BF16 = mybir.dt.bfloat16
I32 = mybir.dt.int32
ALU = mybir.AluOpType
AF = mybir.ActivationFunctionType
AX = mybir.AxisListType

SEM_ROLL = 30000
D = 1024
KC = 8
L = 2048
NCTX = 256
T = L + NCTX
EPS = 1e-6
TBS = [(0, 512), (512, 512), (1024, 512), (1536, 512), (2048, 256)]
FFN_DIM = 2816
NEXP = 8
EXPERT_DIM = 3584
NEG = -30000.0
SLOW = dict(allow_slow_non_contiguous=True)


class Buf:
    __slots__ = ("name", "w", "r", "dsem", "dcnt")

    def __init__(self, name=""):
        self.name = name
        self.w = None
        self.r = []
        self.dsem = None
        self.dcnt = 0


class FW:
    ENGS = ("sync", "tensor", "vector", "scalar", "gpsimd")

    def __init__(self, nc):
        self.nc = nc
        self.es = contextlib.ExitStack()
        self.ops = {e: [] for e in self.ENGS}
        self.esem = {}
        self.ecnt = {e: 0 for e in self.ENGS}
        self.seen = {e: {} for e in self.ENGS}
        self.nsem = 0
        self.pending_dma = []
        for e in self.ENGS:
            self.esem[e] = self.new_sem("e_" + e)
        self.n_ops = 0

    def new_sem(self, name):
        self.nsem += 1
        return self.es.enter_context(self.nc.semaphore(f"{name}_{self.nsem}"))

    def sb(self, name, shape, dt):
        return self.es.enter_context(self.nc.sbuf_tensor(name, list(shape), dt))

    def ps(self, name, shape, dt):
        return self.es.enter_context(self.nc.psum_tensor(name, list(shape), dt))

    def _need(self, eng, tok, waits):
        if tok is None:
            return
        sem, val, weng = tok
        if weng == eng == "tensor":
            return
        k = id(sem)
        cur = self.seen[eng].get(k)
        if cur is not None and cur[1] >= val:
            return
        for i, (s, v) in enumerate(waits):
            if s is sem:
                if v < val:
                    waits[i] = (s, val)
                return
        waits.append((sem, val))

    def op(self, eng, fn, reads=(), writes=(), dma_dst=None):
        waits = []
        for b in reads:
            self._need(eng, b.w, waits)
        for b in writes:
            self._need(eng, b.w, waits)
            for t in b.r:
                self._need(eng, t, waits)
        for s, v in waits:
            self.seen[eng][id(s)] = (s, v)
        if dma_dst is not None:
            if dma_dst.dsem is None or dma_dst.dcnt + 16 > SEM_ROLL:
                dma_dst.dsem = self.new_sem("d_" + dma_dst.name)
                dma_dst.dcnt = 0
            dma_dst.dcnt += 16
            tok = (dma_dst.dsem, dma_dst.dcnt, "dma")
            inc = (dma_dst.dsem, 16)
            self.pending_dma.append(tok)
        else:
            if self.ecnt[eng] + 1 > SEM_ROLL:
                self.esem[eng] = self.new_sem("e_" + eng)
                self.ecnt[eng] = 0
            self.ecnt[eng] += 1
            tok = (self.esem[eng], self.ecnt[eng], eng)
            inc = (self.esem[eng], 1)
        for b in writes:
            b.w = tok
            b.r = []
        for b in reads:
            if b not in writes:
                b.r.append(tok)
        wl = list(waits)

        def emit(e, fn=fn, wl=wl, inc=inc):
            for s, v in wl:
                e.wait_ge(s, v)
            fn(e).then_inc(inc[0], inc[1])

        self.ops[eng].append(emit)
        self.n_ops += 1
        return tok

    def barrier(self):
        toks = [(self.esem[e], self.ecnt[e], e) for e in self.ENGS if self.ecnt[e] > 0]
        toks += self.pending_dma
        self.pending_dma = []
        for eng in self.ENGS:
            waits = []
            for t in toks:
                if t[2] == eng:
                    continue
                self._need(eng, t, waits)
            for s, v in waits:
                self.seen[eng][id(s)] = (s, v)
            wl = list(waits)

            def emit(e, wl=wl):
                for s, v in wl:
                    e.wait_ge(s, v)
            self.ops[eng].append(emit)

    def dma(self, out, in_, reads=(), writes=(), q="sync", **kw):
        return self.op(q, lambda e: e.dma_start(out=out, in_=in_, **kw),
                       reads=reads, writes=writes, dma_dst=writes[0])

    def mm(self, out, lhsT, rhs, start, stop, reads, writes, tp=None):
        if tp is not None:
            return self.op("tensor", lambda e: e.matmul(out, lhsT, rhs, start=start, stop=stop, tile_position=tp),
                           reads=reads, writes=writes)
        return self.op("tensor", lambda e: e.matmul(out, lhsT, rhs, start=start, stop=stop),
                       reads=reads, writes=writes)

    def tr(self, out, in_, ident, reads, writes):
        return self.op("tensor", lambda e: e.transpose(out, in_, ident), reads=reads, writes=writes)

    def act(self, out, in_, func, reads, writes, scale=1.0, bias=0.0):
        return self.op("scalar", lambda e: e.activation(out=out, in_=in_, func=func, scale=scale, bias=bias),
                       reads=reads, writes=writes)

    def tt(self, eng, out, in0, in1, op, reads, writes):
        return self.op(eng, lambda e: e.tensor_tensor(out, in0, in1, op), reads=reads, writes=writes)

    def ts(self, eng, out, in0, s1, s2, op0, op1, reads, writes):
        if s2 is None:
            return self.op(eng, lambda e: e.tensor_scalar(out, in0, s1, None, op0), reads=reads, writes=writes)
        return self.op(eng, lambda e: e.tensor_scalar(out, in0, s1, s2, op0, op1), reads=reads, writes=writes)

    def stt(self, out, in0, scalar, in1, op0, op1, reads, writes):
        return self.op("vector", lambda e: e.scalar_tensor_tensor(out, in0, scalar, in1, op0, op1),
                       reads=reads, writes=writes)

    def copy(self, eng, out, in_, reads, writes):
        if eng == "scalar":
            return self.act(out, in_, AF.Identity, reads, writes)
        return self.op(eng, lambda e: e.tensor_copy(out, in_), reads=reads, writes=writes)

    def memset(self, eng, ap, val, writes):
        return self.op(eng, lambda e: e.memset(ap, val), writes=writes)

    def finish(self, final_bufs):
        toks = [b.w for b in final_bufs if b.w is not None]
        ops = self.ops

        def fin(e):
            for s, v, _ in toks:
                e.wait_ge(s, v)
        ops["sync"].append(fin)
        with self.nc.Block() as block:
            @block.sync
            def _(e):
                for f in ops["sync"]:
                    f(e)

            @block.tensor
            def _(e):
                for f in ops["tensor"]:
                    f(e)

            @block.vector
            def _(e):
                for f in ops["vector"]:
                    f(e)

            @block.scalar
            def _(e):
                for f in ops["scalar"]:
                    f(e)

            @block.gpsimd
            def _(e):
                for f in ops["gpsimd"]:
                    f(e)
        self.es.close()


class Arena:
    def __init__(self, fw, nwords):
        self.t32 = fw.sb("arena", [128, nwords], F32)
        self.t16 = self.t32.bitcast(BF16)
        self.ti32 = self.t32.bitcast(I32)
        self.n = nwords
        self.top = 0
        self.peak = 0

    def ap32(self, off, dims):
        return bass.AP(self.t32, off, [[self.n, 128]] + [list(d) for d in dims])

    def ap16(self, off, dims):
        return bass.AP(self.t16, off, [[2 * self.n, 128]] + [list(d) for d in dims])

    def _take(self, nwords):
        self.last_off = self.top
        a = self.top
        self.top += nwords
        self.peak = max(self.peak, self.top)
        assert self.top <= self.n, f"arena overflow {self.top} > {self.n}"
        return a

    def f32(self, n):
        a = self._take(n)
        return self.t32[:, a:a + n]

    def i32(self, n):
        a = self._take(n)
        return self.ti32[:, a:a + n]

    def bf16(self, n):
        nw = (n + 1) // 2
        a = self._take(nw)
        return self.t16[:, 2 * a:2 * a + n]

    def mark(self):
        return self.top

    def release(self, m):
        self.top = m


class Prog:
    pass


def build_program(layers=(0, 1), tap=None, in_feature_major=False):
    nc = bass.Bass("TRN2", target_bir_lowering=False)
    fw = FW(nc)
    P = Prog()
    P.nc, P.fw = nc, fw

    def din(name, shape, dt=F32):
        return nc.dram_tensor(name, list(shape), dt, kind="ExternalInput").ap()

    I = {}
    I["x"] = din("x", [L, D])
    I["ctx"] = din("ctx", [NCTX, D])
    I["cc"] = din("cc", [2, D])
    I["ada_w"] = din("ada_w", [2, D, 6 * D])
    I["ada_b"] = din("ada_b", [2, 6 * D])
    I["norm1_g"] = din("norm1_g", [2, D])
    I["norm2_g"] = din("norm2_g", [2, D])
    if 0 in layers:
        I["mix_w_in"] = din("mix_w_in", [D, 2560])
        I["q_norm_g"] = din("q_norm_g", [64])
        I["k_norm_g"] = din("k_norm_g", [64])
        I["na_rpb"] = din("na_rpb", [8, 15, 31])
        I["conv_dw_w"] = din("conv_dw_w", [31, 512])
        I["conv_dw_b"] = din("conv_dw_b", [512])
        I["conv_ln_g"] = din("conv_ln_g", [512])
        I["conv_ln_b"] = din("conv_ln_b", [512])
        I["mix_w_out"] = din("mix_w_out", [D, D])
        I["ffn_w1"] = din("ffn_w1", [D, FFN_DIM])
        I["ffn_w3"] = din("ffn_w3", [D, FFN_DIM])
        I["ffn_w2"] = din("ffn_w2", [FFN_DIM, D])
    if 1 in layers:
        I["ssm_w_in"] = din("ssm_w_in", [D, D])
        I["ssm_a_re"] = din("ssm_a_re", [2, 64, 64])
        I["ssm_a_im"] = din("ssm_a_im", [2, 64, 64])
        I["ssm_log_dt"] = din("ssm_log_dt", [2, 64])
        I["ssm_b_re"] = din("ssm_b_re", [2, 64, 64, 16])
        I["ssm_b_im"] = din("ssm_b_im", [2, 64, 64, 16])
        I["ssm_c_re"] = din("ssm_c_re", [2, 64, 16, 64])
        I["ssm_c_im"] = din("ssm_c_im", [2, 64, 16, 64])
        I["ssm_d"] = din("ssm_d", [D])
        I["ssm_w_out"] = din("ssm_w_out", [D, 2 * D])
        I["moe_router"] = din("moe_router", [D, NEXP])
        I["moe_w1"] = din("moe_w1", [NEXP, D, EXPERT_DIM])
        I["moe_w3"] = din("moe_w3", [NEXP, D, EXPERT_DIM])
        I["moe_w2"] = din("moe_w2", [NEXP, EXPERT_DIM, D])
    P.I = I
    P.out_lat = nc.dram_tensor("out_lat", [L, D], F32, kind="ExternalOutput").ap()
    P.out_bufs = []
    if 1 not in layers:
        P.out_ctx = nc.dram_tensor("out_ctx", [NCTX, D], F32, kind="ExternalOutput").ap()
    P.tap = tap
    P.dbgsel = globals().get("DBGSEL", (0, 0, 0, 0))
    if tap is not None:
        P.dbg = nc.dram_tensor("dbg", [128, 8 * T], F32, kind="ExternalOutput").ap()
        P.bdbg = Buf("dbg")

    A = Arena(fw, 53150)
    P.A = A
    P.psum = [fw.ps(f"ps{i}", [128, 512], F32) for i in range(8)]
    P.bps = [Buf(f"ps{i}") for i in range(8)]

    setup_consts(P)
    P.hT = A.bf16(KC * T).rearrange("p (c t) -> p c t", c=KC)
    P.hT_off = A.last_off
    P.bh = [Buf(f"hT{i}") for i in range(5)]
    P.xT = None
    P.layers = layers
    done = False
    for l in layers:
        if l not in P.adaln_done and not (l == 0 and globals().get("ADA0_INTERLEAVE", False)):
            adaln(P, l)
            P.adaln_done.add(l)
        if l == 0:
            done = layer0(P)
        else:
            done = layer1(P)
        if done:
            break
    if not done:
        write_output(P)
    fw.finish(P.out_bufs + ([P.bdbg] if tap is not None else []))
    P.peak = A.peak
    return nc, P


def setup_consts(P):
    fw, A = P.fw, P.A
    P.bconst = Buf("const")
    bc = P.bconst
    io = A.f32(128)
    pio = A.f32(1)
    P.ident = A.f32(128)
    P.identb = A.bf16(128)
    P.onesD = A.f32(128)
    P.ones512 = A.f32(128)
    P.ones64 = A.f32(128)
    P.ones1 = A.bf16(128)
    P.iota_f = io
    P.pio = pio
    P.halfpi = A.f32(1)
    fw.memset("vector", P.halfpi, math.pi / 2.0, [bc])
    fw.op("gpsimd", lambda e: e.iota(io, [[1, 128]], base=0, channel_multiplier=0,
                                     allow_small_or_imprecise_dtypes=True), writes=[bc])
    fw.op("gpsimd", lambda e: e.iota(pio, [[0, 1]], base=0, channel_multiplier=1,
                                     allow_small_or_imprecise_dtypes=True), writes=[bc])
    fw.ts("vector", P.ident, io, pio[:, 0:1], None, ALU.is_equal, None, [bc], [bc])
    fw.copy("vector", P.identb, P.ident, [bc], [bc])
    fw.memset("vector", P.onesD, 1.0 / D, [bc])
    fw.memset("vector", P.ones512, 1.0 / 512, [bc])
    fw.memset("vector", P.ones1, 1.0 / D, [bc])
    fw.memset("vector", P.ones64, 0.0, [bc])
    fw.memset("vector", P.ones64[0:64, 0:64], 1.0 / 64, [bc])
    fw.memset("vector", P.ones64[64:128, 64:128], 1.0 / 64, [bc])
    P.mod = [A.f32(96).rearrange("p (m s) -> p m s", s=2) for _ in range(2)]
    P.modA = [A.f32(32).rearrange("p (n c s) -> p n c s", n=2, s=2) for _ in range(2)]
    P.gn = A.f32(32).rearrange("p (l n c) -> p l n c", l=2, n=2)
    P.bmod = Buf("mod")
    P.adaln_done = set()
    m_g = A.mark()
    gnat = A.f32(128)
    for n_, nm in enumerate(("norm1_g", "norm2_g")):
        fw.dma(gnat[n_ * 16:(n_ + 1) * 16, :], P.I[nm].rearrange("l (c p) -> (l c) p", p=128), writes=[P.bmod])
    fw.tr(P.psum[0][:, 0:32], gnat[0:32, :], P.ident[0:32, 0:32], [P.bmod, bc], [P.bps[0]])
    for n_ in range(2):
        fw.copy("vector", P.gn[:, :, n_, :], P.psum[0][:, n_ * 16:(n_ + 1) * 16].rearrange("p (l c) -> p l c", l=2),
                [P.bps[0]], [P.bmod])
    fw.barrier()
    A.release(m_g)


def adaln(P, l, hold=False, banks=(7, 6)):
    fw, A, I = P.fw, P.A, P.I
    m0 = A.mark()
    ccT = A.f32(16).rearrange("p (c s) -> p c s", s=2)
    scT = A.f32(16).rearrange("p (c s) -> p c s", s=2)
    adab = A.f32(48)
    bcc = Buf("cc")
    pan = [A.bf16(8 * 512).rearrange("p (k n) -> p k n", k=8) for _ in range(3)]
    bpan = [Buf("adapan0"), Buf("adapan1"), Buf("adapan2")]
    scTb = A.bf16(16).rearrange("p (c s) -> p c s", s=2)
    nat = A.f32(128)
    fw.dma(nat[0:16, :], I["cc"].rearrange("s (c p) -> (s c) p", p=128), writes=[bcc])
    fw.dma(nat[16:64, :], I["ada_b"][l].rearrange("(m p) -> m p", p=128), writes=[bcc])
    fw.tr(P.psum[banks[1]][:, 0:64], nat[0:64, :], P.ident[0:64, 0:64], [bcc, P.bconst], [P.bps[banks[1]]])
    fw.copy("vector", ccT, P.psum[banks[1]][:, 0:16].rearrange("p (s c) -> p c s", s=2), [P.bps[banks[1]]], [bcc])
    fw.copy("vector", adab, P.psum[banks[1]][:, 16:64], [P.bps[banks[1]]], [bcc])
    fw.act(scT, ccT, AF.Silu, [bcc], [bcc])
    fw.copy("vector", scTb, scT, [bcc], [bcc])
    wv = I["ada_w"][l].rearrange("(k p) n -> p k n", p=128)
    ps = P.psum[banks[0]]
    bps = P.bps[banks[0]]
    for pi in range(12):
        j = pi % 3
        fw.dma(pan[j], wv[:, :, pi * 512:(pi + 1) * 512], writes=[bpan[j]], q="gpsimd")
        for mi in range(4):
            m = pi * 4 + mi
            for k in range(8):
                fw.mm(ps[:, 2 * m:2 * m + 2], pan[j][:, k, mi * 128:(mi + 1) * 128], scTb[:, k, :],
                      k == 0, k == 7, [bpan[j], bcc], [bps])
    mod = P.mod[l]
    for s in range(2):
        fw.tt("vector", mod[:, :, s], ps[:, 0:96].rearrange("p (m s) -> p m s", s=2)[:, :, s], adab, ALU.add,
              [bps, bcc], [P.bmod])
    for n in range(2):
        for s in range(2):
            sc = mod[:, (3 * n + 1) * 8:(3 * n + 2) * 8, s]
            fw.stt(P.modA[l][:, n, :, s], sc, 1.0, P.gn[:, l, n, :], ALU.add, ALU.mult, [P.bmod], [P.bmod])
    if hold:
        return
    fw.barrier()
    A.release(m0)


def mod_cols(P, l, j, c, s):
    return P.mod[l][:, j * 8 + c, s:s + 1]


def load_xT_block(P, tb, dst, bdst, ev=0):
    fw, A = P.fw, P.A
    t0, tl = TBS[tb]
    for tt in range(tl // 128):
        j = P.xin_i % 2
        P.xin_i += 1
        tok = t0 + tt * 128
        src = P.I["x"][tok:tok + 128, :] if tok < L else P.I["ctx"][tok - L:tok - L + 128, :]
        fw.dma(P.xin[j], src, writes=[P.bxin[j]])
        for half in range(2):
            pb = P.tps[P.tps_i % 2]
            ps, bps = P.psum[pb], P.bps[pb]
            P.tps_i += 1
            for cc in range(4):
                c = half * 4 + cc
                fw.tr(ps[:, cc * 128:(cc + 1) * 128], P.xin[j][:, c * 128:(c + 1) * 128], P.ident,
                      [P.bxin[j], P.bconst], [bps])
            eng = "vector" if (P.tps_i % 2) else "scalar"
            fw.copy(eng, dst[:, half * 4:half * 4 + 4, tt * 128:(tt + 1) * 128],
                    ps[:, 0:512].rearrange("p (c t) -> p c t", c=4), [bps], [bdst])


def norm_mod_block(P, l, n, tb, xsrc, bx, scr):
    fw = P.fw
    t0, tl = TBS[tb]
    s = 1 if tb == 4 else 0
    sq, sd, tmp, bsq, bsd, btmp = scr
    pb = 6
    ps, bps = P.psum[pb], P.bps[pb]
    for c in range(KC):
        fw.act(sq[c % 2][:, 0:tl], xsrc[:, c, 0:tl], AF.Square, [bx], [bsq[c % 2]])
        fw.mm(ps[:, 0:tl], P.ones1, sq[c % 2][:, 0:tl], c == 0, c == KC - 1, [bsq[c % 2], P.bconst], [bps])
    fw.act(sd[:, 0:tl], ps[:, 0:tl], AF.Sqrt, [bps], [bsd], bias=P.epsc[:, 0:1])
    fw.op("vector", lambda e: e.reciprocal(sd[:, 0:tl], sd[:, 0:tl]), reads=[bsd], writes=[bsd])
    for c in range(KC):
        j = c % 2
        fw.stt(tmp[j][:, 0:tl], xsrc[:, c, 0:tl], P.modA[l][:, n, c, s:s + 1], sd[:, 0:tl], ALU.mult, ALU.mult,
               [bx, bsd, P.bmod], [btmp[j]])
        fw.act(P.hT[:, c, t0:t0 + tl], tmp[j][:, 0:tl], AF.Identity, [btmp[j], P.bmod], [P.bh[tb]],
               bias=mod_cols(P, l, 3 * n, c, s))


def norm_scratch(P):
    A = P.A
    sq = [A.bf16(512), A.bf16(512)]
    sd = A.f32(512)
    tmp = [A.f32(512), A.f32(512)]
    return (sq, sd, tmp, [Buf("sq0"), Buf("sq1")], Buf("sd"), [Buf("tmp0"), Buf("tmp1")])


def tap_out(P, ap2d, ncols, bufs):
    P.fw.dma(P.dbg[:, 0:ncols], ap2d, reads=bufs, writes=[P.bdbg])


def tap_bf16(P, src3, n, bufs):
    fw, A = P.fw, P.A
    m = A.mark()
    t = A.f32(2304)
    bt = Buf("tapt")
    for c in range(n // 2304 if n >= 2304 else 1):
        w = min(n, 2304)
        fw.copy("vector", t[:, 0:w], src3[:, c * 2304:c * 2304 + w], bufs, [bt])
        fw.dma(P.dbg[:, c * 2304:c * 2304 + w], t[:, 0:w], reads=[bt], writes=[P.bdbg])
    A.release(m)


def layer0(P):
    fw, A, I = P.fw, P.A, P.I
    l = 0
    if not hasattr(P, "epsc"):
        P.epsc = A.f32(1)
        fw.memset("vector", P.epsc, EPS, [P.bconst])
    mM = A.mark()
    P.xin = [A.f32(D), A.f32(D)]
    P.bxin = [Buf("xin0"), Buf("xin1")]
    P.xin_i = 0
    P.tps = [4, 5]
    P.tps_i = 0
    xblk = [A.f32(KC * 512).rearrange("p (c t) -> p c t", c=KC) for _ in range(2)]
    bxb = [Buf("xblk0"), Buf("xblk1")]
    scr = norm_scratch(P)

    def phase0():
        for tb in range(5):
            j = tb % 2
            load_xT_block(P, tb, xblk[j], bxb[j])
            norm_mod_block(P, l, 0, tb, xblk[j], bxb[j], scr)
    if 0 not in P.adaln_done:
        def rec0(fn, *a_, **k_):
            calls = []
            real = fw.op
            fw.op = lambda *aa, **kk: calls.append((aa, kk))
            try:
                fn(*a_, **k_)
            finally:
                fw.op = real
            return calls
        cb = rec0(adaln, P, 0, hold=True, banks=(7, 3))
        P.adaln_done.add(0)
        ca = rec0(phase0)
        na, nb = len(ca), len(cb)
        ia = ib = 0
        while ia < na or ib < nb:
            if ia >= na or (ib < nb and ib * max(na, 1) <= ia * max(nb, 1) * 3):
                aa, kk = cb[ib]; ib += 1
            else:
                aa, kk = ca[ia]; ia += 1
            fw.op(*aa, **kk)
    else:
        phase0()
    if P.tap == "h1_0":
        tap_bf16(P, P.hT.rearrange("p c t -> p (c t)"), KC * T, P.bh)
        return True
    fw.barrier()
    A.release(mM)
    qkT = A.bf16(8 * T).rearrange("p (c t) -> p c t", c=8)
    bqk = [[Buf(f"qk{c}_{tb}") for tb in range(5)] for c in range(8)]
    V = A.bf16(18 * 8 * 65).rearrange("p (j h d) -> p j h d", j=18, h=8)
    bV = [Buf(f"V{j}") for j in range(18)]
    mB3 = A.mark()
    ULEN = 2078 + 286
    u = A.bf16(4 * ULEN).rearrange("p (c t) -> p c t", c=4)
    bu = [[Buf(f"u{c}_{tb}") for tb in range(5)] for c in range(4)]
    mB2 = A.mark()
    pans = [A.bf16(8 * 512).rearrange("p (k n) -> p k n", k=8) for _ in range(4)]
    bpan = [Buf(f"pan{i}") for i in range(4)]
    sq = [A.bf16(512) for _ in range(2)]
    bsq = [Buf("qsq0"), Buf("qsq1")]
    ones64b = A.bf16(128)
    fw.copy("vector", ones64b, P.ones64, [P.bconst], [P.bconst])
    sd = [A.f32(512) for _ in range(2)]
    bsd = [Buf("qsd0"), Buf("qsd1")]
    gq = A.f32(1)
    gk = A.f32(1)
    bg = Buf("gqk")
    for hh in range(2):
        fw.dma(gq[hh * 64:(hh + 1) * 64, :], I["q_norm_g"].rearrange("(d o) -> d o", o=1), writes=[bg], **SLOW)
        fw.dma(gk[hh * 64:(hh + 1) * 64, :], I["k_norm_g"].rearrange("(d o) -> d o", o=1), writes=[bg], **SLOW)
    fw.ts("vector", gq, gq, 0.125, None, ALU.mult, None, [bg], [bg])
    fw.memset("vector", V[:, :, :, 64:65], 1.0, bV)
    fw.memset("vector", u.rearrange("p c t -> p (c t)"), 0.0, [b for r in bu for b in r])
    wv = I["mix_w_in"].rearrange("(k p) n -> p k n", p=128)
    for pi in range(5):
        fw.dma(pans[pi % 4], wv[:, :, pi * 512:(pi + 1) * 512], writes=[bpan[pi % 4]], q="gpsimd")
        if pi == 3:
            break
    prj = [0, 1, 2, 3]
    prj_i = [0]
    st_i = [0]
    pending = [None]

    def qk_tail(args):
        c, tb, pb = args
        t0, tl = TBS[tb]
        ps, bps = P.psum[pb], P.bps[pb]
        sb_ = 4 + st_i[0] % 2
        j = st_i[0] % 2
        st_i[0] += 1
        ps2, bps2 = P.psum[sb_], P.bps[sb_]
        fw.mm(ps2[:, 0:tl], ones64b, sq[j][:, 0:tl], True, True, [bsq[j], P.bconst], [bps2])
        fw.act(sd[j][:, 0:tl], ps2[:, 0:tl], AF.Sqrt, [bps2], [bsd[j]], bias=P.epsc[:, 0:1])
        fw.op("vector", lambda e: e.reciprocal(sd[j][:, 0:tl], sd[j][:, 0:tl]), reads=[bsd[j]], writes=[bsd[j]])
        g = gq if c < 4 else gk
        fw.stt(qkT[:, c, t0:t0 + tl], ps[:, 0:tl], g[:, 0:1], sd[j][:, 0:tl], ALU.mult, ALU.mult,
               [bps, bsd[j], bg], [bqk[c][tb]])

    def b1_qk():
        for pi in range(2):
            pan, bp = pans[pi], bpan[pi]
            for tb in range(5):
                t0, tl = TBS[tb]
                for mc in range(4):
                    c = pi * 4 + mc
                    pb = prj[prj_i[0] % 4]
                    prj_i[0] += 1
                    ps, bps = P.psum[pb], P.bps[pb]
                    for k in range(KC):
                        fw.mm(ps[:, 0:tl], pan[:, k, mc * 128:(mc + 1) * 128], P.hT[:, k, t0:t0 + tl],
                              k == 0, k == KC - 1, [bp, P.bh[tb]], [bps])
                    j = (st_i[0] + (1 if pending[0] is not None else 0)) % 2
                    fw.act(sq[j][:, 0:tl], ps[:, 0:tl], AF.Square, [bps], [bsq[j]])
                    if pending[0] is not None:
                        qk_tail(pending[0])
                    pending[0] = (c, tb, pb)
        qk_tail(pending[0])
        pending[0] = None

    if 1 in P.layers and 1 not in P.adaln_done:
        def record_(fn, *a_, **k_):
            calls = []
            real = fw.op
            fw.op = lambda *aa, **kk: calls.append((aa, kk))
            try:
                fn(*a_, **k_)
            finally:
                fw.op = real
            return calls
        ca = record_(b1_qk)
        cb = record_(adaln, P, 1, hold=True, banks=(7, 6))
        P.adaln_done.add(1)
        na, nb = len(ca), len(cb)
        ia = ib = 0
        while ia < na or ib < nb:
            if ib >= nb or (ia < na and ia * max(nb, 1) <= ib * max(na, 1)):
                aa, kk = ca[ia]; ia += 1
            else:
                aa, kk = cb[ib]; ib += 1
            fw.op(*aa, **kk)
    else:
        b1_qk()
    pan, bp = pans[2], bpan[2]
    fw.dma(pans[0], wv[:, :, 4 * 512:5 * 512], writes=[bpan[0]], q="gpsimd")
    for tt in range(18):
        pb = prj[prj_i[0] % 4]
        prj_i[0] += 1
        ps, bps = P.psum[pb], P.bps[pb]
        tb = min(tt // 4, 4)
        for k in range(KC):
            fw.mm(ps[:, :], P.hT[:, k, tt * 128:(tt + 1) * 128], pan[:, k, :], k == 0, k == KC - 1,
                  [bp, P.bh[tb]], [bps])
        fw.copy("scalar" if tt % 2 else "vector", V[:, tt, :, 0:64], ps[:, :].rearrange("p (h d) -> p h d", h=8),
                [bps], [bV[tt]])
    sg = [A.f32(512) for _ in range(2)]
    bsg = [Buf("sg0"), Buf("sg1")]
    pa, bpa, pg, bpg = pans[3], bpan[3], pans[0], bpan[0]

    def uoff(tb):
        t0, tl = TBS[tb]
        return (15 + t0) if tb < 4 else (2078 + 15)
    for tb in range(5):
        t0, tl = TBS[tb]
        for mc in range(4):
            pba = prj[prj_i[0] % 4]
            pbg = prj[(prj_i[0] + 1) % 4]
            prj_i[0] += 2
            for (pn, bpn, pb) in ((pa, bpa, pba), (pg, bpg, pbg)):
                for k in range(KC):
                    fw.mm(P.psum[pb][:, 0:tl], pn[:, k, mc * 128:(mc + 1) * 128], P.hT[:, k, t0:t0 + tl],
                          k == 0, k == KC - 1, [bpn, P.bh[tb]], [P.bps[pb]])
            j = (tb * 4 + mc) % 2
            fw.act(sg[j][:, 0:tl], P.psum[pbg][:, 0:tl], AF.Sigmoid, [P.bps[pbg]], [bsg[j]])
            o = uoff(tb)
            fw.tt("vector", u[:, mc, o:o + tl], P.psum[pba][:, 0:tl], sg[j][:, 0:tl], ALU.mult,
                  [P.bps[pba], bsg[j]], [bu[mc][tb]])
    fw.barrier()
    A.release(mB2)
    catT = P.hT
    bcat = P.bh
    wrow = A.f32(512)
    cw = A.f32(4 * 31).rearrange("p (c k) -> p c k", c=4)
    cvec = A.f32(12).rearrange("p (v c) -> p v c", v=3)
    bcw = Buf("convw")
    fw.dma(wrow[0:31, :], I["conv_dw_w"], writes=[bcw])
    for vi, nm in enumerate(("conv_dw_b", "conv_ln_g", "conv_ln_b")):
        fw.dma(cvec[:, vi, :], I[nm].rearrange("(c p) -> p c", p=128), writes=[bcw], **SLOW)
    for c in range(4):
        fw.tr(P.psum[0][:, c * 32:c * 32 + 31], wrow[0:31, c * 128:(c + 1) * 128], P.ident[0:31, 0:31],
              [bcw, P.bconst], [P.bps[0]])
    fw.copy("vector", cw, P.psum[0][:, 0:128].rearrange("p (c k) -> p c k", c=4)[:, :, 0:31], [P.bps[0]], [bcw])
    acc = [A.f32(512) for _ in range(4)]
    bacc = [Buf(f"acc{c}") for c in range(4)]
    Dg = [[A.bf16(128) for _ in range(31)] for _ in range(4)]
    bDg = Buf("Dg")
    for c in range(4):
        for k in range(31):
            fw.ts("vector", Dg[c][k], P.identb, cw[:, c, k:k + 1], None, ALU.mult, None, [bcw, P.bconst], [bDg])
    csq = [A.f32(512) for _ in range(2)]
    bcsq = [Buf("csq0"), Buf("csq1")]
    mean = A.f32(512)
    msq = A.f32(512)
    rs = A.f32(512)
    bst = Buf("lnstat")
    t1 = [A.f32(512) for _ in range(2)]
    bt1 = [Buf("lt0"), Buf("lt1")]
    for tb in range(5):
        t0, tl = TBS[tb]
        ub = t0 if tb < 4 else 2078
        pm, bpm = P.psum[1 + 2 * (tb % 2)], P.bps[1 + 2 * (tb % 2)]
        pe2, bpe2 = P.psum[2 + 2 * (tb % 2)], P.bps[2 + 2 * (tb % 2)]
        for c in range(4):
            pcv = 5 + (tb * 4 + c) % 3
            for k in range(31):
                fw.mm(P.psum[pcv][:, 0:tl], Dg[c][k], u[:, c, ub + k:ub + k + tl], k == 0, k == 30,
                      [bu[c][tbb] for tbb in range(5)] + [bDg], [P.bps[pcv]])
            fw.act(acc[c][:, 0:tl], P.psum[pcv][:, 0:tl], AF.Identity, [P.bps[pcv], bcw], [bacc[c]],
                   bias=cvec[:, 0, c:c + 1])
            fw.mm(pm[:, 0:tl], P.ones512, acc[c][:, 0:tl], c == 0, c == 3, [bacc[c], P.bconst], [bpm])
            fw.act(csq[c % 2][:, 0:tl], acc[c][:, 0:tl], AF.Square, [bacc[c]], [bcsq[c % 2]])
            fw.mm(pe2[:, 0:tl], P.ones512, csq[c % 2][:, 0:tl], c == 0, c == 3, [bcsq[c % 2], P.bconst], [bpe2])
        fw.act(mean[:, 0:tl], pm[:, 0:tl], AF.Identity, [bpm], [bst])
        fw.act(msq[:, 0:tl], pm[:, 0:tl], AF.Square, [bpm], [bst])
        fw.tt("vector", rs[:, 0:tl], pe2[:, 0:tl], msq[:, 0:tl], ALU.subtract, [bpe2, bst], [bst])
        fw.ts("vector", rs[:, 0:tl], rs[:, 0:tl], 0.0, None, ALU.max, None, [bst], [bst])
        fw.act(rs[:, 0:tl], rs[:, 0:tl], AF.Sqrt, [bst], [bst], bias=P.epsc[:, 0:1])
        fw.op("vector", lambda e, tl=tl: e.reciprocal(rs[:, 0:tl], rs[:, 0:tl]), reads=[bst], writes=[bst])
        for c in range(4):
            j = c % 2
            fw.tt("vector", t1[j][:, 0:tl], acc[c][:, 0:tl], mean[:, 0:tl], ALU.subtract, [bacc[c], bst], [bt1[j]])
            fw.tt("vector", t1[j][:, 0:tl], t1[j][:, 0:tl], rs[:, 0:tl], ALU.mult, [bst], [bt1[j]])
            fw.act(catT[:, 4 + c, t0:t0 + tl], t1[j][:, 0:tl], AF.Silu, [bt1[j], bcw], [bcat[tb]],
                   scale=cvec[:, 1, c:c + 1], bias=cvec[:, 2, c:c + 1])
    fw.barrier()
    A.release(mB3)
    Fs = A.f32(127)
    bF = Buf("Fs")
    Fd = P.nc.dram_tensor("Fd_scratch", [120, 64, 127], F32).ap()
    Fs_off = A.last_off
    bFd = Buf("Fd")
    fw.memset("vector", Fs, 0.0, [bF])
    rp = I["na_rpb"]
    for h_ in range(8):
        fw.dma(Fs[h_ * 15:(h_ + 1) * 15, 48:79], bass.AP(rp.tensor, h_ * 15 * 31 + 30, [[31, 15], [-1, 31]]),
               writes=[bF], **SLOW)
    for h_ in range(8):
        fw.dma(Fd[h_ * 15:(h_ + 1) * 15], bass.AP(A.t32, Fs_off + h_ * 15 * A.n, [[A.n, 15], [0, 64], [1, 127]]),
               reads=[bF], writes=[bFd])
    TT = A.f32(8 * 14 * 64)
    TT_off = A.last_off
    TT4 = TT.rearrange("p (h e q) -> p h e q", h=8, e=14)
    bTT = Buf("TT")
    for h in range(8):
        for krl in range(2):
            src = bass.AP(Fd.tensor, (h * 15 + krl) * 64 * 127 + 63, [[126, 64], [64 * 127, 14], [1, 64]])
            fw.dma(TT4[krl * 64:(krl + 1) * 64, h, :, :], src, reads=[bFd], writes=[bTT])
    cm = A.f32(64)
    cm_off = A.last_off
    cs = A.f32(64)
    kcol = A.f32(1)
    bcm = Buf("cm")
    fw.op("gpsimd", lambda e: e.iota(kcol[0:64, :], [[0, 1]], base=0, channel_multiplier=1,
                                     allow_small_or_imprecise_dtypes=True), writes=[bcm])
    fw.op("gpsimd", lambda e: e.iota(kcol[64:128, :], [[0, 1]], base=0, channel_multiplier=1,
                                     allow_small_or_imprecise_dtypes=True), writes=[bcm])
    fw.ts("vector", cs, P.iota_f[:, 0:64], -8.0, 0.0, ALU.add, ALU.max, [P.bconst], [bcm])
    fw.ts("vector", cs, cs, 48.0, kcol[:, 0:1], ALU.min, ALU.subtract, [bcm], [bcm])
    fw.ts("vector", cm, cs, 0.0, None, ALU.is_le, None, [bcm], [bcm])
    fw.ts("vector", cs, cs, -15.0, None, ALU.is_ge, None, [bcm], [bcm])
    fw.tt("vector", cm, cm, cs, ALU.mult, [bcm], [bcm])
    fw.ts("vector", cm, cm, -NEG, NEG, ALU.mult, ALU.add, [bcm], [bcm])
    fw.tt("vector", TT.rearrange("p (m q) -> p m q", q=64), TT.rearrange("p (m q) -> p m q", q=64),
          A.ap32(cm_off, [[0, 112], [1, 64]]), ALU.add, [bcm, bTT], [bTT])
    ssb = [A.f32(320) for _ in range(2)]
    bssb = [Buf("ssb0"), Buf("ssb1")]
    pT = [A.bf16(448) for _ in range(3)]
    bpT = [Buf(f"pT{i}") for i in range(3)]
    rec = A.f32(8)
    A_rec_off = [A.last_off]
    brec = Buf("rec")
    osb = [A.bf16(512) for _ in range(2)]
    bosb = [Buf("osb0"), Buf("osb1")]
    its = []
    for qr in range(36):
        if qr < 32:
            ws = min(max(qr - 4, 0), 24)
            j0, j1 = ws // 2, (ws + 7) // 2
            nw = j1 - j0 + 1
            qtok = qr * 64
            e0 = 2 * j0 - qr + 7
            qtb = qr // 8
        else:
            ws, j0, nw, e0 = 0, 0, 0, 0
            qtok = L + (qr - 32) * 64
            qtb = 4
        for h in range(8):
            its.append((qr, h, ws, j0, nw, qtok, e0, qtb))

    def att_S(it):
        qr, h, ws, j0, nw, qtok, e0, qtb = its[it]
        pb = (h % 2) * 64
        cq, ck = h // 2, 4 + h // 2
        ps, bps = P.psum[it % 3], P.bps[it % 3]
        for i in range(nw + 2):
            ktok = (j0 + i) * 128 if i < nw else L + (i - nw) * 128
            ktb = min(ktok // 512, 4)
            fw.mm(ps[:, i * 64:(i + 1) * 64], qkT[pb:pb + 64, ck, ktok:ktok + 128],
                  qkT[pb:pb + 64, cq, qtok:qtok + 64], True, True, [bqk[ck][ktb], bqk[cq][qtb]], [bps])

    def att_SM(it):
        qr, h, ws, j0, nw, qtok, e0, qtb = its[it]
        ps, bps = P.psum[it % 3], P.bps[it % 3]
        ntile = nw + 2
        p_, bp_ = pT[it % 3], bpT[it % 3]
        if nw > 0:
            s_, bs_ = ssb[it % 2], bssb[it % 2]
            fw.tt("vector", s_[:, 0:nw * 64].rearrange("p (i q) -> p i q", q=64),
                  ps[:, 0:nw * 64].rearrange("p (i q) -> p i q", q=64),
                  A.ap32(TT_off + (h * 14 + e0) * 64, [[128, nw], [1, 64]]), ALU.add, [bps, bTT], [bs_])
            fw.act(p_[:, 0:nw * 64], s_[:, 0:nw * 64], AF.Exp, [bs_], [bp_])
        fw.act(p_[:, nw * 64:ntile * 64], ps[:, nw * 64:ntile * 64], AF.Exp, [bps], [bp_])

    def att_PV(it):
        qr, h, ws, j0, nw, qtok, e0, qtb = its[it]
        ntile = nw + 2
        p_, bp_ = pT[it % 3], bpT[it % 3]
        pso = [3 + 2 * (qr % 2), 4 + 2 * (qr % 2)]
        po, bpo = P.psum[pso[h // 4]], P.bps[pso[h // 4]]
        oc = (h % 4) * 65
        for i in range(ntile):
            if i < nw:
                vt = j0 + i
                r0, r1 = 0, 128
                if ws % 2 == 1 and i == 0:
                    r0 = 64
                if ws % 2 == 1 and i == nw - 1:
                    r1 = 64
            else:
                vt = 16 + (i - nw)
                r0, r1 = 0, 128
            fw.mm(po[0:64, oc:oc + 65], p_[r0:r1, i * 64:(i + 1) * 64], V[r0:r1, vt, h, :],
                  i == 0, i == ntile - 1, [bp_, bV[vt]], [bpo])

    def att_FIN(qr, qtok, qtb):
        pso = [3 + 2 * (qr % 2), 4 + 2 * (qr % 2)]
        ob, bob = osb[qr % 2], bosb[qr % 2]
        for half in range(2):
            po, bpo = P.psum[pso[half]], P.bps[pso[half]]
            pv = po[0:64, 0:260].rearrange("p (h d) -> p h d", h=4)
            fw.op("vector", lambda e, pv=pv, half=half: e.reciprocal(rec[0:64, half * 4:half * 4 + 4], pv[:, :, 64]),
                  reads=[bpo], writes=[brec])
            rb = bass.AP(A.t32, A_rec_off[0] + half * 4, [[A.n, 64], [1, 4], [0, 64]])
            fw.tt("vector", ob[0:64, half * 256:(half + 1) * 256].rearrange("p (h d) -> p h d", h=4),
                  pv[:, :, 0:64], rb, ALU.mult, [bpo, brec], [bob])
        pt, bpt = P.psum[7], P.bps[7]
        for c in range(4):
            fw.mm(pt[:, c * 64:(c + 1) * 64], ob[0:64, c * 128:(c + 1) * 128], P.identb[0:64, 0:64], True, True,
                  [bob, P.bconst], [bpt])
        fw.copy("scalar", catT[:, 0:4, qtok:qtok + 64], pt[:, 0:256].rearrange("p (c t) -> p c t", c=4),
                [bpt], [bcat[qtb]])

    NA = len(its)
    LOOK = 2
    for i in range(min(LOOK, NA)):
        att_S(i)
    pend_fin = None
    for it in range(NA):
        if it + LOOK < NA:
            att_S(it + LOOK)
        att_SM(it)
        att_PV(it)
        if pend_fin is not None:
            att_FIN(*pend_fin)
            pend_fin = None
        if its[it][1] == 7:
            pend_fin = (its[it][0], its[it][5], its[it][7])
    if pend_fin is not None:
        att_FIN(*pend_fin)
    fw.barrier()
    A.release(mM)
    xT = A.f32(KC * T).rearrange("p (c t) -> p c t", c=KC)
    P.xT = xT
    P.bx = [Buf(f"xT{i}") for i in range(5)]
    mX = A.mark()
    P.xin = [A.f32(D), A.f32(D)]
    P.bxin = [Buf("xin0"), Buf("xin1")]
    P.xin_i = 0
    P.tps = [4, 5]
    P.tps_i = 0
    wo = [A.bf16(8 * 512).rearrange("p (k n) -> p k n", k=8) for _ in range(2)]
    bwo = [Buf("wo0"), Buf("wo1")]
    wov = I["mix_w_out"].rearrange("(k p) n -> p k n", p=128)
    for i in range(2):
        fw.dma(wo[i], wov[:, :, i * 512:(i + 1) * 512], writes=[bwo[i]], q="gpsimd")
    for tb in range(5):
        t0, tl = TBS[tb]
        s_ = 1 if tb == 4 else 0
        load_xT_block(P, tb, xT[:, :, t0:t0 + tl], P.bx[tb])
        for m in range(8):
            pb = m % 4
            ps, bps = P.psum[pb], P.bps[pb]
            for k in range(KC):
                fw.mm(ps[:, 0:tl], wo[m // 4][:, k, (m % 4) * 128:(m % 4 + 1) * 128], catT[:, k, t0:t0 + tl],
                      k == 0, k == KC - 1, [bwo[m // 4], bcat[tb]], [bps])
            fw.stt(xT[:, m, t0:t0 + tl], ps[:, 0:tl], mod_cols(P, l, 2, m, s_), xT[:, m, t0:t0 + tl], ALU.mult, ALU.add,
                   [bps, P.bmod], [P.bx[tb]])
    fw.barrier()
    A.release(mX)
    if P.tap == "xmix0":
        tap_out(P, xT.rearrange("p c t -> p (c t)"), KC * T, P.bx)
        return True
    scr = norm_scratch(P)
    for tb in range(5):
        t0, tl = TBS[tb]
        norm_mod_block(P, l, 1, tb, xT[:, :, t0:t0 + tl], P.bx[tb], scr)
    fw.barrier()
    A.release(mX)
    fb = ffn_buffers(P, T)
    ffn(P, l, fb, I["ffn_w1"], I["ffn_w3"], I["ffn_w2"], FFN_DIM, [(0, 1024), (1024, 1024), (2048, 256)], 5)
    fw.barrier()
    A.release(mX)
    if P.tap == "xffn0":
        tap_out(P, xT.rearrange("p c t -> p (c t)"), KC * T, P.bx)
        return True
    return False


def ffn_buffers(P, ntok):
    A = P.A
    fb = Prog()
    fb.w1p = [A.bf16(8 * 512).rearrange("p (k n) -> p k n", k=8) for _ in range(2)]
    fb.w3p = [A.bf16(8 * 512).rearrange("p (k n) -> p k n", k=8) for _ in range(2)]
    fb.w2p = [A.bf16(4 * D).rearrange("p (f d) -> p f d", f=4) for _ in range(2)]
    fb.bw = [[Buf(f"w{n}p{i}") for i in range(2)] for n in range(3)]
    fb.act = A.bf16(4 * ntok).rearrange("p (f t) -> p f t", f=4)
    fb.bact = [[Buf(f"act{f}_{h}") for h in range(3)] for f in range(4)]
    fb.s = [A.bf16(1024) for _ in range(2)]
    fb.bs = [Buf("s0"), Buf("s1")]
    fb.a = [A.bf16(1024) for _ in range(2)]
    fb.ba = [Buf("a0"), Buf("a1")]
    fb.yi = 0
    fb.si = 0
    return fb


def ffn(P, l, fb, w1, w3, w2, F, halves, ntb, gate=None, bgate=None, first=True, nxt=None):
    fw = P.fw
    nfg = (F + 511) // 512
    w1v = w1.rearrange("(k p) n -> p k n", p=128)
    w3v = w3.rearrange("(k p) n -> p k n", p=128)

    def load(fg, wset):
        w1v_, w3v_, w2_ = wset
        j = fb.ldi % 2
        fb.ldi += 1
        nc_ = min(512, w2_.shape[0] - fg * 512)
        fw.dma(fb.w1p[j][:, :, 0:nc_], w1v_[:, :, fg * 512:fg * 512 + nc_], writes=[fb.bw[0][j]], q="gpsimd")
        fw.dma(fb.w3p[j][:, :, 0:nc_], w3v_[:, :, fg * 512:fg * 512 + nc_], writes=[fb.bw[1][j]], q="gpsimd")
        fw.dma(fb.w2p[j][:, 0:nc_ // 128, :], w2_[fg * 512:fg * 512 + nc_, :].rearrange("(f p) d -> p f d", p=128),
               writes=[fb.bw[2][j]], q="gpsimd")
    me = (w1v, w3v, w2)
    if first:
        fb.ldi = 0
        fb.usei = 0
        load(0, me)
    for fg in range(nfg):
        if fg + 1 < nfg:
            load(fg + 1, me)
        elif nxt is not None:
            n1, n3, n2 = nxt
            load(0, (n1.rearrange("(k p) n -> p k n", p=128), n3.rearrange("(k p) n -> p k n", p=128), n2))
        j = fb.usei % 2
        fb.usei += 1
        yre = globals().get("Y_REUSE", True) and ntb == 4
        ub = 4 * j if yre else 0
        ncol = min(512, F - fg * 512)
        nfc = ncol // 128
        for hi, (t0, tl) in enumerate(halves):
            nb = (tl + 511) // 512
            for fc in range(nfc):
                for (wp, bw, banks) in ((fb.w1p[j], fb.bw[0][j], (ub, ub + 1)), (fb.w3p[j], fb.bw[1][j], (ub + 2, ub + 3))):
                    for k in range(KC):
                        for b in range(nb):
                            bl = min(512, tl - b * 512)
                            tb = min((t0 + b * 512) // 512, 4)
                            fw.mm(P.psum[banks[b]][:, 0:bl], wp[:, k, fc * 128:(fc + 1) * 128],
                                  P.hT[:, k, t0 + b * 512:t0 + b * 512 + bl], k == 0, k == KC - 1,
                                  [bw, P.bh[tb]], [P.bps[banks[b]]])
                sj = fb.si % 2
                fb.si += 1
                for b in range(nb):
                    bl = min(512, tl - b * 512)
                    fw.act(fb.s[sj][:, b * 512:b * 512 + bl], P.psum[ub + b][:, 0:bl], AF.Silu, [P.bps[ub + b]], [fb.bs[sj]])
                for b in range(nb):
                    bl = min(512, tl - b * 512)
                    a0 = t0 + b * 512
                    if gate is None:
                        fw.tt("vector", fb.act[:, fc, a0:a0 + bl], P.psum[ub + 2 + b][:, 0:bl], fb.s[sj][:, b * 512:b * 512 + bl],
                              ALU.mult, [P.bps[ub + 2 + b], fb.bs[sj]], [fb.bact[fc][hi]])
                    else:
                        fw.tt("vector", fb.a[sj][:, b * 512:b * 512 + bl], P.psum[ub + 2 + b][:, 0:bl],
                              fb.s[sj][:, b * 512:b * 512 + bl], ALU.mult, [P.bps[ub + 2 + b], fb.bs[sj]], [fb.ba[sj]])
                if gate is not None:
                    fw.tt("vector", fb.act[:, fc, t0:t0 + tl], fb.a[sj][:, 0:tl], gate[:, t0:t0 + tl], ALU.mult,
                          [fb.ba[sj], bgate], [fb.bact[fc][hi]])
        if globals().get("Y_REUSE", True) and ntb == 4:
            for mi, m in enumerate(range(8)):
                base = (ub ^ 4) if (mi % 2 == 0) else ub
                for fc in range(nfc):
                    for tb in range(4):
                        t0, tl = TBS[tb]
                        hi = [i for i, (h0, hl) in enumerate(halves) if h0 <= t0 < h0 + hl][0]
                        fw.mm(P.psum[base + tb][:, 0:tl], fb.w2p[j][:, fc, m * 128:(m + 1) * 128],
                              fb.act[:, fc, t0:t0 + tl], fc == 0, fc == nfc - 1,
                              [fb.bw[2][j], fb.bact[fc][hi]], [P.bps[base + tb]])
                for tb in range(4):
                    t0, tl = TBS[tb]
                    fw.stt(P.xT[:, m, t0:t0 + tl], P.psum[base + tb][:, 0:tl], mod_cols(P, l, 5, m, 0),
                           P.xT[:, m, t0:t0 + tl], ALU.mult, ALU.add, [P.bps[base + tb], P.bmod], [P.bx[tb]])
            continue
        for m in range(8):
            for tb in range(ntb):
                t0, tl = TBS[tb]
                s_ = 1 if tb == 4 else 0
                hi = [i for i, (h0, hl) in enumerate(halves) if h0 <= t0 < h0 + hl][0]
                pb = 4 + fb.yi % 4
                fb.yi += 1
                ps, bps = P.psum[pb], P.bps[pb]
                for fc in range(nfc):
                    fw.mm(ps[:, 0:tl], fb.w2p[j][:, fc, m * 128:(m + 1) * 128], fb.act[:, fc, t0:t0 + tl],
                          fc == 0, fc == nfc - 1, [fb.bw[2][j], fb.bact[fc][hi]], [bps])
                fw.stt(P.xT[:, m, t0:t0 + tl], ps[:, 0:tl], mod_cols(P, l, 5, m, s_), P.xT[:, m, t0:t0 + tl],
                       ALU.mult, ALU.add, [bps, P.bmod], [P.bx[tb]])


def rev(ap):
    dims = [list(d) for d in ap.ap]
    st, n = dims[-1]
    dims[-1] = [-st, n]
    return bass.AP(ap.tensor, ap.offset + st * (n - 1), dims)


def s5_chunked(P):
    fw, A, I = P.fw, P.A, P.I
    hT = P.hT
    V_ = "vector"
    TWO_PI = 2.0 * math.pi
    TWO_PI_S = 6.2831845
    NCH = T // 8
    NL = L // 8
    bprm = Buf("s5prm")
    B1 = [bprm]
    ident, identb = P.ident, P.identb

    def AP(v, dims, off=0):
        return bass.AP(v.tensor, v.offset + off, [list(v.ap[0])] + [list(d_) for d_ in dims])

    U8 = A.bf16(64 * NCH).rearrange("p (g c) -> p g c", g=64)
    bU8 = [Buf(f"U8_{g}") for g in range(64)]
    mW = A.mark()
    wi = [A.bf16(8 * 512).rearrange("p (k n) -> p k n", k=8) for _ in range(2)]
    bwi = [Buf("wi0"), Buf("wi1")]
    wiv = I["ssm_w_in"].rearrange("(k p) n -> p k n", p=128)
    for i in range(2):
        fw.dma(wi[i], wiv[:, :, i * 512:(i + 1) * 512], writes=[bwi[i]], q="gpsimd")
    utok = A.bf16(8 * D).rearrange("p (g s x) -> p g s x", g=64, s=8)
    butok = Buf("utok")
    pi = 0
    for (c0, M) in ((0, 128), (128, 128), (256, 32)):
        for s_ in range(8):
            for half in range(2):
                pb = pi % 4
                pi += 1
                t_lo = c0 * 8 + s_
                tbs = sorted(set(min(tt // 512, 4) for tt in (t_lo, t_lo + 8 * (M - 1))))
                for k in range(KC):
                    fw.mm(P.psum[pb][0:M, :], hT[:, k, t_lo:t_lo + 8 * (M - 1) + 1:8], wi[half][:, k, :], k == 0, k == KC - 1,
                          [P.bh[tb] for tb in range(tbs[0], tbs[-1] + 1)] + [bwi[half]], [P.bps[pb]])
                fw.copy("scalar" if pi % 2 else "vector", utok[0:M, half * 32:(half + 1) * 32, s_, :],
                        P.psum[pb][0:M, :].rearrange("p (g x) -> p g x", x=16), [P.bps[pb]], [butok])
        for g in range(64):
            pb = 4 + g % 4
            fw.mm(P.psum[pb][:, 0:M], utok[0:M, g, :, :].rearrange("p s x -> p (s x)"), identb[0:M, 0:M], True, True,
                  [butok, P.bconst], [P.bps[pb]])
            fw.copy("scalar" if g % 2 else "vector", U8[:, g, c0:c0 + M], P.psum[pb][:, 0:M], [P.bps[pb]], [bU8[g]])
    fw.barrier()
    A.release(mW)
    pw = [A.bf16(2048).rearrange("p (m e) -> p m e", e=16) for _ in range(2)]
    bb = [A.bf16(2048).rearrange("p (m x) -> p m x", x=16) for _ in range(2)]
    ctm = [A.bf16(2048).rearrange("p (m x) -> p m x", x=16) for _ in range(3)]
    thp8 = A.f32(64).rearrange("p (d s) -> p d s", d=2)
    r8 = A.f32(64).rearrange("p (d s) -> p d s", d=2)
    dvec = A.f32(64)
    Wsel = [A.t16[:, 2 * (P.hT_off + t_ * 1152 + 1024):2 * (P.hT_off + t_ * 1152 + 1024) + 240] for t_ in range(8)]
    mask = [A.f32(128), A.f32(128)]
    iotaC = [A.f32(NCH), A.f32(NCH)]
    XLre = [[A.bf16(128) for _ in range(2)] for _ in range(2)]
    XLim = [[A.bf16(128) for _ in range(2)] for _ in range(2)]
    DLre = [[A.bf16(128) for _ in range(2)] for _ in range(2)]
    DLim = [[A.bf16(128) for _ in range(2)] for _ in range(2)]
    Mg = [[A.bf16(128) for _ in range(2)] for _ in range(2)]
    bMat = [[Buf(f"mat{s_}{g_}") for g_ in range(2)] for s_ in range(2)]
    ybf = A.bf16(8 * NL).rearrange("p (g c) -> p g c", g=8)
    bybf = [Buf(f"ybf{g}") for g in range(8)]
    mW = A.mark()
    a_nat = A.f32(512).rearrange("p (i c) -> p i c", i=4)
    aT = A.f32(256).rearrange("p (i g) -> p i g", i=4)
    for ai, nm in enumerate(("ssm_a_re", "ssm_a_im")):
        for d in range(2):
            for rep in range(2):
                fw.dma(a_nat[0:64, ai * 2 + d, rep * 64:(rep + 1) * 64], I[nm][d], writes=[bprm])
    for idx in range(4):
        fw.tr(P.psum[idx][:, 0:64], a_nat[0:64, idx, :], ident[0:64, 0:64], B1 + [P.bconst], [P.bps[idx]])
        fw.copy(V_, aT[:, idx, :], P.psum[idx][:, 0:64], [P.bps[idx]], B1)
    are = aT[:, 0:2, :].rearrange("p d g -> p (d g)")
    aim = aT[:, 2:4, :].rearrange("p d g -> p (d g)")
    ldtb = A.f32(128)
    fw.dma(ldtb, bass.AP(I["ssm_log_dt"].tensor, 0, [[0, 128], [1, 128]]), writes=[bprm])
    dtb = A.f32(128); xre = A.f32(128); thp = A.f32(128)
    fw.act(dtb, ldtb, AF.Exp, B1, B1)
    fw.tt(V_, xre, are, dtb, ALU.mult, B1, B1)
    fw.tt(V_, thp, aim, dtb, ALU.mult, B1, B1)
    fw.ts(V_, thp, thp, 1.0 / TWO_PI, None, ALU.mult, None, B1, B1)
    evec = A.f32(16)
    fw.op("gpsimd", lambda e: e.iota(evec, [[1, 16]], base=-7, channel_multiplier=0,
                                     allow_small_or_imprecise_dtypes=True), writes=B1)
    ho = P.hT_off
    ang = A.t32[:, ho:ho + 2048]; ki = A.ti32[:, ho + 2048:ho + 4096]
    kf = A.t32[:, ho + 4096:ho + 6144]; mexp = A.t32[:, ho + 6144:ho + 8192]
    ang3 = ang.rearrange("p (m e) -> p m e", e=16)
    mexp3 = mexp.rearrange("p (m e) -> p m e", e=16)
    fw.tt(V_, ang3, AP(thp, [[1, 128], [0, 16]]), AP(evec, [[0, 128], [1, 16]]), ALU.mult, B1, B1)
    fw.tt(V_, mexp3, AP(xre, [[1, 128], [0, 16]]), AP(evec, [[0, 128], [1, 16]]), ALU.mult, B1, B1)
    fw.copy(V_, ki, ang, B1, B1)
    fw.copy(V_, kf, ki, B1, B1)
    fw.tt(V_, ang, ang, kf, ALU.subtract, B1, B1)
    fw.act(kf, ang, AF.Sin, B1, B1, scale=TWO_PI_S)
    fw.act(ang, ang, AF.Abs, B1, B1)
    fw.act(ang, ang, AF.Sin, B1, B1, scale=-TWO_PI, bias=P.halfpi[:, 0:1])
    fw.act(mexp, mexp, AF.Exp, B1, B1)
    fw.tt(V_, pw[0].rearrange("p m e -> p (m e)"), mexp, ang, ALU.mult, B1, B1)
    fw.tt(V_, pw[1].rearrange("p m e -> p (m e)"), mexp, kf, ALU.mult, B1, B1)
    lr1 = A.f32(128); li1 = A.f32(128); nrm = A.f32(128); ta = A.f32(128); tb_ = A.f32(128)
    kre = A.f32(128); kim = A.f32(128)
    kf3 = kf.rearrange("p (m e) -> p m e", e=16)
    fw.tt(V_, lr1, mexp3[:, :, 8], ang3[:, :, 8], ALU.mult, B1, B1)
    fw.ts(V_, lr1, lr1, -1.0, None, ALU.add, None, B1, B1)
    fw.tt(V_, li1, mexp3[:, :, 8], kf3[:, :, 8], ALU.mult, B1, B1)
    fw.tt(V_, nrm, are, are, ALU.mult, B1, B1)
    fw.tt(V_, ta, aim, aim, ALU.mult, B1, B1)
    fw.tt(V_, nrm, nrm, ta, ALU.add, B1, B1)
    fw.op(V_, lambda e: e.reciprocal(nrm, nrm), reads=B1, writes=B1)
    fw.tt(V_, ta, lr1, are, ALU.mult, B1, B1)
    fw.tt(V_, tb_, li1, aim, ALU.mult, B1, B1)
    fw.tt(V_, ta, ta, tb_, ALU.add, B1, B1)
    fw.tt(V_, kre, ta, nrm, ALU.mult, B1, B1)
    fw.tt(V_, ta, li1, are, ALU.mult, B1, B1)
    fw.tt(V_, tb_, lr1, aim, ALU.mult, B1, B1)
    fw.tt(V_, ta, ta, tb_, ALU.subtract, B1, B1)
    fw.tt(V_, kim, ta, nrm, ALU.mult, B1, B1)
    bn = [A.t32[:, ho + 2048:ho + 4096].rearrange("p (m x) -> p m x", x=16),
          A.t32[:, ho + 4096:ho + 6144].rearrange("p (m x) -> p m x", x=16)]
    for ri, nm in enumerate(("ssm_b_re", "ssm_b_im")):
        srcb = I[nm].rearrange("d g p x -> p (d g) x")
        for rep in range(2):
            for q8 in range(8):
                fw.dma(bn[ri][rep * 64:(rep + 1) * 64, q8 * 16:(q8 + 1) * 16, :], srcb[:, q8 * 16:(q8 + 1) * 16, :], writes=[bprm])
    t3 = ang3
    t4 = mexp3
    kreb = AP(kre, [[1, 128], [0, 16]])
    kimb = AP(kim, [[1, 128], [0, 16]])
    fw.tt(V_, t3, bn[0], kreb, ALU.mult, B1, B1)
    fw.tt(V_, t4, bn[1], kimb, ALU.mult, B1, B1)
    fw.tt(V_, t3, t3, t4, ALU.subtract, B1, B1)
    fw.copy(V_, bb[0][0:64], t3[0:64], B1, B1)
    fw.copy(V_, bb[1][64:128], t3[64:128], B1, B1)
    fw.tt(V_, t3, bn[1], kreb, ALU.mult, B1, B1)
    fw.tt(V_, t4, bn[0], kimb, ALU.mult, B1, B1)
    fw.tt(V_, t3, t3, t4, ALU.add, B1, B1)
    fw.copy(V_, bb[0][64:128], t3[64:128], B1, B1)
    fw.ts(V_, bb[1][0:64], t3[0:64], -1.0, None, ALU.mult, None, B1, B1)
    Cn = A.t32[:, ho:ho + 2048].rearrange("p (m c) -> p m c", m=16)
    for ri, nm in enumerate(("ssm_c_re", "ssm_c_im")):
        src = I[nm].rearrange("d g q p -> (d g q) p").rearrange("(m r) p -> r m p", r=128)
        for dup in range(2):
            for hm in range(2):
                fw.dma(Cn[:, hm * 8:(hm + 1) * 8, dup * 64:(dup + 1) * 64], src[:, hm * 8:(hm + 1) * 8, :], writes=[bprm])
        cv = A.t16[:, 2 * (ho + 4096 + 1024 * ri):2 * (ho + 4096 + 1024 * ri) + 2048]
        for mm_ in range(16):
            pb = mm_ % 4
            fw.tr(P.psum[pb][:, 0:128], Cn[:, mm_, :], ident, B1 + [P.bconst], [P.bps[pb]])
            fw.copy("scalar", cv[:, mm_ * 128:(mm_ + 1) * 128], P.psum[pb][:, 0:128], [P.bps[pb]], B1)
    cre_ = A.t16[:, 2 * (ho + 4096):2 * (ho + 4096) + 2048]
    cim_ = A.t16[:, 2 * (ho + 5120):2 * (ho + 5120) + 2048]
    cA, cB, cC = (ctm[i].rearrange("p m x -> p (m x)") for i in range(3))
    fw.copy(V_, cA[0:64], cre_[0:64], B1, B1)
    fw.ts(V_, cA[64:128], cim_[64:128], -1.0, None, ALU.mult, None, B1, B1)
    fw.ts(V_, cB[0:64], cim_[0:64], -1.0, None, ALU.mult, None, B1, B1)
    fw.ts(V_, cB[64:128], cre_[64:128], -1.0, None, ALU.mult, None, B1, B1)
    fw.ts(V_, cC[0:64], cre_[0:64], -1.0, None, ALU.mult, None, B1, B1)
    fw.copy(V_, cC[64:128], cre_[64:128], B1, B1)
    rows = A.f32(128)
    prm = A.f32(128)
    prm4 = prm.rearrange("p (a d s) -> p a d s", a=2, d=2)
    ldt = A.f32(64).rearrange("p (d s) -> p d s", d=2)
    for a_i, nm in enumerate(("ssm_a_re", "ssm_a_im")):
        for d in range(2):
            r0 = (a_i * 2 + d) * 32
            fw.dma(rows[r0:r0 + 32, :], I[nm][d].rearrange("g p -> (g p)").rearrange("(s q) -> s q", q=128), writes=[bprm])
    fw.tr(P.psum[4][:, 0:128], rows, ident, B1 + [P.bconst], [P.bps[4]])
    fw.copy(V_, prm, P.psum[4][:, 0:128], [P.bps[4]], B1)
    dt2 = A.f32(64).rearrange("p (d s) -> p d s", d=2)
    ldv = ldtb.rearrange("p (d s two) -> p d s two", d=2, two=2)
    for gl in range(2):
        fw.act(dt2[gl * 64:(gl + 1) * 64], ldv[gl * 64:(gl + 1) * 64, :, :, gl], AF.Exp, B1, B1)
    fw.tt(V_, r8, prm4[:, 0], dt2, ALU.mult, B1, B1)
    fw.act(r8, r8, AF.Exp, B1, B1, scale=8.0)
    fw.tt(V_, thp8, prm4[:, 1], dt2, ALU.mult, B1, B1)
    fw.ts(V_, thp8, thp8, 8.0 / TWO_PI, None, ALU.mult, None, B1, B1)
    dnat = A.f32(16)
    dT = A.f32(64)
    fw.dma(dnat[0:64, :], I["ssm_d"].rearrange("(g q) -> g q", q=16), writes=[bprm])
    fw.tr(P.psum[5][0:16, 0:64], dnat[0:64, :], ident[0:64, 0:64], B1 + [P.bconst], [P.bps[5]])
    fw.copy(V_, dT[0:16, :], P.psum[5][0:16, 0:64], [P.bps[5]], B1)
    for t_ in range(8):
        fw.dma(dvec[16 * t_:16 * t_ + 16, :], dT[0:16, :], reads=B1, writes=[bprm])
    for t_ in range(8):
        fw.memset(V_, Wsel[t_], 0.0, B1)
        fw.copy(V_, Wsel[t_][:, 112:128], identb[:, 16 * t_:16 * t_ + 16], B1 + [P.bconst], B1)
    rbi = A.i32(1); cbi = A.i32(128); rbf = A.f32(1); cbf = A.f32(128)
    fw.op("gpsimd", lambda e: e.iota(rbi, [[0, 1]], base=0, channel_multiplier=1), writes=B1)
    fw.op("gpsimd", lambda e: e.iota(cbi, [[1, 128]], base=0, channel_multiplier=0), writes=B1)
    fw.ts(V_, rbi, rbi, 4, None, ALU.arith_shift_right, None, B1, B1)
    fw.ts(V_, cbi, cbi, 4, None, ALU.arith_shift_right, None, B1, B1)
    fw.copy(V_, rbf, rbi, B1, B1)
    fw.copy(V_, cbf, cbi, B1, B1)
    fw.ts(V_, mask[0], cbf, rbf[:, 0:1], None, ALU.is_ge, None, B1, B1)
    fw.ts(V_, mask[1], cbf, rbf[:, 0:1], None, ALU.is_le, None, B1, B1)
    iop = dict(channel_multiplier=0, allow_small_or_imprecise_dtypes=True)
    fw.op("gpsimd", lambda e: e.iota(iotaC[0][:, 0:NL], [[1, NL]], base=32, **iop), writes=B1)
    fw.op("gpsimd", lambda e: e.iota(iotaC[0][:, NL:NCH], [[1, 32]], base=0, **iop), writes=B1)
    fw.op("gpsimd", lambda e: e.iota(iotaC[1][:, 0:NL], [[-1, NL]], base=NCH - 1, **iop), writes=B1)
    fw.op("gpsimd", lambda e: e.iota(iotaC[1][:, NL:NCH], [[-1, 32]], base=31, **iop), writes=B1)
    for bufl in (XLre, XLim, DLre, DLim):
        for s_ in range(2):
            for gl in range(2):
                fw.memset(V_, bufl[s_][gl], 0.0, [bMat[s_][gl]])
    fw.barrier()
    A.release(mW)
    LRP = [(A.f32(128), A.f32(128), A.f32(128)) for _ in range(2)]
    bLRP = [(Buf("Lm0"), Buf("Rm0"), Buf("Pm0")), (Buf("Lm1"), Buf("Rm1"), Buf("Pm1"))]
    c1 = A.f32(128); c2 = A.f32(128)
    bc12 = Buf("c12")
    TAU = A.f32(NCH); KI = A.i32(NCH); KF = A.f32(NCH); TA = A.f32(NCH); TB_ = A.f32(NCH)
    Vr = A.f32(NCH); Vi = A.f32(NCH); Gr = A.f32(NCH); Gi = A.f32(NCH)
    Sp = [A.bf16(NL), A.bf16(NL)]
    gtmp = A.f32(NL); gt2 = A.f32(NL)
    bTAB, bKI, bSIN, bTA, bTBb, bVr, bVi, bGr, bGi = (Buf(n) for n in ("tab", "ki", "sin", "ta", "tb", "vr", "vi", "gr", "gi"))
    bSp = [Buf("spre"), Buf("spim")]
    bgt = Buf("gtmp")
    COS, SIN = TAU, KF
    gen_i = [0]
    out_i = [0]

    c3 = A.f32(128); c4 = A.f32(128)
    bc34 = Buf("c34")
    TOEP = globals().get("S5_TOEP", True)
    Esh = c3.tensor[:, 0:1]
    if TOEP:
        cbase = A.last_off - 128
        Esh = A.t16[:, 2 * cbase:2 * cbase + 352]
        cst32 = A.t32[:, cbase + 176:cbase + 192]
        kst = [A.t16[:, 2 * (cbase + 192):2 * (cbase + 192) + 16], A.t16[:, 2 * (cbase + 200):2 * (cbase + 200) + 16]]
        bE = Buf("Esh"); bcst = Buf("cst32"); bkst = [Buf("kst0"), Buf("kst1")]
        fw.memset(V_, Esh, 0.0, [bE])
        fw.copy(V_, Esh[:, 112:240], identb, [P.bconst], [bE])

    def cmul(eng, out, rows, mg, tab, e0, es, VA, VB):
        r0, r1 = rows
        pr_ = pw[0][r0:r1, mg, :]
        pi_ = pw[1][r0:r1, mg, :]
        va_ = VA[r0:r1, mg, :]
        vb_ = VB[r0:r1, mg, :]
        pe = lambda v: AP(v, [[es, 8], [0, 16]], off=e0)
        vb = lambda v: AP(v, [[0, 8], [1, 16]])
        o3 = out[r0:r1, :].rearrange("p (i x) -> p i x", x=16)
        s1, s2, bs_ = (c1, c2, bc12) if eng == "vector" else (c3, c4, bc34)
        a1 = s1[r0:r1, :].rearrange("p (i x) -> p i x", x=16)
        a2 = s2[r0:r1, :].rearrange("p (i x) -> p i x", x=16)
        fw.tt(eng, a1, pe(pr_), vb(va_), ALU.mult, B1, [bs_])
        fw.tt(eng, a2, pe(pi_), vb(vb_), ALU.mult, B1, [bs_])
        fw.tt(eng, o3, a1, a2, ALU.add, [bs_], tab)

    def gen(it):
        gp, d = it // 2, it % 2
        st_ = it % 2
        g0 = 2 * gp
        eL = (7, -1) if d == 0 else (0, 1)
        eR = (7, 1) if d == 0 else (14, -1)
        eD = (8, 1) if d == 0 else (15, -1)
        eP = (14, -1) if d == 0 else (7, 1)
        for gl in range(2):
            g = g0 + gl
            mg = d * 64 + g
            q_ = gen_i[0] % 2
            Lm_, Rm_, Pm_ = LRP[q_]
            bL_, bR_, bP_ = bLRP[q_]
            ALLR = (0, 128)
            if not TOEP:
                cmul(V_, Lm_, ALLR, mg, [bL_], eL[0], eL[1], bb[0], bb[1])
                cmul(V_, Rm_, ALLR, mg, [bR_], eR[0], eR[1], ctm[0], ctm[1])
            cmul(V_, Pm_, ALLR, mg, [bP_], eP[0], eP[1], bb[0], bb[1])
            rws = (gl * 64, gl * 64 + 64)
            bm = bMat[st_][gl]
            DE = "gpsimd" if globals().get("S5_POOL_D", False) else V_
            if gl == 0:
                cmul(DE, DLre[st_][gl], rws, mg, [bm], eD[0], eD[1], ctm[0], ctm[1])
                cmul(DE, DLim[st_][gl], rws, mg, [bm], eD[0], eD[1], ctm[1], ctm[2])
            else:
                cmul(DE, DLre[st_][gl], rws, mg, [bm], eD[0], eD[1], ctm[2], ctm[0])
                cmul(DE, DLim[st_][gl], rws, mg, [bm], eD[0], eD[1], ctm[0], ctm[1])
            pbm = 4 + gen_i[0] % 2
            gen_i[0] += 1
            if not TOEP:
                fw.mm(P.psum[pbm][:, 0:128], Lm_, Rm_, True, True, [bL_, bR_], [P.bps[pbm]])
                fw.tr(P.psum[pbm][:, 128:256], Pm_, ident, [bP_, P.bconst], [P.bps[pbm]])
                fw.tt(V_, Mg[st_][gl], P.psum[pbm][:, 0:128], mask[d], ALU.mult, [P.bps[pbm]] + B1, [bm])
            else:
                kq = gen_i[0] % 2
                fw.act(cst32, ctm[0][:, mg, :], AF.Identity, B1, [bcst])
                fw.mm(P.psum[pbm][:, 256:272], Pm_, cst32, True, True, [bP_, bcst], [P.bps[pbm]])
                fw.tr(P.psum[pbm][:, 128:256], Pm_, ident, [bP_, P.bconst], [P.bps[pbm]])
                fw.act(kst[kq], P.psum[pbm][:, 256:272], AF.Identity, [P.bps[pbm]], [bkst[kq]])
                for t_ in range(8):
                    off = (112 + 16 * (7 - t_)) if d == 0 else (112 - 16 * t_)
                    fw.mm(P.psum[pbm][:, t_ * 16:(t_ + 1) * 16], Esh[:, off:off + 128], kst[kq], True, True,
                          [bE, bkst[kq]], [P.bps[pbm]])
                fw.act(Mg[st_][gl], P.psum[pbm][:, 0:128], AF.Identity, [P.bps[pbm]], [bm])
            fw.copy("scalar", XLre[st_][gl][:, gl * 64:gl * 64 + 64], P.psum[pbm][:, 128:192], [P.bps[pbm]], [bm])
            fw.copy("scalar", XLim[st_][gl][:, gl * 64:gl * 64 + 64], P.psum[pbm][:, 192:256], [P.bps[pbm]], [bm])

    def xmm(it):
        gp, d = it // 2, it % 2
        st_ = it % 2
        g0 = 2 * gp
        for gl in range(2):
            fw.mm(P.psum[2][:, 0:NCH], XLre[st_][gl], U8[:, g0 + gl, :], gl == 0, gl == 1,
                  [bMat[st_][gl], bU8[g0 + gl]], [P.bps[2]])
        for gl in range(2):
            fw.mm(P.psum[3][:, 0:NCH], XLim[st_][gl], U8[:, g0 + gl, :], gl == 0, gl == 1,
                  [bMat[st_][gl], bU8[g0 + gl]], [P.bps[3]])

    def scan(it):
        gp, d = it // 2, it % 2
        thc = thp8[:, d, gp:gp + 1]
        rcol = r8[:, d, gp:gp + 1]
        fw.ts(V_, TAU, iotaC[d], thc, None, ALU.mult, None, B1, [bTAB])
        fw.copy(V_, KI, TAU, [bTAB], [bKI])
        fw.copy(V_, KF, KI, [bKI], [bSIN])
        fw.tt(V_, TAU, TAU, KF, ALU.subtract, [bSIN], [bTAB])
        fw.act(SIN, TAU, AF.Sin, [bTAB], [bSIN], scale=TWO_PI_S)
        fw.act(TAU, TAU, AF.Abs, [], [bTAB])
        fw.act(COS, TAU, AF.Sin, [], [bTAB], scale=-TWO_PI, bias=P.halfpi[:, 0:1])
        xr, xi = P.psum[2][:, 0:NCH], P.psum[3][:, 0:NCH]
        fw.tt(V_, TA, xr, COS, ALU.mult, [P.bps[2], bTAB], [bTA])
        fw.tt(V_, TB_, xi, SIN, ALU.mult, [P.bps[3], bSIN], [bTBb])
        fw.tt(V_, Vr, TA, TB_, ALU.add, [bTA, bTBb], [bVr])
        fw.tt(V_, TA, xi, COS, ALU.mult, [P.bps[3], bTAB], [bTA])
        fw.tt(V_, TB_, xr, SIN, ALU.mult, [P.bps[2], bSIN], [bTBb])
        fw.tt(V_, Vi, TA, TB_, ALU.subtract, [bTA, bTBb], [bVi])
        for (Vx, bVx, Gx, bGx) in ((Vr, bVr, Gr, bGr), (Vi, bVi, Gi, bGi)):
            segA = (lambda v: v[:, NL:NCH]) if d == 0 else (lambda v: rev(v[:, NL:NCH]))
            segB = (lambda v: v[:, 0:NL]) if d == 0 else (lambda v: rev(v[:, 0:NL]))
            lastA = NCH - 1 if d == 0 else NL
            rbA = AP(rcol, [[0, 32]])
            rbB = AP(rcol, [[0, NL]])
            fw.op(V_, lambda e, o=segA(Gx), r_=rbA, v=segA(Vx): e.tensor_tensor_scan(o, r_, v, 0.0, ALU.mult, ALU.add),
                  reads=[bVx] + B1, writes=[bGx])
            fw.op(V_, lambda e, o=segB(Gx), r_=rbB, v=segB(Vx), i_=Gx[:, lastA:lastA + 1]:
                  e.tensor_tensor_scan(o, r_, v, i_, ALU.mult, ALU.add), reads=[bVx] + B1, writes=[bGx])
        if d == 0:
            pieces = ((slice(1, NL), slice(0, NL - 1)), (slice(0, 1), slice(NCH - 1, NCH)))
        else:
            pieces = ((slice(0, NL - 1), slice(1, NL)), (slice(NL - 1, NL), slice(NL, NL + 1)))
        fw.tt(V_, TA, Gr, COS, ALU.mult, [bGr, bTAB], [bTA])
        fw.tt(V_, TB_, Gi, SIN, ALU.mult, [bGi, bSIN], [bTBb])
        for (do, so) in pieces:
            fw.tt(V_, Sp[0][:, do], TA[:, so], TB_[:, so], ALU.subtract, [bTA, bTBb], [bSp[0]])
        fw.tt(V_, TA, Gr, SIN, ALU.mult, [bGr, bSIN], [bTA])
        fw.tt(V_, TB_, Gi, COS, ALU.mult, [bGi, bTAB], [bTBb])
        for (do, so) in pieces:
            fw.tt(V_, Sp[1][:, do], TA[:, so], TB_[:, so], ALU.add, [bTA, bTBb], [bSp[1]])

    def ymm(it):
        gp, d = it // 2, it % 2
        st_ = it % 2
        g0 = 2 * gp
        for gl in range(2):
            g = g0 + gl
            bm = bMat[st_][gl]
            fw.mm(P.psum[gl][:, 0:NL], Mg[st_][gl], U8[:, g, 0:NL], d == 0, False, [bm, bU8[g]], [P.bps[gl]])
            fw.mm(P.psum[gl][:, 0:NL], DLre[st_][gl], Sp[0], False, False, [bm, bSp[0]], [P.bps[gl]])
            fw.mm(P.psum[gl][:, 0:NL], DLim[st_][gl], Sp[1], False, d == 1, [bm, bSp[1]], [P.bps[gl]])

    def epi(gp):
        g0 = 2 * gp
        oc = gp // 4
        for gl in range(2):
            g = g0 + gl
            fw.stt(gtmp, U8[:, g, 0:NL], dvec[:, g:g + 1], P.psum[gl][:, 0:NL], ALU.mult, ALU.add,
                   [P.bps[gl], bU8[g]] + B1, [bgt])
            fw.act(gt2, gtmp, AF.Square, [bgt], [bgt])
            fw.ts(V_, gt2, gt2, 0.044715, 1.0, ALU.mult, ALU.add, [bgt], [bgt])
            fw.tt(V_, gt2, gt2, gtmp, ALU.mult, [bgt], [bgt])
            fw.act(gt2, gt2, AF.Sigmoid, [bgt], [bgt], scale=2.0 * math.sqrt(2.0 / math.pi))
            fw.tt(V_, ybf[:, g % 8, :], gt2, gtmp, ALU.mult, [bgt], [bybf[g % 8]])
        if gp % 4 == 3:
            for t_ in range(8):
                pbo = 6 + out_i[0] % 2
                out_i[0] += 1
                for gg in range(8):
                    fw.mm(P.psum[pbo][:, 0:NL], Wsel[t_][:, 112 - 16 * gg:240 - 16 * gg], ybf[:, gg, :], gg == 0, gg == 7,
                          [bybf[gg]] + B1, [P.bps[pbo]])
                fw.copy("scalar", hT[:, oc, t_:L - 7 + t_:8], P.psum[pbo][:, 0:NL], [P.bps[pbo]],
                        [P.bh[0], P.bh[1], P.bh[2], P.bh[3]])

    def record(fn, *a):
        calls = []
        real = fw.op
        fw.op = lambda *aa, **kk: calls.append((aa, kk))
        try:
            fn(*a)
        finally:
            fw.op = real
        return calls

    def replay_interleaved(ca, cb):
        na, nb = len(ca), len(cb)
        ia = ib = 0
        while ia < na or ib < nb:
            if ib >= nb or (ia < na and ia * max(nb, 1) <= ib * max(na, 1)):
                aa, kk = ca[ia]; ia += 1
            else:
                aa, kk = cb[ib]; ib += 1
            fw.op(*aa, **kk)

    def merge(ca, cb):
        out = []
        na, nb = len(ca), len(cb)
        ia = ib = 0
        while ia < na or ib < nb:
            if ib >= nb or (ia < na and ia * max(nb, 1) <= ib * max(na, 1)):
                out.append(ca[ia]); ia += 1
            else:
                out.append(cb[ib]); ib += 1
        return out

    NIT = 64
    gen(0)
    pend_epi = []
    for it in range(NIT):
        xmm(it)
        cg = record(gen, it + 1) if it + 1 < NIT else []
        cs = record(scan, it)
        if globals().get("S5_INTERLEAVE", True):
            for aa, kk in merge(merge(cs, cg), pend_epi):
                fw.op(*aa, **kk)
        else:
            for aa, kk in pend_epi + cg + cs:
                fw.op(*aa, **kk)
        pend_epi = []
        ymm(it)
        if it % 2 == 1:
            if it + 1 < NIT and globals().get("S5_EPI_DEFER", True):
                pend_epi = record(epi, it // 2)
            else:
                epi(it // 2)
    fw.barrier()


def layer1(P):
    fw, A, I = P.fw, P.A, P.I
    l = 1
    PCE = "gpsimd" if globals().get("USE_POOL", False) else "vector"
    if not hasattr(P, "epsc"):
        P.epsc = A.f32(1)
        fw.memset("vector", P.epsc, EPS, [P.bconst])
    if P.xT is None:
        P.xT = A.f32(KC * T).rearrange("p (c t) -> p c t", c=KC)
        P.bx = [Buf(f"xT{i}") for i in range(5)]
        m_ = A.mark()
        P.xin = [A.f32(D), A.f32(D)]
        P.bxin = [Buf("xin0"), Buf("xin1")]
        P.xin_i = 0
        P.tps = [4, 5]
        P.tps_i = 0
        for tb in range(5):
            t0, tl = TBS[tb]
            load_xT_block(P, tb, P.xT[:, :, t0:t0 + tl], P.bx[tb])
        fw.barrier()
        A.release(m_)
    xT = P.xT
    mX = A.mark()
    hT = P.hT
    scr = norm_scratch(P)
    for tb in range(5):
        t0, tl = TBS[tb]
        norm_mod_block(P, l, 0, tb, xT[:, :, t0:t0 + tl], P.bx[tb], scr)
    fw.barrier()
    A.release(mX)
    if globals().get("S5_MODE", "chunk") == "chunk":
        s5_chunked(P)
        A.release(mX)
        if P.tap == "y1":
            tap_bf16(P, hT.rearrange("p c t -> p (c t)"), KC * T, P.bh)
            return True
        return layer1_tail(P, mX)
    wi = [A.bf16(8 * 512).rearrange("p (k n) -> p k n", k=8) for _ in range(2)]
    bwi = [Buf("wi0"), Buf("wi1")]
    wiv = I["ssm_w_in"].rearrange("(k p) n -> p k n", p=128)
    for i in range(2):
        fw.dma(wi[i], wiv[:, :, i * 512:(i + 1) * 512], writes=[bwi[i]], q="gpsimd")
    for tb in range(5):
        t0, tl = TBS[tb]
        for m in range(8):
            for k in range(KC):
                fw.mm(P.psum[m][:, 0:tl], wi[m // 4][:, k, (m % 4) * 128:(m % 4 + 1) * 128], hT[:, k, t0:t0 + tl],
                      k == 0, k == KC - 1, [bwi[m // 4], P.bh[tb]], [P.bps[m]])
        for m in range(8):
            fw.copy("scalar" if m % 2 else "vector", hT[:, m, t0:t0 + tl], P.psum[m][:, 0:tl], [P.bps[m]], [P.bh[tb]])
    fw.barrier()
    A.release(mX)
    uT = hT
    TWO_PI = 2.0 * math.pi
    TWO_PI_S = 6.2831845
    bprm = Buf("s5prm")
    prm = A.f32(128)
    prm4 = prm.rearrange("p (a d s) -> p a d s", a=2, d=2)
    ldt = A.f32(64).rearrange("p (d s) -> p d s", d=2)
    thp = A.f32(64).rearrange("p (d s) -> p d s", d=2)
    th2 = A.f32(64).rearrange("p (d s) -> p d s", d=2)
    rr_ = A.f32(64).rearrange("p (d s) -> p d s", d=2)
    kap = A.f32(128).rearrange("p (a d s) -> p a d s", a=2, d=2)
    dcol = A.f32(8)
    Bl = [A.bf16(16 * 128).rearrange("p (m c) -> p m c", m=16) for _ in range(2)]
    LT = [A.bf16(16 * 128).rearrange("p (m c) -> p m c", m=16) for _ in range(2)]
    LTm = [[A.bf16(128) for _ in range(4)] for _ in range(2)]
    bLTm = [[Buf(f"LTm{ri}{pr}") for pr in range(4)] for ri in range(2)]
    BlZ = [[A.bf16(128) for _ in range(4)] for _ in range(2)]
    bBlZ = [[Buf(f"BlZ{ri}{pr}") for pr in range(4)] for ri in range(2)]
    JH = 1152
    iotaJ = A.f32(JH)
    st2 = A.f32(2)
    mS = A.mark()
    rows = A.f32(128)
    for a_i, nm in enumerate(("ssm_a_re", "ssm_a_im")):
        for d in range(2):
            r0 = (a_i * 2 + d) * 32
            fw.dma(rows[r0:r0 + 32, :], I[nm][d].rearrange("g p -> (g p)").rearrange("(s q) -> s q", q=128), writes=[bprm])
    fw.tr(P.psum[0][:, 0:128], rows, P.ident, [bprm, P.bconst], [P.bps[0]])
    fw.copy("vector", prm, P.psum[0][:, 0:128], [P.bps[0]], [bprm])
    ld_t = I["ssm_log_dt"].tensor
    for gl in range(2):
        fw.dma(ldt[gl * 64:(gl + 1) * 64, :, :], bass.AP(ld_t, gl, [[0, 64], [64, 2], [2, 32]]), writes=[bprm], **SLOW)
    fw.dma(dcol, I["ssm_d"].rearrange("(c p) -> p c", p=128), writes=[bprm], **SLOW)
    fw.op("gpsimd", lambda e: e.iota(iotaJ, [[1, JH]], base=0, channel_multiplier=0,
                                     allow_small_or_imprecise_dtypes=True), writes=[bprm])
    dt_ = A.f32(64).rearrange("p (d s) -> p d s", d=2)
    xre = A.f32(64).rearrange("p (d s) -> p d s", d=2)
    th = A.f32(64).rearrange("p (d s) -> p d s", d=2)
    ki = A.i32(64).rearrange("p (d s) -> p d s", d=2)
    kf = A.f32(64).rearrange("p (d s) -> p d s", d=2)
    sn = A.f32(64).rearrange("p (d s) -> p d s", d=2)
    cs_ = A.f32(64).rearrange("p (d s) -> p d s", d=2)
    lr = A.f32(64).rearrange("p (d s) -> p d s", d=2)
    li = A.f32(64).rearrange("p (d s) -> p d s", d=2)
    t_a = A.f32(64).rearrange("p (d s) -> p d s", d=2)
    t_b = A.f32(64).rearrange("p (d s) -> p d s", d=2)
    nrm = A.f32(64).rearrange("p (d s) -> p d s", d=2)
    V_ = "vector"
    B1 = [bprm]
    fw.act(dt_, ldt, AF.Exp, B1, B1)
    fw.tt(V_, xre, prm4[:, 0], dt_, ALU.mult, B1, B1)
    fw.tt(V_, th, prm4[:, 1], dt_, ALU.mult, B1, B1)
    fw.act(rr_, xre, AF.Exp, B1, B1)
    fw.ts(V_, thp, th, 1.0 / TWO_PI, None, ALU.mult, None, B1, B1)
    fw.ts(V_, th2, thp, float(JH), None, ALU.mult, None, B1, B1)
    fw.copy(V_, ki, thp, B1, B1)
    fw.copy(V_, kf, ki, B1, B1)
    fw.tt(V_, kf, thp, kf, ALU.subtract, B1, B1)
    fw.act(sn, kf, AF.Sin, B1, B1, scale=TWO_PI_S)
    fw.act(kf, kf, AF.Abs, B1, B1)
    fw.act(cs_, kf, AF.Sin, B1, B1, scale=-TWO_PI, bias=P.halfpi[:, 0:1])
    fw.tt(V_, lr, rr_, cs_, ALU.mult, B1, B1)
    fw.tt(V_, li, rr_, sn, ALU.mult, B1, B1)
    fw.ts(V_, lr, lr, -1.0, None, ALU.add, None, B1, B1)
    fw.tt(V_, nrm, prm4[:, 0], prm4[:, 0], ALU.mult, B1, B1)
    fw.tt(V_, t_a, prm4[:, 1], prm4[:, 1], ALU.mult, B1, B1)
    fw.tt(V_, nrm, nrm, t_a, ALU.add, B1, B1)
    fw.op(V_, lambda e: e.reciprocal(nrm, nrm), reads=B1, writes=B1)
    fw.tt(V_, t_a, lr, prm4[:, 0], ALU.mult, B1, B1)
    fw.tt(V_, t_b, li, prm4[:, 1], ALU.mult, B1, B1)
    fw.tt(V_, t_a, t_a, t_b, ALU.add, B1, B1)
    fw.tt(V_, kap[:, 0], t_a, nrm, ALU.mult, B1, B1)
    fw.tt(V_, t_a, li, prm4[:, 0], ALU.mult, B1, B1)
    fw.tt(V_, t_b, lr, prm4[:, 1], ALU.mult, B1, B1)
    fw.tt(V_, t_a, t_a, t_b, ALU.subtract, B1, B1)
    fw.tt(V_, kap[:, 1], t_a, nrm, ALU.mult, B1, B1)
    bn = [A.f32(64 * 16).rearrange("p (m x) -> p m x", m=64) for _ in range(2)]
    for ri, nm in enumerate(("ssm_b_re", "ssm_b_im")):
        fw.dma(bn[ri], I[nm].rearrange("d g p x -> (d g p) x").rearrange("(m q) x -> q m x", q=128), writes=[bprm])
    kap_off = A.last_off
    Nn = [A.f32(64 * 16).rearrange("p (m x) -> p m x", m=64) for _ in range(2)]
    tN = A.f32(64 * 16).rearrange("p (m x) -> p m x", m=64)

    def kb(a):
        v = kap[:, a].rearrange("p d s -> p (d s)")
        dims = [list(d_) for d_ in v.ap]
        return bass.AP(v.tensor, v.offset, dims + [[0, 16]])
    fw.tt(V_, Nn[0], bn[0], kb(0), ALU.mult, B1, B1)
    fw.tt(V_, tN, bn[1], kb(1), ALU.mult, B1, B1)
    fw.tt(V_, Nn[0], Nn[0], tN, ALU.subtract, B1, B1)
    fw.tt(V_, Nn[1], bn[1], kb(0), ALU.mult, B1, B1)
    fw.tt(V_, tN, bn[0], kb(1), ALU.mult, B1, B1)
    fw.tt(V_, Nn[1], Nn[1], tN, ALU.add, B1, B1)
    Zb = A.f32(64 * 32).rearrange("p (m x) -> p m x", m=64)
    for ri in range(2):
        fw.memset(V_, Zb, 0.0, B1)
        fw.copy(V_, Zb[0:64, :, 0:16], Nn[ri][0:64, :, :], B1, B1)
        fw.copy(V_, Zb[64:128, :, 16:32], Nn[ri][64:128, :, :], B1, B1)
        Zf = Zb.rearrange("p m x -> p (m x)")
        for mm_ in range(16):
            pb = mm_ % 4
            fw.tr(P.psum[pb][:, 0:128], Zf[:, mm_ * 128:(mm_ + 1) * 128], P.ident, B1 + [P.bconst], [P.bps[pb]])
            fw.copy("scalar", Bl[ri][:, mm_, :], P.psum[pb][:, 0:128], [P.bps[pb]], B1)
    msk = A.f32(128)
    fw.memset(V_, msk, 0.0, B1)
    for gg in range(8):
        h_ = gg % 2
        fw.memset(V_, msk[h_ * 64:(h_ + 1) * 64, gg * 16:(gg + 1) * 16], 1.0, B1)
    Cn = A.f32(16 * 128).rearrange("p (m c) -> p m c", m=16)
    for ri, nm in enumerate(("ssm_c_re", "ssm_c_im")):
        src = I[nm].rearrange("d g q p -> (d g q) p").rearrange("(m r) p -> r m p", r=128)
        for dup in range(2):
            fw.dma(Cn[:, :, dup * 64:(dup + 1) * 64], src, writes=[bprm])
        for mm_ in range(16):
            pb = mm_ % 4
            fw.tr(P.psum[pb][:, 0:128], Cn[:, mm_, :], P.ident, B1 + [P.bconst], [P.bps[pb]])
            if ri == 0:
                fw.tt(V_, LT[ri][:, mm_, :], P.psum[pb][:, 0:128], msk, ALU.mult, [P.bps[pb]] + B1, B1)
            else:
                fw.stt(LT[ri][:, mm_, :], P.psum[pb][:, 0:128], -1.0, msk, ALU.mult, ALU.mult, [P.bps[pb]] + B1, B1)
    for ri in range(2):
        for pr in range(4):
            fw.memset(V_, LTm[ri][pr], 0.0, [bLTm[ri][pr]])
            fw.memset(V_, BlZ[ri][pr], 0.0, [bBlZ[ri][pr]])
    if P.tap == "s5lt":
        for ri in range(2):
            fw.copy(V_, Zb.rearrange("p m x -> p (m x)"), LT[ri].rearrange("p m c -> p (m c)"), B1, B1)
            fw.dma(P.dbg[:, ri * 2048:(ri + 1) * 2048], Zb.rearrange("p m x -> p (m x)"), reads=B1, writes=[P.bdbg])
            fw.copy(V_, Zb.rearrange("p m x -> p (m x)"), Bl[ri].rearrange("p m c -> p (m c)"), B1, B1)
            fw.dma(P.dbg[:, 4096 + ri * 2048:4096 + (ri + 1) * 2048], Zb.rearrange("p m x -> p (m x)"), reads=B1, writes=[P.bdbg])
        return True
    fw.barrier()
    A.release(mS)
    NB = JH
    TAU = A.f32(NB); KI = A.i32(NB); KF = A.f32(NB)
    TA = A.f32(NB); TB_ = A.f32(NB)
    Vr = A.f32(NB); Vi = A.f32(NB); Gr = A.f32(NB); Gi = A.f32(NB)
    Hh = [A.bf16(L), A.bf16(L)]
    gtmp = A.f32(512); gt2 = A.f32(512)
    yacc = A.f32(L)
    byacc = [Buf(f"yacc{i}") for i in range(4)]
    bTAB, bKI, bSIN, bTA, bTBb, bVr, bVi, bGr, bGi = (Buf(n) for n in ("tab", "ki", "sin", "ta", "tb", "vr", "vi", "gr", "gi"))
    bH = [Buf("Hre"), Buf("Him")]
    bst2 = Buf("st2")
    bgt = Buf("gtmp")
    COS, SIN = TAU, KF

    def segs(d, hf):
        out = []
        j0 = hf * JH
        if d == 0:
            whole = [(L, NCTX, False, False)] + [(0, L, False, True)]
        else:
            whole = [(L, NCTX, True, False)] + [(0, L, True, True)]
        pos = 0
        for (a, n, rv, lat) in whole:
            lo, hi = max(pos, j0), min(pos + n, j0 + JH)
            if lo < hi:
                cnt = hi - lo
                off = lo - pos
                while cnt > 0:
                    c_ = min(512, cnt)
                    if not rv:
                        out.append((a + off, c_, lo - j0, False, lat))
                    else:
                        out.append((a + n - off - c_, c_, lo - j0, True, lat))
                    off += c_
                    lo += c_
                    cnt -= c_
            pos += n
        return out

    bui = 0
    for oc in range(8):
        for d in range(2):
            for pr in range(4):
                st = oc * 4 + pr
                m16 = d * 8 + oc
                for ri in range(2):
                    if globals().get("SKIPCP", False) and (oc, d, pr) != (0, 0, 0):
                        continue
                    fw.copy(PCE, LTm[ri][pr][:, pr * 32:(pr + 1) * 32], LT[ri][:, m16, pr * 32:(pr + 1) * 32],
                            [bprm], [bLTm[ri][pr]])
                    fw.copy(PCE, BlZ[ri][pr][pr * 32:(pr + 1) * 32, :], Bl[ri][pr * 32:(pr + 1) * 32, m16, :],
                            [bprm], [bBlZ[ri][pr]])
                if P.tap == "s5dbg5" and (oc, d, pr) == P.dbgsel[:3]:
                    for ri in range(2):
                        for p4 in range(4):
                            fw.copy(V_, gtmp[:, 0:128], LTm[ri][p4], [bLTm[ri][p4]], [bgt])
                            fw.dma(P.dbg[:, (ri * 4 + p4) * 128:(ri * 4 + p4 + 1) * 128], gtmp[:, 0:128], reads=[bgt], writes=[P.bdbg])
                    return True
                thc = thp[:, d, st:st + 1]
                rcol = rr_[:, d, st:st + 1]
                rb = bass.AP(rcol.tensor, rcol.offset, [list(rcol.ap[0]), [0, NB]])
                for hf in range(2):
                    if globals().get("S5BAR2", False):
                        fw.barrier()
                    if hf == 0:
                        fw.ts(V_, TAU, iotaJ, thc, None, ALU.mult, None, [bprm], [bTAB])
                    else:
                        fw.ts(V_, TAU, iotaJ, thc, th2[:, d, st:st + 1], ALU.mult, ALU.add, [bprm], [bTAB])
                    fw.copy(V_, KI, TAU, [bTAB], [bKI])
                    fw.copy(V_, KF, KI, [bKI], [bSIN])
                    fw.tt(V_, TAU, TAU, KF, ALU.subtract, [bSIN], [bTAB])
                    fw.act(SIN, TAU, AF.Sin, [bTAB], [bSIN], scale=TWO_PI_S)
                    fw.act(TAU, TAU, AF.Abs, [], [bTAB])
                    fw.act(COS, TAU, AF.Sin, [], [bTAB], scale=-TWO_PI, bias=P.halfpi[:, 0:1])
                    sg_ = segs(d, hf)
                    for (a, n, jl, rv, lat) in sg_:
                        pre, pim = 4 + (bui % 2) * 2, 5 + (bui % 2) * 2
                        bui += 1
                        tb = min(a // 512, 4)
                        tb2 = min((a + n - 1) // 512, 4)
                        rd = [P.bh[tb]] + ([P.bh[tb2]] if tb2 != tb else [])
                        for ri, pb in ((0, pre), (1, pim)):
                            fw.mm(P.psum[pb][:, 0:n], BlZ[ri][pr], uT[:, oc, a:a + n], True, True,
                                  rd + [bBlZ[ri][pr]], [P.bps[pb]])
                        bre = P.psum[pre][:, 0:n]
                        bim = P.psum[pim][:, 0:n]
                        if rv:
                            bre, bim = rev(bre), rev(bim)
                        cs_s, sn_s = COS[:, jl:jl + n], SIN[:, jl:jl + n]
                        fw.tt(V_, TA[:, jl:jl + n], bre, cs_s, ALU.mult, [P.bps[pre], bTAB], [bTA])
                        fw.tt(V_, TB_[:, jl:jl + n], bim, sn_s, ALU.mult, [P.bps[pim], bSIN], [bTBb])
                        fw.tt(PCE, Vr[:, jl:jl + n], TA[:, jl:jl + n], TB_[:, jl:jl + n], ALU.add, [bTA, bTBb], [bVr])
                        fw.tt(V_, TA[:, jl:jl + n], bim, cs_s, ALU.mult, [P.bps[pim], bTAB], [bTA])
                        fw.tt(V_, TB_[:, jl:jl + n], bre, sn_s, ALU.mult, [P.bps[pre], bSIN], [bTBb])
                        fw.tt(PCE, Vi[:, jl:jl + n], TA[:, jl:jl + n], TB_[:, jl:jl + n], ALU.subtract, [bTA, bTBb], [bVi])
                    for (Vx, bVx, Gx, bGx, si) in ((Vr, bVr, Gr, bGr, 0), (Vi, bVi, Gi, bGi, 1)):
                        init = 0.0 if hf == 0 else st2[:, si:si + 1]
                        fw.op(V_, lambda e, Gx=Gx, Vx=Vx, init=init, rb=rb: e.tensor_tensor_scan(Gx, rb, Vx, init, ALU.mult, ALU.add),
                              reads=[bVx, bprm, bst2], writes=[bGx])
                        if hf == 0:
                            fw.copy(V_, st2[:, si:si + 1], Gx[:, NB - 1:NB], [bGx], [bst2])
                    if P.tap == "s5dbg" and (oc, d, pr, hf) == P.dbgsel:
                        for i_, (ap_, b_) in enumerate(((COS, bTAB), (SIN, bSIN), (Vr, bVr), (Vi, bVi), (Gr, bGr), (Gi, bGi))):
                            fw.dma(P.dbg[:, i_ * NB:(i_ + 1) * NB], ap_, reads=[b_], writes=[P.bdbg])
                        return True
                    for (a, n, jl, rv, lat) in sg_:
                        if not lat:
                            continue
                        cs_s, sn_s = COS[:, jl:jl + n], SIN[:, jl:jl + n]
                        hr = Hh[0][:, a:a + n]
                        hi_ = Hh[1][:, a:a + n]
                        if rv:
                            hr, hi_ = rev(hr), rev(hi_)
                        fw.tt(PCE, TA[:, jl:jl + n], Gr[:, jl:jl + n], cs_s, ALU.mult, [bGr, bTAB], [bTA])
                        fw.tt(PCE, TB_[:, jl:jl + n], Gi[:, jl:jl + n], sn_s, ALU.mult, [bGi, bSIN], [bTBb])
                        fw.tt(V_, hr, TA[:, jl:jl + n], TB_[:, jl:jl + n], ALU.subtract, [bTA, bTBb], [bH[0]])
                        fw.tt(PCE, TA[:, jl:jl + n], Gr[:, jl:jl + n], sn_s, ALU.mult, [bGr, bSIN], [bTA])
                        fw.tt(PCE, TB_[:, jl:jl + n], Gi[:, jl:jl + n], cs_s, ALU.mult, [bGi, bTAB], [bTBb])
                        fw.tt(V_, hi_, TA[:, jl:jl + n], TB_[:, jl:jl + n], ALU.add, [bTA, bTBb], [bH[1]])
                if P.tap == "s5dbg2" and (oc, d, pr) == P.dbgsel[:3]:
                    for i_ in range(2):
                        fw.copy(V_, Vr[:, 0:1024], Hh[i_][:, 0:1024], [bH[i_]], [bVr])
                        fw.dma(P.dbg[:, i_ * 2048:i_ * 2048 + 1024], Vr[:, 0:1024], reads=[bVr], writes=[P.bdbg])
                        fw.copy(V_, Vi[:, 0:1024], Hh[i_][:, 1024:2048], [bH[i_]], [bVi])
                        fw.dma(P.dbg[:, i_ * 2048 + 1024:i_ * 2048 + 2048], Vi[:, 0:1024], reads=[bVi], writes=[P.bdbg])
                    return True
                if globals().get("S5BAR", False):
                    fw.barrier()
                for tb in range(4):
                    for ri in range(2):
                        fw.mm(P.psum[tb][:, :], LTm[ri][pr], Hh[ri][:, tb * 512:(tb + 1) * 512], ri == 0, ri == 1,
                              [bLTm[ri][pr], bH[ri]], [P.bps[tb]])
                    ysl = yacc[:, tb * 512:(tb + 1) * 512]
                    if d == 0 and pr == 0:
                        fw.copy("scalar", ysl, P.psum[tb][:, :], [P.bps[tb]], [byacc[tb]])
                    else:
                        fw.tt(V_, ysl, P.psum[tb][:, :], ysl, ALU.add, [P.bps[tb]], [byacc[tb]])
                if P.tap == "s5dbg4" and (oc, d, pr) == (0, 0, 0) and P.dbgsel[:3] != (0, 0, 0):
                    fw.dma(P.dbg[:, 8192:8192 + 2048], yacc, reads=byacc, writes=[P.bdbg])
                    for i_ in range(2):
                        fw.copy(V_, Vr[:, 0:1024], Hh[i_][:, 0:1024], [bH[i_]], [bVr])
                        fw.dma(P.dbg[:, 10240 + i_ * 2048:10240 + i_ * 2048 + 1024], Vr[:, 0:1024], reads=[bVr], writes=[P.bdbg])
                        fw.copy(V_, Vi[:, 0:1024], Hh[i_][:, 1024:2048], [bH[i_]], [bVi])
                        fw.dma(P.dbg[:, 10240 + i_ * 2048 + 1024:10240 + i_ * 2048 + 2048], Vi[:, 0:1024], reads=[bVi], writes=[P.bdbg])
                        fw.copy(V_, gt2[:, 0:128], LTm[i_][0], [bLTm[i_][0]], [bgt])
                        fw.dma(P.dbg[:, 14336 + i_ * 128:14336 + (i_ + 1) * 128], gt2[:, 0:128], reads=[bgt], writes=[P.bdbg])
                if globals().get("S5BAR", False):
                    fw.barrier()
                if P.tap == "s5dbg4" and (oc, d, pr) == P.dbgsel[:3]:
                    for tb in range(4):
                        fw.dma(P.dbg[:, tb * 512:(tb + 1) * 512], yacc[:, tb * 512:(tb + 1) * 512], reads=[byacc[tb]], writes=[P.bdbg])
                    for tb in range(4):
                        fw.copy(V_, gtmp, P.psum[tb][:, :], [P.bps[tb]], [bgt])
                        fw.dma(P.dbg[:, 2048 + tb * 512:2048 + (tb + 1) * 512], gtmp, reads=[bgt], writes=[P.bdbg])
                    for ri in range(2):
                        fw.copy(V_, gtmp[:, 0:128], LTm[ri][pr], [bLTm[ri][pr]], [bgt])
                        fw.dma(P.dbg[:, 4096 + ri * 128:4096 + (ri + 1) * 128], gtmp[:, 0:128], reads=[bgt], writes=[P.bdbg])
                    return True
        if P.tap == "s5dbg3" and oc == P.dbgsel[0]:
            for tb in range(4):
                fw.copy(V_, gtmp, P.psum[tb][:, :], [P.bps[tb]], [bgt])
                fw.dma(P.dbg[:, tb * 512:(tb + 1) * 512], gtmp, reads=[bgt], writes=[P.bdbg])
            return True
        for tb in range(4):
            blk = hT[:, oc, tb * 512:(tb + 1) * 512]
            fw.stt(gtmp, blk, dcol[:, oc:oc + 1], yacc[:, tb * 512:(tb + 1) * 512], ALU.mult, ALU.add,
                   [byacc[tb], P.bh[tb], bprm], [bgt])
            fw.act(gt2, gtmp, AF.Square, [bgt], [bgt])
            fw.ts(V_, gt2, gt2, 0.044715, 1.0, ALU.mult, ALU.add, [bgt], [bgt])
            fw.tt(V_, gt2, gt2, gtmp, ALU.mult, [bgt], [bgt])
            fw.act(gt2, gt2, AF.Sigmoid, [bgt], [bgt], scale=2.0 * math.sqrt(2.0 / math.pi))
            fw.tt(V_, blk, gt2, gtmp, ALU.mult, [bgt], [P.bh[tb]])
    fw.barrier()
    A.release(mX)
    if P.tap == "y1":
        tap_bf16(P, hT.rearrange("p c t -> p (c t)"), KC * T, P.bh)
        return True
    return layer1_tail(P, mX)


def layer1_tail(P, mX):
    fw, A, I = P.fw, P.A, P.I
    l = 1
    xT, hT = P.xT, P.hT
    V_ = "vector"
    wo = [A.bf16(8 * 512).rearrange("p (k n) -> p k n", k=8) for _ in range(4)]
    bwo = [Buf(f"so{i}") for i in range(4)]
    wov = I["ssm_w_out"].rearrange("(k p) n -> p k n", p=128)
    sgm = [A.f32(512), A.f32(512)]
    bsgm = [Buf("sgm0"), Buf("sgm1")]
    for i in range(4):
        fw.dma(wo[i], wov[:, :, i * 512:(i + 1) * 512], writes=[bwo[i]], q="gpsimd")
    it = 0
    for tb in range(4):
        t0, tl = TBS[tb]
        for m in range(8):
            pa, pg = (it % 4) * 2, (it % 4) * 2 + 1
            it += 1
            for (pb, wi_) in ((pa, m // 4), (pg, 2 + m // 4)):
                for k in range(KC):
                    fw.mm(P.psum[pb][:, :], wo[wi_][:, k, (m % 4) * 128:(m % 4 + 1) * 128], hT[:, k, t0:t0 + tl],
                          k == 0, k == KC - 1, [bwo[wi_], P.bh[tb]], [P.bps[pb]])
            j = it % 2
            fw.act(sgm[j], P.psum[pg][:, :], AF.Sigmoid, [P.bps[pg]], [bsgm[j]])
            fw.tt(V_, sgm[j], P.psum[pa][:, :], sgm[j], ALU.mult, [P.bps[pa]], [bsgm[j]])
            fw.stt(xT[:, m, t0:t0 + tl], sgm[j], mod_cols(P, l, 2, m, 0), xT[:, m, t0:t0 + tl], ALU.mult, ALU.add,
                   [bsgm[j], P.bmod], [P.bx[tb]])
    fw.barrier()
    A.release(mX)
    if P.tap == "xmix1":
        tap_out(P, xT.rearrange("p c t -> p (c t)"), KC * T, P.bx)
        return True
    scr = norm_scratch(P)
    for tb in range(4):
        t0, tl = TBS[tb]
        norm_mod_block(P, l, 1, tb, xT[:, :, t0:t0 + tl], P.bx[tb], scr)
    fw.barrier()
    A.release(mX)
    wr = A.bf16(64).rearrange("p (k e) -> p k e", k=8)
    bwr = Buf("wr")
    fw.dma(wr, I["moe_router"].rearrange("(k p) e -> p k e", p=128), writes=[bwr], q="gpsimd", **SLOW)
    lg = A.f32(128).rearrange("p (t e) -> p t e", e=8)
    lg2 = A.f32(128).rearrange("p (t e) -> p t e", e=8)
    eq1 = A.f32(128).rearrange("p (t e) -> p t e", e=8)
    eq2 = A.f32(128).rearrange("p (t e) -> p t e", e=8)
    gate = A.f32(128).rearrange("p (t e) -> p t e", e=8)
    m1 = A.f32(16); m2 = A.f32(16); w1_ = A.f32(16); w2_ = A.f32(16)
    bg_ = Buf("gate")
    for tt in range(16):
        for k in range(KC):
            fw.mm(P.psum[0][:, tt * 8:(tt + 1) * 8], hT[:, k, tt * 128:(tt + 1) * 128], wr[:, k, :], k == 0, k == KC - 1,
                  [P.bh[tt // 4], bwr], [P.bps[0]])
    G1 = [bg_]
    fw.copy(V_, lg, P.psum[0][:, 0:128].rearrange("p (t e) -> p t e", e=8), [P.bps[0]], G1)

    def bc8(v):
        return bass.AP(v.tensor, v.offset, [list(d_) for d_ in v.ap] + [[0, 8]])
    fw.op(V_, lambda e: e.tensor_reduce(m1, lg, AX.X, ALU.max), reads=G1, writes=G1)
    fw.tt(V_, eq1, lg, bc8(m1), ALU.is_equal, G1, G1)
    fw.stt(lg2, eq1, -1.0e30, lg, ALU.mult, ALU.add, G1, G1)
    fw.op(V_, lambda e: e.tensor_reduce(m2, lg2, AX.X, ALU.max), reads=G1, writes=G1)
    fw.tt(V_, eq2, lg2, bc8(m2), ALU.is_equal, G1, G1)
    fw.tt(V_, w2_, m2, m1, ALU.subtract, G1, G1)
    fw.act(w2_, w2_, AF.Exp, G1, G1)
    fw.ts(V_, w1_, w2_, 1.0, None, ALU.add, None, G1, G1)
    fw.op(V_, lambda e: e.reciprocal(w1_, w1_), reads=G1, writes=G1)
    fw.tt(V_, w2_, w2_, w1_, ALU.mult, G1, G1)
    fw.tt(V_, gate, eq1, bc8(w1_), ALU.mult, G1, G1)
    fw.tt(V_, eq2, eq2, bc8(w2_), ALU.mult, G1, G1)
    fw.tt(V_, gate, gate, eq2, ALU.add, G1, G1)
    gbc = [A.bf16(L), A.bf16(L)]
    bgbc = [Buf("gbc0"), Buf("gbc1")]
    fb = ffn_buffers(P, L)
    halves = [(0, 1024), (1024, 1024)]
    for e_ in range(NEXP):
        gj = e_ % 2
        for tb in range(4):
            pb = 4 + (e_ * 4 + tb) % 4
            for t4 in range(4):
                tt = tb * 4 + t4
                gcol = gate[:, tt, e_:e_ + 1]
                gl_ = bass.AP(gcol.tensor, gcol.offset, [list(gcol.ap[0]), [0, 128]])
                fw.mm(P.psum[pb][:, t4 * 128:(t4 + 1) * 128], gl_, P.ident, True, True, G1 + [P.bconst], [P.bps[pb]])
            fw.copy("scalar", gbc[gj][:, tb * 512:(tb + 1) * 512], P.psum[pb][:, :], [P.bps[pb]], [bgbc[gj]])
        nxt = None
        if e_ + 1 < NEXP:
            nxt = (I["moe_w1"][e_ + 1], I["moe_w3"][e_ + 1], I["moe_w2"][e_ + 1])
        ffn(P, l, fb, I["moe_w1"][e_], I["moe_w3"][e_], I["moe_w2"][e_], EXPERT_DIM, halves, 4,
            gate=gbc[gj], bgate=bgbc[gj], first=(e_ == 0), nxt=nxt)
    fw.barrier()
    A.release(mX)
    if P.tap == "xffn1":
        tap_out(P, xT.rearrange("p c t -> p (c t)"), KC * T, P.bx)
        return True
    return False


def write_output(P):
    fw, A = P.fw, P.A
    m0 = A.mark()
    xo = [A.f32(D) for _ in range(2)]
    bxo = [Buf("xo0"), Buf("xo1")]
    ntt = 18 if 1 not in P.layers else 16
    bo = Buf("out_lat")
    boc = Buf("out_ctx")
    pi = 0
    for tt in range(ntt):
        j = tt % 2
        tb = min(tt // 4, 4)
        for half in range(2):
            pb = pi % 4
            pi += 1
            ps, bps = P.psum[pb], P.bps[pb]
            for cc in range(4):
                c = half * 4 + cc
                fw.tr(ps[:, cc * 128:(cc + 1) * 128], P.xT[:, c, tt * 128:(tt + 1) * 128], P.ident,
                      [P.bx[tb], P.bconst], [bps])
            fw.copy("vector" if half else "scalar", xo[j][:, half * 512:(half + 1) * 512], ps[:, :], [bps], [bxo[j]])
        if tt < 16:
            fw.dma(P.out_lat[tt * 128:(tt + 1) * 128, :], xo[j], reads=[bxo[j]], writes=[bo])
        else:
            fw.dma(P.out_ctx[(tt - 16) * 128:(tt - 15) * 128, :], xo[j], reads=[bxo[j]], writes=[boc])
    P.out_bufs.append(bo)
    if ntt == 18:
        P.out_bufs.append(boc)
    A.release(m0)


_CACHE = {}


def _get_prog(key, **kw):
    if key not in _CACHE:
        _CACHE[key] = build_program(**kw)
    return _CACHE[key]


def make_in_map(inputs, b, layers=(0, 1), x_override=None, ctx_override=None):
    f = lambda a: np.ascontiguousarray(np.asarray(a, dtype=np.float32))
    m = {}
    m["x"] = f(inputs["x"][b] if x_override is None else x_override)
    m["ctx"] = f(inputs["ctx"][b] if ctx_override is None else ctx_override)
    m["cc"] = f(np.stack([np.asarray(inputs["c"][b]), np.asarray(inputs["c_ctx"])]))
    for k in ("ada_w", "ada_b", "norm1_g", "norm2_g"):
        m[k] = f(inputs[k])
    if 0 in layers:
        for k in ("mix_w_in", "q_norm_g", "k_norm_g", "na_rpb", "conv_dw_w", "conv_dw_b", "conv_ln_g",
                  "conv_ln_b", "mix_w_out", "ffn_w1", "ffn_w3", "ffn_w2"):
            m[k] = f(inputs[k][0])
    if 1 in layers:
        for k in ("ssm_w_in", "ssm_a_re", "ssm_a_im", "ssm_log_dt", "ssm_b_re", "ssm_b_im", "ssm_c_re",
                  "ssm_c_im", "ssm_d", "ssm_w_out", "moe_router", "moe_w1", "moe_w3", "moe_w2"):
            m[k] = f(inputs[k][0])
    return m


MODE = "fused"


def kernel(**inputs):
    if MODE == "fused":
        nc, P = _get_prog("full", layers=(0, 1))
        in_maps = [make_in_map(inputs, b) for b in range(8)]
        res = run_bass_kernel_spmd(nc, in_maps, core_ids=list(range(8)))
        return np.stack([np.asarray(r["out_lat"], dtype=np.float32) for r in res.results], axis=0)
    ncA, PA = _get_prog("L0", layers=(0,))
    in_maps = [make_in_map(inputs, b, layers=(0,)) for b in range(8)]
    resA = run_bass_kernel_spmd(ncA, in_maps, core_ids=list(range(8)))
    ncB, PB = _get_prog("L1", layers=(1,))
    in_maps = [make_in_map(inputs, b, layers=(1,), x_override=np.asarray(resA.results[b]["out_lat"]),
                           ctx_override=np.asarray(resA.results[b]["out_ctx"])) for b in range(8)]
    resB = run_bass_kernel_spmd(ncB, in_maps, core_ids=list(range(8)))
    return np.stack([np.asarray(r["out_lat"], dtype=np.float32) for r in resB.results], axis=0)
```

```python
import contextlib
import math
import numpy as np
import concourse.bass as bass
import concourse.mybir as mybir
from concourse.bass_utils import run_bass_kernel_spmd

F32 = mybir.dt.float32
BF16 = mybir.dt.bfloat16
I32 = mybir.dt.int32
ALU = mybir.AluOpType
AF = mybir.ActivationFunctionType
AX = mybir.AxisListType

SEM_ROLL = 30000
D = 1024
KC = 8
L = 2048
NCTX = 256
T = L + NCTX
EPS = 1e-6
TBS = [(0, 512), (512, 512), (1024, 512), (1536, 512), (2048, 256)]
FFN_DIM = 2816
NEXP = 8
EXPERT_DIM = 3584
NEG = -30000.0
SLOW = dict(allow_slow_non_contiguous=True)


class Buf:
    __slots__ = ("name", "w", "r", "dsem", "dcnt")

    def __init__(self, name=""):
        self.name = name
        self.w = None
        self.r = []
        self.dsem = None
        self.dcnt = 0


class FW:
    ENGS = ("sync", "tensor", "vector", "scalar", "gpsimd")

    def __init__(self, nc):
        self.nc = nc
        self.es = contextlib.ExitStack()
        self.ops = {e: [] for e in self.ENGS}
        self.esem = {}
        self.ecnt = {e: 0 for e in self.ENGS}
        self.seen = {e: {} for e in self.ENGS}
        self.nsem = 0
        self.pending_dma = []
        for e in self.ENGS:
            self.esem[e] = self.new_sem("e_" + e)
        self.n_ops = 0

    def new_sem(self, name):
        self.nsem += 1
        return self.es.enter_context(self.nc.semaphore(f"{name}_{self.nsem}"))

    def sb(self, name, shape, dt):
        return self.es.enter_context(self.nc.sbuf_tensor(name, list(shape), dt))

    def ps(self, name, shape, dt):
        return self.es.enter_context(self.nc.psum_tensor(name, list(shape), dt))

    def _need(self, eng, tok, waits):
        if tok is None:
            return
        sem, val, weng = tok
        if weng == eng == "tensor":
            return
        k = id(sem)
        cur = self.seen[eng].get(k)
        if cur is not None and cur[1] >= val:
            return
        for i, (s, v) in enumerate(waits):
            if s is sem:
                if v < val:
                    waits[i] = (s, val)
                return
        waits.append((sem, val))

    def op(self, eng, fn, reads=(), writes=(), dma_dst=None):
        waits = []
        for b in reads:
            self._need(eng, b.w, waits)
        for b in writes:
            self._need(eng, b.w, waits)
            for t in b.r:
                self._need(eng, t, waits)
        for s, v in waits:
            self.seen[eng][id(s)] = (s, v)
        if dma_dst is not None:
            if dma_dst.dsem is None or dma_dst.dcnt + 16 > SEM_ROLL:
                dma_dst.dsem = self.new_sem("d_" + dma_dst.name)
                dma_dst.dcnt = 0
            dma_dst.dcnt += 16
            tok = (dma_dst.dsem, dma_dst.dcnt, "dma")
            inc = (dma_dst.dsem, 16)
            self.pending_dma.append(tok)
        else:
            if self.ecnt[eng] + 1 > SEM_ROLL:
                self.esem[eng] = self.new_sem("e_" + eng)
                self.ecnt[eng] = 0
            self.ecnt[eng] += 1
            tok = (self.esem[eng], self.ecnt[eng], eng)
            inc = (self.esem[eng], 1)
        for b in writes:
            b.w = tok
            b.r = []
        for b in reads:
            if b not in writes:
                b.r.append(tok)
        wl = list(waits)

        def emit(e, fn=fn, wl=wl, inc=inc):
            for s, v in wl:
                e.wait_ge(s, v)
            fn(e).then_inc(inc[0], inc[1])

        self.ops[eng].append(emit)
        self.n_ops += 1
        return tok

    def barrier(self):
        toks = [(self.esem[e], self.ecnt[e], e) for e in self.ENGS if self.ecnt[e] > 0]
        toks += self.pending_dma
        self.pending_dma = []
        for eng in self.ENGS:
            waits = []
            for t in toks:
                if t[2] == eng:
                    continue
                self._need(eng, t, waits)
            for s, v in waits:
                self.seen[eng][id(s)] = (s, v)
            wl = list(waits)

            def emit(e, wl=wl):
                for s, v in wl:
                    e.wait_ge(s, v)
            self.ops[eng].append(emit)

    def dma(self, out, in_, reads=(), writes=(), q="sync", **kw):
        return self.op(q, lambda e: e.dma_start(out=out, in_=in_, **kw),
                       reads=reads, writes=writes, dma_dst=writes[0])

    def mm(self, out, lhsT, rhs, start, stop, reads, writes, tp=None):
        if tp is not None:
            return self.op("tensor", lambda e: e.matmul(out, lhsT, rhs, start=start, stop=stop, tile_position=tp),
                           reads=reads, writes=writes)
        return self.op("tensor", lambda e: e.matmul(out, lhsT, rhs, start=start, stop=stop),
                       reads=reads, writes=writes)

    def tr(self, out, in_, ident, reads, writes):
        return self.op("tensor", lambda e: e.transpose(out, in_, ident), reads=reads, writes=writes)

    def act(self, out, in_, func, reads, writes, scale=1.0, bias=0.0):
        return self.op("scalar", lambda e: e.activation(out=out, in_=in_, func=func, scale=scale, bias=bias),
                       reads=reads, writes=writes)

    def tt(self, eng, out, in0, in1, op, reads, writes):
        return self.op(eng, lambda e: e.tensor_tensor(out, in0, in1, op), reads=reads, writes=writes)

    def ts(self, eng, out, in0, s1, s2, op0, op1, reads, writes):
        if s2 is None:
            return self.op(eng, lambda e: e.tensor_scalar(out, in0, s1, None, op0), reads=reads, writes=writes)
        return self.op(eng, lambda e: e.tensor_scalar(out, in0, s1, s2, op0, op1), reads=reads, writes=writes)

    def stt(self, out, in0, scalar, in1, op0, op1, reads, writes):
        return self.op("vector", lambda e: e.scalar_tensor_tensor(out, in0, scalar, in1, op0, op1),
                       reads=reads, writes=writes)

    def copy(self, eng, out, in_, reads, writes):
        if eng == "scalar":
            return self.act(out, in_, AF.Identity, reads, writes)
        return self.op(eng, lambda e: e.tensor_copy(out, in_), reads=reads, writes=writes)

    def memset(self, eng, ap, val, writes):
        return self.op(eng, lambda e: e.memset(ap, val), writes=writes)

    def finish(self, final_bufs):
        toks = [b.w for b in final_bufs if b.w is not None]
        ops = self.ops

        def fin(e):
            for s, v, _ in toks:
                e.wait_ge(s, v)
        ops["sync"].append(fin)
        with self.nc.Block() as block:
            @block.sync
            def _(e):
                for f in ops["sync"]:
                    f(e)

            @block.tensor
            def _(e):
                for f in ops["tensor"]:
                    f(e)

            @block.vector
            def _(e):
                for f in ops["vector"]:
                    f(e)

            @block.scalar
            def _(e):
                for f in ops["scalar"]:
                    f(e)

            @block.gpsimd
            def _(e):
                for f in ops["gpsimd"]:
                    f(e)
        self.es.close()


class Arena:
    def __init__(self, fw, nwords):
        self.t32 = fw.sb("arena", [128, nwords], F32)
        self.t16 = self.t32.bitcast(BF16)
        self.ti32 = self.t32.bitcast(I32)
        self.n = nwords
        self.top = 0
        self.peak = 0

    def ap32(self, off, dims):
        return bass.AP(self.t32, off, [[self.n, 128]] + [list(d) for d in dims])

    def ap16(self, off, dims):
        return bass.AP(self.t16, off, [[2 * self.n, 128]] + [list(d) for d in dims])

    def _take(self, nwords):
        self.last_off = self.top
        a = self.top
        self.top += nwords
        self.peak = max(self.peak, self.top)
        assert self.top <= self.n, f"arena overflow {self.top} > {self.n}"
        return a

    def f32(self, n):
        a = self._take(n)
        return self.t32[:, a:a + n]

    def i32(self, n):
        a = self._take(n)
        return self.ti32[:, a:a + n]

    def bf16(self, n):
        nw = (n + 1) // 2
        a = self._take(nw)
        return self.t16[:, 2 * a:2 * a + n]

    def mark(self):
        return self.top

    def release(self, m):
        self.top = m


class Prog:
    pass


def build_program(layers=(0, 1), tap=None, in_feature_major=False):
    nc = bass.Bass("TRN2", target_bir_lowering=False)
    fw = FW(nc)
    P = Prog()
    P.nc, P.fw = nc, fw

    def din(name, shape, dt=F32):
        return nc.dram_tensor(name, list(shape), dt, kind="ExternalInput").ap()

    I = {}
    I["x"] = din("x", [L, D])
    I["ctx"] = din("ctx", [NCTX, D])
    I["cc"] = din("cc", [2, D])
    I["ada_w"] = din("ada_w", [2, D, 6 * D])
    I["ada_b"] = din("ada_b", [2, 6 * D])
    I["norm1_g"] = din("norm1_g", [2, D])
    I["norm2_g"] = din("norm2_g", [2, D])
    if 0 in layers:
        I["mix_w_in"] = din("mix_w_in", [D, 2560])
        I["q_norm_g"] = din("q_norm_g", [64])
        I["k_norm_g"] = din("k_norm_g", [64])
        I["na_rpb"] = din("na_rpb", [8, 15, 31])
        I["conv_dw_w"] = din("conv_dw_w", [31, 512])
        I["conv_dw_b"] = din("conv_dw_b", [512])
        I["conv_ln_g"] = din("conv_ln_g", [512])
        I["conv_ln_b"] = din("conv_ln_b", [512])
        I["mix_w_out"] = din("mix_w_out", [D, D])
        I["ffn_w1"] = din("ffn_w1", [D, FFN_DIM])
        I["ffn_w3"] = din("ffn_w3", [D, FFN_DIM])
        I["ffn_w2"] = din("ffn_w2", [FFN_DIM, D])
    if 1 in layers:
        I["ssm_w_in"] = din("ssm_w_in", [D, D])
        I["ssm_a_re"] = din("ssm_a_re", [2, 64, 64])
        I["ssm_a_im"] = din("ssm_a_im", [2, 64, 64])
        I["ssm_log_dt"] = din("ssm_log_dt", [2, 64])
        I["ssm_b_re"] = din("ssm_b_re", [2, 64, 64, 16])
        I["ssm_b_im"] = din("ssm_b_im", [2, 64, 64, 16])
        I["ssm_c_re"] = din("ssm_c_re", [2, 64, 16, 64])
        I["ssm_c_im"] = din("ssm_c_im", [2, 64, 16, 64])
        I["ssm_d"] = din("ssm_d", [D])
        I["ssm_w_out"] = din("ssm_w_out", [D, 2 * D])
        I["moe_router"] = din("moe_router", [D, NEXP])
        I["moe_w1"] = din("moe_w1", [NEXP, D, EXPERT_DIM])
        I["moe_w3"] = din("moe_w3", [NEXP, D, EXPERT_DIM])
        I["moe_w2"] = din("moe_w2", [NEXP, EXPERT_DIM, D])
    P.I = I
    P.out_lat = nc.dram_tensor("out_lat", [L, D], F32, kind="ExternalOutput").ap()
    P.out_bufs = []
    if 1 not in layers:
        P.out_ctx = nc.dram_tensor("out_ctx", [NCTX, D], F32, kind="ExternalOutput").ap()
    P.tap = tap
    P.dbgsel = globals().get("DBGSEL", (0, 0, 0, 0))
    if tap is not None:
        P.dbg = nc.dram_tensor("dbg", [128, 8 * T], F32, kind="ExternalOutput").ap()
        P.bdbg = Buf("dbg")

    A = Arena(fw, 53150)
    P.A = A
    P.psum = [fw.ps(f"ps{i}", [128, 512], F32) for i in range(8)]
    P.bps = [Buf(f"ps{i}") for i in range(8)]

    setup_consts(P)
    P.hT = A.bf16(KC * T).rearrange("p (c t) -> p c t", c=KC)
    P.hT_off = A.last_off
    P.bh = [Buf(f"hT{i}") for i in range(5)]
    P.xT = None
    P.layers = layers
    done = False
    for l in layers:
        if l not in P.adaln_done and not (l == 0 and globals().get("ADA0_INTERLEAVE", False)):
            adaln(P, l)
            P.adaln_done.add(l)
        if l == 0:
            done = layer0(P)
        else:
            done = layer1(P)
        if done:
            break
    if not done:
        write_output(P)
    fw.finish(P.out_bufs + ([P.bdbg] if tap is not None else []))
    P.peak = A.peak
    return nc, P


def setup_consts(P):
    fw, A = P.fw, P.A
    P.bconst = Buf("const")
    bc = P.bconst
    io = A.f32(128)
    pio = A.f32(1)
    P.ident = A.f32(128)
    P.identb = A.bf16(128)
    P.onesD = A.f32(128)
    P.ones512 = A.f32(128)
    P.ones64 = A.f32(128)
    P.ones1 = A.bf16(128)
    P.iota_f = io
    P.pio = pio
    P.halfpi = A.f32(1)
    fw.memset("vector", P.halfpi, math.pi / 2.0, [bc])
    fw.op("gpsimd", lambda e: e.iota(io, [[1, 128]], base=0, channel_multiplier=0,
                                     allow_small_or_imprecise_dtypes=True), writes=[bc])
    fw.op("gpsimd", lambda e: e.iota(pio, [[0, 1]], base=0, channel_multiplier=1,
                                     allow_small_or_imprecise_dtypes=True), writes=[bc])
    fw.ts("vector", P.ident, io, pio[:, 0:1], None, ALU.is_equal, None, [bc], [bc])
    fw.copy("vector", P.identb, P.ident, [bc], [bc])
    fw.memset("vector", P.onesD, 1.0 / D, [bc])
    fw.memset("vector", P.ones512, 1.0 / 512, [bc])
    fw.memset("vector", P.ones1, 1.0 / D, [bc])
    fw.memset("vector", P.ones64, 0.0, [bc])
    fw.memset("vector", P.ones64[0:64, 0:64], 1.0 / 64, [bc])
    fw.memset("vector", P.ones64[64:128, 64:128], 1.0 / 64, [bc])
    P.mod = [A.f32(96).rearrange("p (m s) -> p m s", s=2) for _ in range(2)]
    P.modA = [A.f32(32).rearrange("p (n c s) -> p n c s", n=2, s=2) for _ in range(2)]
    P.gn = A.f32(32).rearrange("p (l n c) -> p l n c", l=2, n=2)
    P.bmod = Buf("mod")
    P.adaln_done = set()
    m_g = A.mark()
    gnat = A.f32(128)
    for n_, nm in enumerate(("norm1_g", "norm2_g")):
        fw.dma(gnat[n_ * 16:(n_ + 1) * 16, :], P.I[nm].rearrange("l (c p) -> (l c) p", p=128), writes=[P.bmod])
    fw.tr(P.psum[0][:, 0:32], gnat[0:32, :], P.ident[0:32, 0:32], [P.bmod, bc], [P.bps[0]])
    for n_ in range(2):
        fw.copy("vector", P.gn[:, :, n_, :], P.psum[0][:, n_ * 16:(n_ + 1) * 16].rearrange("p (l c) -> p l c", l=2),
                [P.bps[0]], [P.bmod])
    fw.barrier()
    A.release(m_g)


def adaln(P, l, hold=False, banks=(7, 6)):
    fw, A, I = P.fw, P.A, P.I
    m0 = A.mark()
    ccT = A.f32(16).rearrange("p (c s) -> p c s", s=2)
    scT = A.f32(16).rearrange("p (c s) -> p c s", s=2)
    adab = A.f32(48)
    bcc = Buf("cc")
    pan = [A.bf16(8 * 512).rearrange("p (k n) -> p k n", k=8) for _ in range(3)]
    bpan = [Buf("adapan0"), Buf("adapan1"), Buf("adapan2")]
    scTb = A.bf16(16).rearrange("p (c s) -> p c s", s=2)
    nat = A.f32(128)
    fw.dma(nat[0:16, :], I["cc"].rearrange("s (c p) -> (s c) p", p=128), writes=[bcc])
    fw.dma(nat[16:64, :], I["ada_b"][l].rearrange("(m p) -> m p", p=128), writes=[bcc])
    fw.tr(P.psum[banks[1]][:, 0:64], nat[0:64, :], P.ident[0:64, 0:64], [bcc, P.bconst], [P.bps[banks[1]]])
    fw.copy("vector", ccT, P.psum[banks[1]][:, 0:16].rearrange("p (s c) -> p c s", s=2), [P.bps[banks[1]]], [bcc])
    fw.copy("vector", adab, P.psum[banks[1]][:, 16:64], [P.bps[banks[1]]], [bcc])
    fw.act(scT, ccT, AF.Silu, [bcc], [bcc])
    fw.copy("vector", scTb, scT, [bcc], [bcc])
    wv = I["ada_w"][l].rearrange("(k p) n -> p k n", p=128)
    ps = P.psum[banks[0]]
    bps = P.bps[banks[0]]
    for pi in range(12):
        j = pi % 3
        fw.dma(pan[j], wv[:, :, pi * 512:(pi + 1) * 512], writes=[bpan[j]], q="gpsimd")
        for mi in range(4):
            m = pi * 4 + mi
            for k in range(8):
                fw.mm(ps[:, 2 * m:2 * m + 2], pan[j][:, k, mi * 128:(mi + 1) * 128], scTb[:, k, :],
                      k == 0, k == 7, [bpan[j], bcc], [bps])
    mod = P.mod[l]
    for s in range(2):
        fw.tt("vector", mod[:, :, s], ps[:, 0:96].rearrange("p (m s) -> p m s", s=2)[:, :, s], adab, ALU.add,
              [bps, bcc], [P.bmod])
    for n in range(2):
        for s in range(2):
            sc = mod[:, (3 * n + 1) * 8:(3 * n + 2) * 8, s]
            fw.stt(P.modA[l][:, n, :, s], sc, 1.0, P.gn[:, l, n, :], ALU.add, ALU.mult, [P.bmod], [P.bmod])
    if hold:
        return
    fw.barrier()
    A.release(m0)


def mod_cols(P, l, j, c, s):
    return P.mod[l][:, j * 8 + c, s:s + 1]


def load_xT_block(P, tb, dst, bdst, ev=0):
    fw, A = P.fw, P.A
    t0, tl = TBS[tb]
    for tt in range(tl // 128):
        j = P.xin_i % 2
        P.xin_i += 1
        tok = t0 + tt * 128
        src = P.I["x"][tok:tok + 128, :] if tok < L else P.I["ctx"][tok - L:tok - L + 128, :]
        fw.dma(P.xin[j], src, writes=[P.bxin[j]])
        for half in range(2):
            pb = P.tps[P.tps_i % 2]
            ps, bps = P.psum[pb], P.bps[pb]
            P.tps_i += 1
            for cc in range(4):
                c = half * 4 + cc
                fw.tr(ps[:, cc * 128:(cc + 1) * 128], P.xin[j][:, c * 128:(c + 1) * 128], P.ident,
                      [P.bxin[j], P.bconst], [bps])
            eng = "vector" if (P.tps_i % 2) else "scalar"
            fw.copy(eng, dst[:, half * 4:half * 4 + 4, tt * 128:(tt + 1) * 128],
                    ps[:, 0:512].rearrange("p (c t) -> p c t", c=4), [bps], [bdst])


def norm_mod_block(P, l, n, tb, xsrc, bx, scr):
    fw = P.fw
    t0, tl = TBS[tb]
    s = 1 if tb == 4 else 0
    sq, sd, tmp, bsq, bsd, btmp = scr
    pb = 6
    ps, bps = P.psum[pb], P.bps[pb]
    for c in range(KC):
        fw.act(sq[c % 2][:, 0:tl], xsrc[:, c, 0:tl], AF.Square, [bx], [bsq[c % 2]])
        fw.mm(ps[:, 0:tl], P.ones1, sq[c % 2][:, 0:tl], c == 0, c == KC - 1, [bsq[c % 2], P.bconst], [bps])
    fw.act(sd[:, 0:tl], ps[:, 0:tl], AF.Sqrt, [bps], [bsd], bias=P.epsc[:, 0:1])
    fw.op("vector", lambda e: e.reciprocal(sd[:, 0:tl], sd[:, 0:tl]), reads=[bsd], writes=[bsd])
    for c in range(KC):
        j = c % 2
        fw.stt(tmp[j][:, 0:tl], xsrc[:, c, 0:tl], P.modA[l][:, n, c, s:s + 1], sd[:, 0:tl], ALU.mult, ALU.mult,
               [bx, bsd, P.bmod], [btmp[j]])
        fw.act(P.hT[:, c, t0:t0 + tl], tmp[j][:, 0:tl], AF.Identity, [btmp[j], P.bmod], [P.bh[tb]],
               bias=mod_cols(P, l, 3 * n, c, s))


def norm_scratch(P):
    A = P.A
    sq = [A.bf16(512), A.bf16(512)]
    sd = A.f32(512)
    tmp = [A.f32(512), A.f32(512)]
    return (sq, sd, tmp, [Buf("sq0"), Buf("sq1")], Buf("sd"), [Buf("tmp0"), Buf("tmp1")])


def tap_out(P, ap2d, ncols, bufs):
    P.fw.dma(P.dbg[:, 0:ncols], ap2d, reads=bufs, writes=[P.bdbg])


def tap_bf16(P, src3, n, bufs):
    fw, A = P.fw, P.A
    m = A.mark()
    t = A.f32(2304)
    bt = Buf("tapt")
    for c in range(n // 2304 if n >= 2304 else 1):
        w = min(n, 2304)
        fw.copy("vector", t[:, 0:w], src3[:, c * 2304:c * 2304 + w], bufs, [bt])
        fw.dma(P.dbg[:, c * 2304:c * 2304 + w], t[:, 0:w], reads=[bt], writes=[P.bdbg])
    A.release(m)


def layer0(P):
    fw, A, I = P.fw, P.A, P.I
    l = 0
    if not hasattr(P, "epsc"):
        P.epsc = A.f32(1)
        fw.memset("vector", P.epsc, EPS, [P.bconst])
    mM = A.mark()
    P.xin = [A.f32(D), A.f32(D)]
    P.bxin = [Buf("xin0"), Buf("xin1")]
    P.xin_i = 0
    P.tps = [4, 5]
    P.tps_i = 0
    xblk = [A.f32(KC * 512).rearrange("p (c t) -> p c t", c=KC) for _ in range(2)]
    bxb = [Buf("xblk0"), Buf("xblk1")]
    scr = norm_scratch(P)

    def phase0():
        for tb in range(5):
            j = tb % 2
            load_xT_block(P, tb, xblk[j], bxb[j])
            norm_mod_block(P, l, 0, tb, xblk[j], bxb[j], scr)
    if 0 not in P.adaln_done:
        def rec0(fn, *a_, **k_):
            calls = []
            real = fw.op
            fw.op = lambda *aa, **kk: calls.append((aa, kk))
            try:
                fn(*a_, **k_)
            finally:
                fw.op = real
            return calls
        cb = rec0(adaln, P, 0, hold=True, banks=(7, 3))
        P.adaln_done.add(0)
        ca = rec0(phase0)
        na, nb = len(ca), len(cb)
        ia = ib = 0
        while ia < na or ib < nb:
            if ia >= na or (ib < nb and ib * max(na, 1) <= ia * max(nb, 1) * 3):
                aa, kk = cb[ib]; ib += 1
            else:
                aa, kk = ca[ia]; ia += 1
            fw.op(*aa, **kk)
    else:
        phase0()
    if P.tap == "h1_0":
        tap_bf16(P, P.hT.rearrange("p c t -> p (c t)"), KC * T, P.bh)
        return True
    fw.barrier()
    A.release(mM)
    qkT = A.bf16(8 * T).rearrange("p (c t) -> p c t", c=8)
    bqk = [[Buf(f"qk{c}_{tb}") for tb in range(5)] for c in range(8)]
    V = A.bf16(18 * 8 * 65).rearrange("p (j h d) -> p j h d", j=18, h=8)
    bV = [Buf(f"V{j}") for j in range(18)]
    mB3 = A.mark()
    ULEN = 2078 + 286
    u = A.bf16(4 * ULEN).rearrange("p (c t) -> p c t", c=4)
    bu = [[Buf(f"u{c}_{tb}") for tb in range(5)] for c in range(4)]
    mB2 = A.mark()
    pans = [A.bf16(8 * 512).rearrange("p (k n) -> p k n", k=8) for _ in range(4)]
    bpan = [Buf(f"pan{i}") for i in range(4)]
    sq = [A.bf16(512) for _ in range(2)]
    bsq = [Buf("qsq0"), Buf("qsq1")]
    ones64b = A.bf16(128)
    fw.copy("vector", ones64b, P.ones64, [P.bconst], [P.bconst])
    sd = [A.f32(512) for _ in range(2)]
    bsd = [Buf("qsd0"), Buf("qsd1")]
    gq = A.f32(1)
    gk = A.f32(1)
    bg = Buf("gqk")
    for hh in range(2):
        fw.dma(gq[hh * 64:(hh + 1) * 64, :], I["q_norm_g"].rearrange("(d o) -> d o", o=1), writes=[bg], **SLOW)
        fw.dma(gk[hh * 64:(hh + 1) * 64, :], I["k_norm_g"].rearrange("(d o) -> d o", o=1), writes=[bg], **SLOW)
    fw.ts("vector", gq, gq, 0.125, None, ALU.mult, None, [bg], [bg])
    fw.memset("vector", V[:, :, :, 64:65], 1.0, bV)
    fw.memset("vector", u.rearrange("p c t -> p (c t)"), 0.0, [b for r in bu for b in r])
    wv = I["mix_w_in"].rearrange("(k p) n -> p k n", p=128)
    for pi in range(5):
        fw.dma(pans[pi % 4], wv[:, :, pi * 512:(pi + 1) * 512], writes=[bpan[pi % 4]], q="gpsimd")
        if pi == 3:
            break
    prj = [0, 1, 2, 3]
    prj_i = [0]
    st_i = [0]
    pending = [None]

    def qk_tail(args):
        c, tb, pb = args
        t0, tl = TBS[tb]
        ps, bps = P.psum[pb], P.bps[pb]
        sb_ = 4 + st_i[0] % 2
        j = st_i[0] % 2
        st_i[0] += 1
        ps2, bps2 = P.psum[sb_], P.bps[sb_]
        fw.mm(ps2[:, 0:tl], ones64b, sq[j][:, 0:tl], True, True, [bsq[j], P.bconst], [bps2])
        fw.act(sd[j][:, 0:tl], ps2[:, 0:tl], AF.Sqrt, [bps2], [bsd[j]], bias=P.epsc[:, 0:1])
        fw.op("vector", lambda e: e.reciprocal(sd[j][:, 0:tl], sd[j][:, 0:tl]), reads=[bsd[j]], writes=[bsd[j]])
        g = gq if c < 4 else gk
        fw.stt(qkT[:, c, t0:t0 + tl], ps[:, 0:tl], g[:, 0:1], sd[j][:, 0:tl], ALU.mult, ALU.mult,
               [bps, bsd[j], bg], [bqk[c][tb]])

    def b1_qk():
        for pi in range(2):
            pan, bp = pans[pi], bpan[pi]
            for tb in range(5):
                t0, tl = TBS[tb]
                for mc in range(4):
                    c = pi * 4 + mc
                    pb = prj[prj_i[0] % 4]
                    prj_i[0] += 1
                    ps, bps = P.psum[pb], P.bps[pb]
                    for k in range(KC):
                        fw.mm(ps[:, 0:tl], pan[:, k, mc * 128:(mc + 1) * 128], P.hT[:, k, t0:t0 + tl],
                              k == 0, k == KC - 1, [bp, P.bh[tb]], [bps])
                    j = (st_i[0] + (1 if pending[0] is not None else 0)) % 2
                    fw.act(sq[j][:, 0:tl], ps[:, 0:tl], AF.Square, [bps], [bsq[j]])
                    if pending[0] is not None:
                        qk_tail(pending[0])
                    pending[0] = (c, tb, pb)
        qk_tail(pending[0])
        pending[0] = None

    if 1 in P.layers and 1 not in P.adaln_done:
        def record_(fn, *a_, **k_):
            calls = []
            real = fw.op
            fw.op = lambda *aa, **kk: calls.append((aa, kk))
            try:
                fn(*a_, **k_)
            finally:
                fw.op = real
            return calls
        ca = record_(b1_qk)
        cb = record_(adaln, P, 1, hold=True, banks=(7, 6))
        P.adaln_done.add(1)
        na, nb = len(ca), len(cb)
        ia = ib = 0
        while ia < na or ib < nb:
            if ib >= nb or (ia < na and ia * max(nb, 1) <= ib * max(na, 1)):
                aa, kk = ca[ia]; ia += 1
            else:
                aa, kk = cb[ib]; ib += 1
            fw.op(*aa, **kk)
    else:
        b1_qk()
    pan, bp = pans[2], bpan[2]
    fw.dma(pans[0], wv[:, :, 4 * 512:5 * 512], writes=[bpan[0]], q="gpsimd")
    for tt in range(18):
        pb = prj[prj_i[0] % 4]
        prj_i[0] += 1
        ps, bps = P.psum[pb], P.bps[pb]
        tb = min(tt // 4, 4)
        for k in range(KC):
            fw.mm(ps[:, :], P.hT[:, k, tt * 128:(tt + 1) * 128], pan[:, k, :], k == 0, k == KC - 1,
                  [bp, P.bh[tb]], [bps])
        fw.copy("scalar" if tt % 2 else "vector", V[:, tt, :, 0:64], ps[:, :].rearrange("p (h d) -> p h d", h=8),
                [bps], [bV[tt]])
    sg = [A.f32(512) for _ in range(2)]
    bsg = [Buf("sg0"), Buf("sg1")]
    pa, bpa, pg, bpg = pans[3], bpan[3], pans[0], bpan[0]

    def uoff(tb):
        t0, tl = TBS[tb]
        return (15 + t0) if tb < 4 else (2078 + 15)
    for tb in range(5):
        t0, tl = TBS[tb]
        for mc in range(4):
            pba = prj[prj_i[0] % 4]
            pbg = prj[(prj_i[0] + 1) % 4]
            prj_i[0] += 2
            for (pn, bpn, pb) in ((pa, bpa, pba), (pg, bpg, pbg)):
                for k in range(KC):
                    fw.mm(P.psum[pb][:, 0:tl], pn[:, k, mc * 128:(mc + 1) * 128], P.hT[:, k, t0:t0 + tl],
                          k == 0, k == KC - 1, [bpn, P.bh[tb]], [P.bps[pb]])
            j = (tb * 4 + mc) % 2
            fw.act(sg[j][:, 0:tl], P.psum[pbg][:, 0:tl], AF.Sigmoid, [P.bps[pbg]], [bsg[j]])
            o = uoff(tb)
            fw.tt("vector", u[:, mc, o:o + tl], P.psum[pba][:, 0:tl], sg[j][:, 0:tl], ALU.mult,
                  [P.bps[pba], bsg[j]], [bu[mc][tb]])
    fw.barrier()
    A.release(mB2)
    catT = P.hT
    bcat = P.bh
    wrow = A.f32(512)
    cw = A.f32(4 * 31).rearrange("p (c k) -> p c k", c=4)
    cvec = A.f32(12).rearrange("p (v c) -> p v c", v=3)
    bcw = Buf("convw")
    fw.dma(wrow[0:31, :], I["conv_dw_w"], writes=[bcw])
    for vi, nm in enumerate(("conv_dw_b", "conv_ln_g", "conv_ln_b")):
        fw.dma(cvec[:, vi, :], I[nm].rearrange("(c p) -> p c", p=128), writes=[bcw], **SLOW)
    for c in range(4):
        fw.tr(P.psum[0][:, c * 32:c * 32 + 31], wrow[0:31, c * 128:(c + 1) * 128], P.ident[0:31, 0:31],
              [bcw, P.bconst], [P.bps[0]])
    fw.copy("vector", cw, P.psum[0][:, 0:128].rearrange("p (c k) -> p c k", c=4)[:, :, 0:31], [P.bps[0]], [bcw])
    acc = [A.f32(512) for _ in range(4)]
    bacc = [Buf(f"acc{c}") for c in range(4)]
    Dg = [[A.bf16(128) for _ in range(31)] for _ in range(4)]
    bDg = Buf("Dg")
    for c in range(4):
        for k in range(31):
            fw.ts("vector", Dg[c][k], P.identb, cw[:, c, k:k + 1], None, ALU.mult, None, [bcw, P.bconst], [bDg])
    csq = [A.f32(512) for _ in range(2)]
    bcsq = [Buf("csq0"), Buf("csq1")]
    mean = A.f32(512)
    msq = A.f32(512)
    rs = A.f32(512)
    bst = Buf("lnstat")
    t1 = [A.f32(512) for _ in range(2)]
    bt1 = [Buf("lt0"), Buf("lt1")]
    for tb in range(5):
        t0, tl = TBS[tb]
        ub = t0 if tb < 4 else 2078
        pm, bpm = P.psum[1 + 2 * (tb % 2)], P.bps[1 + 2 * (tb % 2)]
        pe2, bpe2 = P.psum[2 + 2 * (tb % 2)], P.bps[2 + 2 * (tb % 2)]
        for c in range(4):
            pcv = 5 + (tb * 4 + c) % 3
            for k in range(31):
                fw.mm(P.psum[pcv][:, 0:tl], Dg[c][k], u[:, c, ub + k:ub + k + tl], k == 0, k == 30,
                      [bu[c][tbb] for tbb in range(5)] + [bDg], [P.bps[pcv]])
            fw.act(acc[c][:, 0:tl], P.psum[pcv][:, 0:tl], AF.Identity, [P.bps[pcv], bcw], [bacc[c]],
                   bias=cvec[:, 0, c:c + 1])
            fw.mm(pm[:, 0:tl], P.ones512, acc[c][:, 0:tl], c == 0, c == 3, [bacc[c], P.bconst], [bpm])
            fw.act(csq[c % 2][:, 0:tl], acc[c][:, 0:tl], AF.Square, [bacc[c]], [bcsq[c % 2]])
            fw.mm(pe2[:, 0:tl], P.ones512, csq[c % 2][:, 0:tl], c == 0, c == 3, [bcsq[c % 2], P.bconst], [bpe2])
        fw.act(mean[:, 0:tl], pm[:, 0:tl], AF.Identity, [bpm], [bst])
        fw.act(msq[:, 0:tl], pm[:, 0:tl], AF.Square, [bpm], [bst])
        fw.tt("vector", rs[:, 0:tl], pe2[:, 0:tl], msq[:, 0:tl], ALU.subtract, [bpe2, bst], [bst])
        fw.ts("vector", rs[:, 0:tl], rs[:, 0:tl], 0.0, None, ALU.max, None, [bst], [bst])
        fw.act(rs[:, 0:tl], rs[:, 0:tl], AF.Sqrt, [bst], [bst], bias=P.epsc[:, 0:1])
        fw.op("vector", lambda e, tl=tl: e.reciprocal(rs[:, 0:tl], rs[:, 0:tl]), reads=[bst], writes=[bst])
        for c in range(4):
            j = c % 2
            fw.tt("vector", t1[j][:, 0:tl], acc[c][:, 0:tl], mean[:, 0:tl], ALU.subtract, [bacc[c], bst], [bt1[j]])
            fw.tt("vector", t1[j][:, 0:tl], t1[j][:, 0:tl], rs[:, 0:tl], ALU.mult, [bst], [bt1[j]])
            fw.act(catT[:, 4 + c, t0:t0 + tl], t1[j][:, 0:tl], AF.Silu, [bt1[j], bcw], [bcat[tb]],
                   scale=cvec[:, 1, c:c + 1], bias=cvec[:, 2, c:c + 1])
    fw.barrier()
    A.release(mB3)
    Fs = A.f32(127)
    bF = Buf("Fs")
    Fd = P.nc.dram_tensor("Fd_scratch", [120, 64, 127], F32).ap()
    Fs_off = A.last_off
    bFd = Buf("Fd")
    fw.memset("vector", Fs, 0.0, [bF])
    rp = I["na_rpb"]
    for h_ in range(8):
        fw.dma(Fs[h_ * 15:(h_ + 1) * 15, 48:79], bass.AP(rp.tensor, h_ * 15 * 31 + 30, [[31, 15], [-1, 31]]),
               writes=[bF], **SLOW)
    for h_ in range(8):
        fw.dma(Fd[h_ * 15:(h_ + 1) * 15], bass.AP(A.t32, Fs_off + h_ * 15 * A.n, [[A.n, 15], [0, 64], [1, 127]]),
               reads=[bF], writes=[bFd])
    TT = A.f32(8 * 14 * 64)
    TT_off = A.last_off
    TT4 = TT.rearrange("p (h e q) -> p h e q", h=8, e=14)
    bTT = Buf("TT")
    for h in range(8):
        for krl in range(2):
            src = bass.AP(Fd.tensor, (h * 15 + krl) * 64 * 127 + 63, [[126, 64], [64 * 127, 14], [1, 64]])
            fw.dma(TT4[krl * 64:(krl + 1) * 64, h, :, :], src, reads=[bFd], writes=[bTT])
    cm = A.f32(64)
    cm_off = A.last_off
    cs = A.f32(64)
    kcol = A.f32(1)
    bcm = Buf("cm")
    fw.op("gpsimd", lambda e: e.iota(kcol[0:64, :], [[0, 1]], base=0, channel_multiplier=1,
                                     allow_small_or_imprecise_dtypes=True), writes=[bcm])
    fw.op("gpsimd", lambda e: e.iota(kcol[64:128, :], [[0, 1]], base=0, channel_multiplier=1,
                                     allow_small_or_imprecise_dtypes=True), writes=[bcm])
    fw.ts("vector", cs, P.iota_f[:, 0:64], -8.0, 0.0, ALU.add, ALU.max, [P.bconst], [bcm])
    fw.ts("vector", cs, cs, 48.0, kcol[:, 0:1], ALU.min, ALU.subtract, [bcm], [bcm])
    fw.ts("vector", cm, cs, 0.0, None, ALU.is_le, None, [bcm], [bcm])
    fw.ts("vector", cs, cs, -15.0, None, ALU.is_ge, None, [bcm], [bcm])
    fw.tt("vector", cm, cm, cs, ALU.mult, [bcm], [bcm])
    fw.ts("vector", cm, cm, -NEG, NEG, ALU.mult, ALU.add, [bcm], [bcm])
    fw.tt("vector", TT.rearrange("p (m q) -> p m q", q=64), TT.rearrange("p (m q) -> p m q", q=64),
          A.ap32(cm_off, [[0, 112], [1, 64]]), ALU.add, [bcm, bTT], [bTT])
    ssb = [A.f32(320) for _ in range(2)]
    bssb = [Buf("ssb0"), Buf("ssb1")]
    pT = [A.bf16(448) for _ in range(3)]
    bpT = [Buf(f"pT{i}") for i in range(3)]
    rec = A.f32(8)
    A_rec_off = [A.last_off]
    brec = Buf("rec")
    osb = [A.bf16(512) for _ in range(2)]
    bosb = [Buf("osb0"), Buf("osb1")]
    its = []
    for qr in range(36):
        if qr < 32:
            ws = min(max(qr - 4, 0), 24)
            j0, j1 = ws // 2, (ws + 7) // 2
            nw = j1 - j0 + 1
            qtok = qr * 64
            e0 = 2 * j0 - qr + 7
            qtb = qr // 8
        else:
            ws, j0, nw, e0 = 0, 0, 0, 0
            qtok = L + (qr - 32) * 64
            qtb = 4
        for h in range(8):
            its.append((qr, h, ws, j0, nw, qtok, e0, qtb))

    def att_S(it):
        qr, h, ws, j0, nw, qtok, e0, qtb = its[it]
        pb = (h % 2) * 64
        cq, ck = h // 2, 4 + h // 2
        ps, bps = P.psum[it % 3], P.bps[it % 3]
        for i in range(nw + 2):
            ktok = (j0 + i) * 128 if i < nw else L + (i - nw) * 128
            ktb = min(ktok // 512, 4)
            fw.mm(ps[:, i * 64:(i + 1) * 64], qkT[pb:pb + 64, ck, ktok:ktok + 128],
                  qkT[pb:pb + 64, cq, qtok:qtok + 64], True, True, [bqk[ck][ktb], bqk[cq][qtb]], [bps])

    def att_SM(it):
        qr, h, ws, j0, nw, qtok, e0, qtb = its[it]
        ps, bps = P.psum[it % 3], P.bps[it % 3]
        ntile = nw + 2
        p_, bp_ = pT[it % 3], bpT[it % 3]
        if nw > 0:
            s_, bs_ = ssb[it % 2], bssb[it % 2]
            fw.tt("vector", s_[:, 0:nw * 64].rearrange("p (i q) -> p i q", q=64),
                  ps[:, 0:nw * 64].rearrange("p (i q) -> p i q", q=64),
                  A.ap32(TT_off + (h * 14 + e0) * 64, [[128, nw], [1, 64]]), ALU.add, [bps, bTT], [bs_])
            fw.act(p_[:, 0:nw * 64], s_[:, 0:nw * 64], AF.Exp, [bs_], [bp_])
        fw.act(p_[:, nw * 64:ntile * 64], ps[:, nw * 64:ntile * 64], AF.Exp, [bps], [bp_])

    def att_PV(it):
        qr, h, ws, j0, nw, qtok, e0, qtb = its[it]
        ntile = nw + 2
        p_, bp_ = pT[it % 3], bpT[it % 3]
        pso = [3 + 2 * (qr % 2), 4 + 2 * (qr % 2)]
        po, bpo = P.psum[pso[h // 4]], P.bps[pso[h // 4]]
        oc = (h % 4) * 65
        for i in range(ntile):
            if i < nw:
                vt = j0 + i
                r0, r1 = 0, 128
                if ws % 2 == 1 and i == 0:
                    r0 = 64
                if ws % 2 == 1 and i == nw - 1:
                    r1 = 64
            else:
                vt = 16 + (i - nw)
                r0, r1 = 0, 128
            fw.mm(po[0:64, oc:oc + 65], p_[r0:r1, i * 64:(i + 1) * 64], V[r0:r1, vt, h, :],
                  i == 0, i == ntile - 1, [bp_, bV[vt]], [bpo])

    def att_FIN(qr, qtok, qtb):
        pso = [3 + 2 * (qr % 2), 4 + 2 * (qr % 2)]
        ob, bob = osb[qr % 2], bosb[qr % 2]
        for half in range(2):
            po, bpo = P.psum[pso[half]], P.bps[pso[half]]
            pv = po[0:64, 0:260].rearrange("p (h d) -> p h d", h=4)
            fw.op("vector", lambda e, pv=pv, half=half: e.reciprocal(rec[0:64, half * 4:half * 4 + 4], pv[:, :, 64]),
                  reads=[bpo], writes=[brec])
            rb = bass.AP(A.t32, A_rec_off[0] + half * 4, [[A.n, 64], [1, 4], [0, 64]])
            fw.tt("vector", ob[0:64, half * 256:(half + 1) * 256].rearrange("p (h d) -> p h d", h=4),
                  pv[:, :, 0:64], rb, ALU.mult, [bpo, brec], [bob])
        pt, bpt = P.psum[7], P.bps[7]
        for c in range(4):
            fw.mm(pt[:, c * 64:(c + 1) * 64], ob[0:64, c * 128:(c + 1) * 128], P.identb[0:64, 0:64], True, True,
                  [bob, P.bconst], [bpt])
        fw.copy("scalar", catT[:, 0:4, qtok:qtok + 64], pt[:, 0:256].rearrange("p (c t) -> p c t", c=4),
                [bpt], [bcat[qtb]])

    NA = len(its)
    LOOK = 2
    for i in range(min(LOOK, NA)):
        att_S(i)
    pend_fin = None
    for it in range(NA):
        if it + LOOK < NA:
            att_S(it + LOOK)
        att_SM(it)
        att_PV(it)
        if pend_fin is not None:
            att_FIN(*pend_fin)
            pend_fin = None
        if its[it][1] == 7:
            pend_fin = (its[it][0], its[it][5], its[it][7])
    if pend_fin is not None:
        att_FIN(*pend_fin)
    fw.barrier()
    A.release(mM)
    xT = A.f32(KC * T).rearrange("p (c t) -> p c t", c=KC)
    P.xT = xT
    P.bx = [Buf(f"xT{i}") for i in range(5)]
    mX = A.mark()
    P.xin = [A.f32(D), A.f32(D)]
    P.bxin = [Buf("xin0"), Buf("xin1")]
    P.xin_i = 0
    P.tps = [4, 5]
    P.tps_i = 0
    wo = [A.bf16(8 * 512).rearrange("p (k n) -> p k n", k=8) for _ in range(2)]
    bwo = [Buf("wo0"), Buf("wo1")]
    wov = I["mix_w_out"].rearrange("(k p) n -> p k n", p=128)
    for i in range(2):
        fw.dma(wo[i], wov[:, :, i * 512:(i + 1) * 512], writes=[bwo[i]], q="gpsimd")
    for tb in range(5):
        t0, tl = TBS[tb]
        s_ = 1 if tb == 4 else 0
        load_xT_block(P, tb, xT[:, :, t0:t0 + tl], P.bx[tb])
        for m in range(8):
            pb = m % 4
            ps, bps = P.psum[pb], P.bps[pb]
            for k in range(KC):
                fw.mm(ps[:, 0:tl], wo[m // 4][:, k, (m % 4) * 128:(m % 4 + 1) * 128], catT[:, k, t0:t0 + tl],
                      k == 0, k == KC - 1, [bwo[m // 4], bcat[tb]], [bps])
            fw.stt(xT[:, m, t0:t0 + tl], ps[:, 0:tl], mod_cols(P, l, 2, m, s_), xT[:, m, t0:t0 + tl], ALU.mult, ALU.add,
                   [bps, P.bmod], [P.bx[tb]])
    fw.barrier()
    A.release(mX)
    if P.tap == "xmix0":
        tap_out(P, xT.rearrange("p c t -> p (c t)"), KC * T, P.bx)
        return True
    scr = norm_scratch(P)
    for tb in range(5):
        t0, tl = TBS[tb]
        norm_mod_block(P, l, 1, tb, xT[:, :, t0:t0 + tl], P.bx[tb], scr)
    fw.barrier()
    A.release(mX)
    fb = ffn_buffers(P, T)
    ffn(P, l, fb, I["ffn_w1"], I["ffn_w3"], I["ffn_w2"], FFN_DIM, [(0, 1024), (1024, 1024), (2048, 256)], 5)
    fw.barrier()
    A.release(mX)
    if P.tap == "xffn0":
        tap_out(P, xT.rearrange("p c t -> p (c t)"), KC * T, P.bx)
        return True
    return False


def ffn_buffers(P, ntok):
    A = P.A
    fb = Prog()
    fb.w1p = [A.bf16(8 * 512).rearrange("p (k n) -> p k n", k=8) for _ in range(2)]
    fb.w3p = [A.bf16(8 * 512).rearrange("p (k n) -> p k n", k=8) for _ in range(2)]
    fb.w2p = [A.bf16(4 * D).rearrange("p (f d) -> p f d", f=4) for _ in range(2)]
    fb.bw = [[Buf(f"w{n}p{i}") for i in range(2)] for n in range(3)]
    fb.act = A.bf16(4 * ntok).rearrange("p (f t) -> p f t", f=4)
    fb.bact = [[Buf(f"act{f}_{h}") for h in range(3)] for f in range(4)]
    fb.s = [A.bf16(1024) for _ in range(2)]
    fb.bs = [Buf("s0"), Buf("s1")]
    fb.a = [A.bf16(1024) for _ in range(2)]
    fb.ba = [Buf("a0"), Buf("a1")]
    fb.yi = 0
    fb.si = 0
    return fb


def ffn(P, l, fb, w1, w3, w2, F, halves, ntb, gate=None, bgate=None, first=True, nxt=None):
    fw = P.fw
    nfg = (F + 511) // 512
    w1v = w1.rearrange("(k p) n -> p k n", p=128)
    w3v = w3.rearrange("(k p) n -> p k n", p=128)

    def load(fg, wset):
        w1v_, w3v_, w2_ = wset
        j = fb.ldi % 2
        fb.ldi += 1
        nc_ = min(512, w2_.shape[0] - fg * 512)
        fw.dma(fb.w1p[j][:, :, 0:nc_], w1v_[:, :, fg * 512:fg * 512 + nc_], writes=[fb.bw[0][j]], q="gpsimd")
        fw.dma(fb.w3p[j][:, :, 0:nc_], w3v_[:, :, fg * 512:fg * 512 + nc_], writes=[fb.bw[1][j]], q="gpsimd")
        fw.dma(fb.w2p[j][:, 0:nc_ // 128, :], w2_[fg * 512:fg * 512 + nc_, :].rearrange("(f p) d -> p f d", p=128),
               writes=[fb.bw[2][j]], q="gpsimd")
    me = (w1v, w3v, w2)
    if first:
        fb.ldi = 0
        fb.usei = 0
        load(0, me)
    for fg in range(nfg):
        if fg + 1 < nfg:
            load(fg + 1, me)
        elif nxt is not None:
            n1, n3, n2 = nxt
            load(0, (n1.rearrange("(k p) n -> p k n", p=128), n3.rearrange("(k p) n -> p k n", p=128), n2))
        j = fb.usei % 2
        fb.usei += 1
        ncol = min(512, F - fg * 512)
        nfc = ncol // 128
        for hi, (t0, tl) in enumerate(halves):
            nb = (tl + 511) // 512
            for fc in range(nfc):
                for (wp, bw, banks) in ((fb.w1p[j], fb.bw[0][j], (0, 1)), (fb.w3p[j], fb.bw[1][j], (2, 3))):
                    for k in range(KC):
                        for b in range(nb):
                            bl = min(512, tl - b * 512)
                            tb = min((t0 + b * 512) // 512, 4)
                            fw.mm(P.psum[banks[b]][:, 0:bl], wp[:, k, fc * 128:(fc + 1) * 128],
                                  P.hT[:, k, t0 + b * 512:t0 + b * 512 + bl], k == 0, k == KC - 1,
                                  [bw, P.bh[tb]], [P.bps[banks[b]]])
                sj = fb.si % 2
                fb.si += 1
                for b in range(nb):
                    bl = min(512, tl - b * 512)
                    fw.act(fb.s[sj][:, b * 512:b * 512 + bl], P.psum[b][:, 0:bl], AF.Silu, [P.bps[b]], [fb.bs[sj]])
                for b in range(nb):
                    bl = min(512, tl - b * 512)
                    a0 = t0 + b * 512
                    if gate is None:
                        fw.tt("vector", fb.act[:, fc, a0:a0 + bl], P.psum[2 + b][:, 0:bl], fb.s[sj][:, b * 512:b * 512 + bl],
                              ALU.mult, [P.bps[2 + b], fb.bs[sj]], [fb.bact[fc][hi]])
                    else:
                        fw.tt("vector", fb.a[sj][:, b * 512:b * 512 + bl], P.psum[2 + b][:, 0:bl],
                              fb.s[sj][:, b * 512:b * 512 + bl], ALU.mult, [P.bps[2 + b], fb.bs[sj]], [fb.ba[sj]])
                if gate is not None:
                    fw.tt("vector", fb.act[:, fc, t0:t0 + tl], fb.a[sj][:, 0:tl], gate[:, t0:t0 + tl], ALU.mult,
                          [fb.ba[sj], bgate], [fb.bact[fc][hi]])
        for m in range(8):
            for tb in range(ntb):
                t0, tl = TBS[tb]
                s_ = 1 if tb == 4 else 0
                hi = [i for i, (h0, hl) in enumerate(halves) if h0 <= t0 < h0 + hl][0]
                pb = 4 + fb.yi % 4
                fb.yi += 1
                ps, bps = P.psum[pb], P.bps[pb]
                for fc in range(nfc):
                    fw.mm(ps[:, 0:tl], fb.w2p[j][:, fc, m * 128:(m + 1) * 128], fb.act[:, fc, t0:t0 + tl],
                          fc == 0, fc == nfc - 1, [fb.bw[2][j], fb.bact[fc][hi]], [bps])
                fw.stt(P.xT[:, m, t0:t0 + tl], ps[:, 0:tl], mod_cols(P, l, 5, m, s_), P.xT[:, m, t0:t0 + tl],
                       ALU.mult, ALU.add, [bps, P.bmod], [P.bx[tb]])


def rev(ap):
    dims = [list(d) for d in ap.ap]
    st, n = dims[-1]
    dims[-1] = [-st, n]
    return bass.AP(ap.tensor, ap.offset + st * (n - 1), dims)


def s5_chunked(P):
    fw, A, I = P.fw, P.A, P.I
    hT = P.hT
    V_ = "vector"
    TWO_PI = 2.0 * math.pi
    TWO_PI_S = 6.2831845
    NCH = T // 8
    NL = L // 8
    bprm = Buf("s5prm")
    B1 = [bprm]
    ident, identb = P.ident, P.identb

    def AP(v, dims, off=0):
        return bass.AP(v.tensor, v.offset + off, [list(v.ap[0])] + [list(d_) for d_ in dims])

    U8 = A.bf16(64 * NCH).rearrange("p (g c) -> p g c", g=64)
    bU8 = [Buf(f"U8_{g}") for g in range(64)]
    mW = A.mark()
    wi = [A.bf16(8 * 512).rearrange("p (k n) -> p k n", k=8) for _ in range(2)]
    bwi = [Buf("wi0"), Buf("wi1")]
    wiv = I["ssm_w_in"].rearrange("(k p) n -> p k n", p=128)
    for i in range(2):
        fw.dma(wi[i], wiv[:, :, i * 512:(i + 1) * 512], writes=[bwi[i]], q="gpsimd")
    utok = A.bf16(8 * D).rearrange("p (g s x) -> p g s x", g=64, s=8)
    butok = Buf("utok")
    pi = 0
    for (c0, M) in ((0, 128), (128, 128), (256, 32)):
        for s_ in range(8):
            for half in range(2):
                pb = pi % 4
                pi += 1
                t_lo = c0 * 8 + s_
                tbs = sorted(set(min(tt // 512, 4) for tt in (t_lo, t_lo + 8 * (M - 1))))
                for k in range(KC):
                    fw.mm(P.psum[pb][0:M, :], hT[:, k, t_lo:t_lo + 8 * (M - 1) + 1:8], wi[half][:, k, :], k == 0, k == KC - 1,
                          [P.bh[tb] for tb in range(tbs[0], tbs[-1] + 1)] + [bwi[half]], [P.bps[pb]])
                fw.copy("scalar" if pi % 2 else "vector", utok[0:M, half * 32:(half + 1) * 32, s_, :],
                        P.psum[pb][0:M, :].rearrange("p (g x) -> p g x", x=16), [P.bps[pb]], [butok])
        for g in range(64):
            pb = 4 + g % 4
            fw.mm(P.psum[pb][:, 0:M], utok[0:M, g, :, :].rearrange("p s x -> p (s x)"), identb[0:M, 0:M], True, True,
                  [butok, P.bconst], [P.bps[pb]])
            fw.copy("scalar" if g % 2 else "vector", U8[:, g, c0:c0 + M], P.psum[pb][:, 0:M], [P.bps[pb]], [bU8[g]])
    fw.barrier()
    A.release(mW)
    pw = [A.bf16(2048).rearrange("p (m e) -> p m e", e=16) for _ in range(2)]
    bb = [A.bf16(2048).rearrange("p (m x) -> p m x", x=16) for _ in range(2)]
    ctm = [A.bf16(2048).rearrange("p (m x) -> p m x", x=16) for _ in range(3)]
    thp8 = A.f32(64).rearrange("p (d s) -> p d s", d=2)
    r8 = A.f32(64).rearrange("p (d s) -> p d s", d=2)
    dvec = A.f32(64)
    Wsel = [A.t16[:, 2 * (P.hT_off + t_ * 1152 + 1024):2 * (P.hT_off + t_ * 1152 + 1024) + 240] for t_ in range(8)]
    mask = [A.f32(128), A.f32(128)]
    iotaC = [A.f32(NCH), A.f32(NCH)]
    XLre = [[A.bf16(128) for _ in range(2)] for _ in range(2)]
    XLim = [[A.bf16(128) for _ in range(2)] for _ in range(2)]
    DLre = [[A.bf16(128) for _ in range(2)] for _ in range(2)]
    DLim = [[A.bf16(128) for _ in range(2)] for _ in range(2)]
    Mg = [[A.bf16(128) for _ in range(2)] for _ in range(2)]
    bMat = [[Buf(f"mat{s_}{g_}") for g_ in range(2)] for s_ in range(2)]
    ybf = A.bf16(8 * NL).rearrange("p (g c) -> p g c", g=8)
    bybf = [Buf(f"ybf{g}") for g in range(8)]
    mW = A.mark()
    a_nat = A.f32(512).rearrange("p (i c) -> p i c", i=4)
    aT = A.f32(256).rearrange("p (i g) -> p i g", i=4)
    for ai, nm in enumerate(("ssm_a_re", "ssm_a_im")):
        for d in range(2):
            for rep in range(2):
                fw.dma(a_nat[0:64, ai * 2 + d, rep * 64:(rep + 1) * 64], I[nm][d], writes=[bprm])
    for idx in range(4):
        fw.tr(P.psum[idx][:, 0:64], a_nat[0:64, idx, :], ident[0:64, 0:64], B1 + [P.bconst], [P.bps[idx]])
        fw.copy(V_, aT[:, idx, :], P.psum[idx][:, 0:64], [P.bps[idx]], B1)
    are = aT[:, 0:2, :].rearrange("p d g -> p (d g)")
    aim = aT[:, 2:4, :].rearrange("p d g -> p (d g)")
    ldtb = A.f32(128)
    fw.dma(ldtb, bass.AP(I["ssm_log_dt"].tensor, 0, [[0, 128], [1, 128]]), writes=[bprm])
    dtb = A.f32(128); xre = A.f32(128); thp = A.f32(128)
    fw.act(dtb, ldtb, AF.Exp, B1, B1)
    fw.tt(V_, xre, are, dtb, ALU.mult, B1, B1)
    fw.tt(V_, thp, aim, dtb, ALU.mult, B1, B1)
    fw.ts(V_, thp, thp, 1.0 / TWO_PI, None, ALU.mult, None, B1, B1)
    evec = A.f32(16)
    fw.op("gpsimd", lambda e: e.iota(evec, [[1, 16]], base=-7, channel_multiplier=0,
                                     allow_small_or_imprecise_dtypes=True), writes=B1)
    ho = P.hT_off
    ang = A.t32[:, ho:ho + 2048]; ki = A.ti32[:, ho + 2048:ho + 4096]
    kf = A.t32[:, ho + 4096:ho + 6144]; mexp = A.t32[:, ho + 6144:ho + 8192]
    ang3 = ang.rearrange("p (m e) -> p m e", e=16)
    mexp3 = mexp.rearrange("p (m e) -> p m e", e=16)
    fw.tt(V_, ang3, AP(thp, [[1, 128], [0, 16]]), AP(evec, [[0, 128], [1, 16]]), ALU.mult, B1, B1)
    fw.tt(V_, mexp3, AP(xre, [[1, 128], [0, 16]]), AP(evec, [[0, 128], [1, 16]]), ALU.mult, B1, B1)
    fw.copy(V_, ki, ang, B1, B1)
    fw.copy(V_, kf, ki, B1, B1)
    fw.tt(V_, ang, ang, kf, ALU.subtract, B1, B1)
    fw.act(kf, ang, AF.Sin, B1, B1, scale=TWO_PI_S)
    fw.act(ang, ang, AF.Abs, B1, B1)
    fw.act(ang, ang, AF.Sin, B1, B1, scale=-TWO_PI, bias=P.halfpi[:, 0:1])
    fw.act(mexp, mexp, AF.Exp, B1, B1)
    fw.tt(V_, pw[0].rearrange("p m e -> p (m e)"), mexp, ang, ALU.mult, B1, B1)
    fw.tt(V_, pw[1].rearrange("p m e -> p (m e)"), mexp, kf, ALU.mult, B1, B1)
    lr1 = A.f32(128); li1 = A.f32(128); nrm = A.f32(128); ta = A.f32(128); tb_ = A.f32(128)
    kre = A.f32(128); kim = A.f32(128)
    kf3 = kf.rearrange("p (m e) -> p m e", e=16)
    fw.tt(V_, lr1, mexp3[:, :, 8], ang3[:, :, 8], ALU.mult, B1, B1)
    fw.ts(V_, lr1, lr1, -1.0, None, ALU.add, None, B1, B1)
    fw.tt(V_, li1, mexp3[:, :, 8], kf3[:, :, 8], ALU.mult, B1, B1)
    fw.tt(V_, nrm, are, are, ALU.mult, B1, B1)
    fw.tt(V_, ta, aim, aim, ALU.mult, B1, B1)
    fw.tt(V_, nrm, nrm, ta, ALU.add, B1, B1)
    fw.op(V_, lambda e: e.reciprocal(nrm, nrm), reads=B1, writes=B1)
    fw.tt(V_, ta, lr1, are, ALU.mult, B1, B1)
    fw.tt(V_, tb_, li1, aim, ALU.mult, B1, B1)
    fw.tt(V_, ta, ta, tb_, ALU.add, B1, B1)
    fw.tt(V_, kre, ta, nrm, ALU.mult, B1, B1)
    fw.tt(V_, ta, li1, are, ALU.mult, B1, B1)
    fw.tt(V_, tb_, lr1, aim, ALU.mult, B1, B1)
    fw.tt(V_, ta, ta, tb_, ALU.subtract, B1, B1)
    fw.tt(V_, kim, ta, nrm, ALU.mult, B1, B1)
    bn = [A.t32[:, ho + 2048:ho + 4096].rearrange("p (m x) -> p m x", x=16),
          A.t32[:, ho + 4096:ho + 6144].rearrange("p (m x) -> p m x", x=16)]
    for ri, nm in enumerate(("ssm_b_re", "ssm_b_im")):
        srcb = I[nm].rearrange("d g p x -> p (d g) x")
        for rep in range(2):
            for q8 in range(8):
                fw.dma(bn[ri][rep * 64:(rep + 1) * 64, q8 * 16:(q8 + 1) * 16, :], srcb[:, q8 * 16:(q8 + 1) * 16, :], writes=[bprm])
    t3 = ang3
    t4 = mexp3
    kreb = AP(kre, [[1, 128], [0, 16]])
    kimb = AP(kim, [[1, 128], [0, 16]])
    fw.tt(V_, t3, bn[0], kreb, ALU.mult, B1, B1)
    fw.tt(V_, t4, bn[1], kimb, ALU.mult, B1, B1)
    fw.tt(V_, t3, t3, t4, ALU.subtract, B1, B1)
    fw.copy(V_, bb[0][0:64], t3[0:64], B1, B1)
    fw.copy(V_, bb[1][64:128], t3[64:128], B1, B1)
    fw.tt(V_, t3, bn[1], kreb, ALU.mult, B1, B1)
    fw.tt(V_, t4, bn[0], kimb, ALU.mult, B1, B1)
    fw.tt(V_, t3, t3, t4, ALU.add, B1, B1)
    fw.copy(V_, bb[0][64:128], t3[64:128], B1, B1)
    fw.ts(V_, bb[1][0:64], t3[0:64], -1.0, None, ALU.mult, None, B1, B1)
    Cn = A.t32[:, ho:ho + 2048].rearrange("p (m c) -> p m c", m=16)
    for ri, nm in enumerate(("ssm_c_re", "ssm_c_im")):
        src = I[nm].rearrange("d g q p -> (d g q) p").rearrange("(m r) p -> r m p", r=128)
        for dup in range(2):
            for hm in range(2):
                fw.dma(Cn[:, hm * 8:(hm + 1) * 8, dup * 64:(dup + 1) * 64], src[:, hm * 8:(hm + 1) * 8, :], writes=[bprm])
        cv = A.t16[:, 2 * (ho + 4096 + 1024 * ri):2 * (ho + 4096 + 1024 * ri) + 2048]
        for mm_ in range(16):
            pb = mm_ % 4
            fw.tr(P.psum[pb][:, 0:128], Cn[:, mm_, :], ident, B1 + [P.bconst], [P.bps[pb]])
            fw.copy("scalar", cv[:, mm_ * 128:(mm_ + 1) * 128], P.psum[pb][:, 0:128], [P.bps[pb]], B1)
    cre_ = A.t16[:, 2 * (ho + 4096):2 * (ho + 4096) + 2048]
    cim_ = A.t16[:, 2 * (ho + 5120):2 * (ho + 5120) + 2048]
    cA, cB, cC = (ctm[i].rearrange("p m x -> p (m x)") for i in range(3))
    fw.copy(V_, cA[0:64], cre_[0:64], B1, B1)
    fw.ts(V_, cA[64:128], cim_[64:128], -1.0, None, ALU.mult, None, B1, B1)
    fw.ts(V_, cB[0:64], cim_[0:64], -1.0, None, ALU.mult, None, B1, B1)
    fw.ts(V_, cB[64:128], cre_[64:128], -1.0, None, ALU.mult, None, B1, B1)
    fw.ts(V_, cC[0:64], cre_[0:64], -1.0, None, ALU.mult, None, B1, B1)
    fw.copy(V_, cC[64:128], cre_[64:128], B1, B1)
    rows = A.f32(128)
    prm = A.f32(128)
    prm4 = prm.rearrange("p (a d s) -> p a d s", a=2, d=2)
    ldt = A.f32(64).rearrange("p (d s) -> p d s", d=2)
    for a_i, nm in enumerate(("ssm_a_re", "ssm_a_im")):
        for d in range(2):
            r0 = (a_i * 2 + d) * 32
            fw.dma(rows[r0:r0 + 32, :], I[nm][d].rearrange("g p -> (g p)").rearrange("(s q) -> s q", q=128), writes=[bprm])
    fw.tr(P.psum[4][:, 0:128], rows, ident, B1 + [P.bconst], [P.bps[4]])
    fw.copy(V_, prm, P.psum[4][:, 0:128], [P.bps[4]], B1)
    dt2 = A.f32(64).rearrange("p (d s) -> p d s", d=2)
    ldv = ldtb.rearrange("p (d s two) -> p d s two", d=2, two=2)
    for gl in range(2):
        fw.act(dt2[gl * 64:(gl + 1) * 64], ldv[gl * 64:(gl + 1) * 64, :, :, gl], AF.Exp, B1, B1)
    fw.tt(V_, r8, prm4[:, 0], dt2, ALU.mult, B1, B1)
    fw.act(r8, r8, AF.Exp, B1, B1, scale=8.0)
    fw.tt(V_, thp8, prm4[:, 1], dt2, ALU.mult, B1, B1)
    fw.ts(V_, thp8, thp8, 8.0 / TWO_PI, None, ALU.mult, None, B1, B1)
    dnat = A.f32(16)
    dT = A.f32(64)
    fw.dma(dnat[0:64, :], I["ssm_d"].rearrange("(g q) -> g q", q=16), writes=[bprm])
    fw.tr(P.psum[5][0:16, 0:64], dnat[0:64, :], ident[0:64, 0:64], B1 + [P.bconst], [P.bps[5]])
    fw.copy(V_, dT[0:16, :], P.psum[5][0:16, 0:64], [P.bps[5]], B1)
    for t_ in range(8):
        fw.dma(dvec[16 * t_:16 * t_ + 16, :], dT[0:16, :], reads=B1, writes=[bprm])
    for t_ in range(8):
        fw.memset(V_, Wsel[t_], 0.0, B1)
        fw.copy(V_, Wsel[t_][:, 112:128], identb[:, 16 * t_:16 * t_ + 16], B1 + [P.bconst], B1)
    rbi = A.i32(1); cbi = A.i32(128); rbf = A.f32(1); cbf = A.f32(128)
    fw.op("gpsimd", lambda e: e.iota(rbi, [[0, 1]], base=0, channel_multiplier=1), writes=B1)
    fw.op("gpsimd", lambda e: e.iota(cbi, [[1, 128]], base=0, channel_multiplier=0), writes=B1)
    fw.ts(V_, rbi, rbi, 4, None, ALU.arith_shift_right, None, B1, B1)
    fw.ts(V_, cbi, cbi, 4, None, ALU.arith_shift_right, None, B1, B1)
    fw.copy(V_, rbf, rbi, B1, B1)
    fw.copy(V_, cbf, cbi, B1, B1)
    fw.ts(V_, mask[0], cbf, rbf[:, 0:1], None, ALU.is_ge, None, B1, B1)
    fw.ts(V_, mask[1], cbf, rbf[:, 0:1], None, ALU.is_le, None, B1, B1)
    iop = dict(channel_multiplier=0, allow_small_or_imprecise_dtypes=True)
    fw.op("gpsimd", lambda e: e.iota(iotaC[0][:, 0:NL], [[1, NL]], base=32, **iop), writes=B1)
    fw.op("gpsimd", lambda e: e.iota(iotaC[0][:, NL:NCH], [[1, 32]], base=0, **iop), writes=B1)
    fw.op("gpsimd", lambda e: e.iota(iotaC[1][:, 0:NL], [[-1, NL]], base=NCH - 1, **iop), writes=B1)
    fw.op("gpsimd", lambda e: e.iota(iotaC[1][:, NL:NCH], [[-1, 32]], base=31, **iop), writes=B1)
    for bufl in (XLre, XLim, DLre, DLim):
        for s_ in range(2):
            for gl in range(2):
                fw.memset(V_, bufl[s_][gl], 0.0, [bMat[s_][gl]])
    fw.barrier()
    A.release(mW)
    LRP = [(A.f32(128), A.f32(128), A.f32(128)) for _ in range(2)]
    bLRP = [(Buf("Lm0"), Buf("Rm0"), Buf("Pm0")), (Buf("Lm1"), Buf("Rm1"), Buf("Pm1"))]
    c1 = A.f32(128); c2 = A.f32(128)
    bc12 = Buf("c12")
    TAU = A.f32(NCH); KI = A.i32(NCH); KF = A.f32(NCH); TA = A.f32(NCH); TB_ = A.f32(NCH)
    Vr = A.f32(NCH); Vi = A.f32(NCH); Gr = A.f32(NCH); Gi = A.f32(NCH)
    Sp = [A.bf16(NL), A.bf16(NL)]
    gtmp = A.f32(NL); gt2 = A.f32(NL)
    bTAB, bKI, bSIN, bTA, bTBb, bVr, bVi, bGr, bGi = (Buf(n) for n in ("tab", "ki", "sin", "ta", "tb", "vr", "vi", "gr", "gi"))
    bSp = [Buf("spre"), Buf("spim")]
    bgt = Buf("gtmp")
    COS, SIN = TAU, KF
    gen_i = [0]
    out_i = [0]

    c3 = A.f32(128); c4 = A.f32(128)
    bc34 = Buf("c34")
    TOEP = globals().get("S5_TOEP", True)
    Esh = c3.tensor[:, 0:1]
    if TOEP:
        cbase = A.last_off - 128
        Esh = A.t16[:, 2 * cbase:2 * cbase + 352]
        cst32 = A.t32[:, cbase + 176:cbase + 192]
        kst = [A.t16[:, 2 * (cbase + 192):2 * (cbase + 192) + 16], A.t16[:, 2 * (cbase + 200):2 * (cbase + 200) + 16]]
        bE = Buf("Esh"); bcst = Buf("cst32"); bkst = [Buf("kst0"), Buf("kst1")]
        fw.memset(V_, Esh, 0.0, [bE])
        fw.copy(V_, Esh[:, 112:240], identb, [P.bconst], [bE])

    def cmul(eng, out, rows, mg, tab, e0, es, VA, VB):
        r0, r1 = rows
        pr_ = pw[0][r0:r1, mg, :]
        pi_ = pw[1][r0:r1, mg, :]
        va_ = VA[r0:r1, mg, :]
        vb_ = VB[r0:r1, mg, :]
        pe = lambda v: AP(v, [[es, 8], [0, 16]], off=e0)
        vb = lambda v: AP(v, [[0, 8], [1, 16]])
        o3 = out[r0:r1, :].rearrange("p (i x) -> p i x", x=16)
        s1, s2, bs_ = (c1, c2, bc12) if eng == "vector" else (LRP[0][0], LRP[0][1], bc34)
        a1 = s1[r0:r1, :].rearrange("p (i x) -> p i x", x=16)
        a2 = s2[r0:r1, :].rearrange("p (i x) -> p i x", x=16)
        fw.tt(eng, a1, pe(pr_), vb(va_), ALU.mult, B1, [bs_])
        fw.tt(eng, a2, pe(pi_), vb(vb_), ALU.mult, B1, [bs_])
        fw.tt(eng, o3, a1, a2, ALU.add, [bs_], tab)

    def gen(it):
        gp, d = it // 2, it % 2
        st_ = it % 2
        g0 = 2 * gp
        eL = (7, -1) if d == 0 else (0, 1)
        eR = (7, 1) if d == 0 else (14, -1)
        eD = (8, 1) if d == 0 else (15, -1)
        eP = (14, -1) if d == 0 else (7, 1)
        for gl in range(2):
            g = g0 + gl
            mg = d * 64 + g
            q_ = gen_i[0] % 2
            Lm_, Rm_, Pm_ = LRP[q_]
            bL_, bR_, bP_ = bLRP[q_]
            ALLR = (0, 128)
            if not TOEP:
                cmul(V_, Lm_, ALLR, mg, [bL_], eL[0], eL[1], bb[0], bb[1])
                cmul(V_, Rm_, ALLR, mg, [bR_], eR[0], eR[1], ctm[0], ctm[1])
            cmul(V_, Pm_, ALLR, mg, [bP_], eP[0], eP[1], bb[0], bb[1])
            rws = (gl * 64, gl * 64 + 64)
            bm = bMat[st_][gl]
            DE = "gpsimd" if globals().get("S5_POOL_D", True) else V_
            if gl == 0:
                cmul(DE, DLre[st_][gl], rws, mg, [bm], eD[0], eD[1], ctm[0], ctm[1])
                cmul(DE, DLim[st_][gl], rws, mg, [bm], eD[0], eD[1], ctm[1], ctm[2])
            else:
                cmul(DE, DLre[st_][gl], rws, mg, [bm], eD[0], eD[1], ctm[2], ctm[0])
                cmul(DE, DLim[st_][gl], rws, mg, [bm], eD[0], eD[1], ctm[0], ctm[1])
            pbm = 4 + gen_i[0] % 2
            gen_i[0] += 1
            if not TOEP:
                fw.mm(P.psum[pbm][:, 0:128], Lm_, Rm_, True, True, [bL_, bR_], [P.bps[pbm]])
                fw.tr(P.psum[pbm][:, 128:256], Pm_, ident, [bP_, P.bconst], [P.bps[pbm]])
                fw.tt(V_, Mg[st_][gl], P.psum[pbm][:, 0:128], mask[d], ALU.mult, [P.bps[pbm]] + B1, [bm])
            else:
                kq = gen_i[0] % 2
                fw.act(cst32, ctm[0][:, mg, :], AF.Identity, B1, [bcst])
                fw.mm(P.psum[pbm][:, 256:272], Pm_, cst32, True, True, [bP_, bcst], [P.bps[pbm]])
                fw.tr(P.psum[pbm][:, 128:256], Pm_, ident, [bP_, P.bconst], [P.bps[pbm]])
                fw.act(kst[kq], P.psum[pbm][:, 256:272], AF.Identity, [P.bps[pbm]], [bkst[kq]])
                for t_ in range(8):
                    off = (112 + 16 * (7 - t_)) if d == 0 else (112 - 16 * t_)
                    fw.mm(P.psum[pbm][:, t_ * 16:(t_ + 1) * 16], Esh[:, off:off + 128], kst[kq], True, True,
                          [bE, bkst[kq]], [P.bps[pbm]])
                fw.act(Mg[st_][gl], P.psum[pbm][:, 0:128], AF.Identity, [P.bps[pbm]], [bm])
            fw.copy("scalar", XLre[st_][gl][:, gl * 64:gl * 64 + 64], P.psum[pbm][:, 128:192], [P.bps[pbm]], [bm])
            fw.copy("scalar", XLim[st_][gl][:, gl * 64:gl * 64 + 64], P.psum[pbm][:, 192:256], [P.bps[pbm]], [bm])

    def xmm(it):
        gp, d = it // 2, it % 2
        st_ = it % 2
        g0 = 2 * gp
        for gl in range(2):
            fw.mm(P.psum[2][:, 0:NCH], XLre[st_][gl], U8[:, g0 + gl, :], gl == 0, gl == 1,
                  [bMat[st_][gl], bU8[g0 + gl]], [P.bps[2]])
        for gl in range(2):
            fw.mm(P.psum[3][:, 0:NCH], XLim[st_][gl], U8[:, g0 + gl, :], gl == 0, gl == 1,
                  [bMat[st_][gl], bU8[g0 + gl]], [P.bps[3]])

    def scan(it):
        gp, d = it // 2, it % 2
        thc = thp8[:, d, gp:gp + 1]
        rcol = r8[:, d, gp:gp + 1]
        fw.ts(V_, TAU, iotaC[d], thc, None, ALU.mult, None, B1, [bTAB])
        fw.copy(V_, KI, TAU, [bTAB], [bKI])
        fw.copy(V_, KF, KI, [bKI], [bSIN])
        fw.tt(V_, TAU, TAU, KF, ALU.subtract, [bSIN], [bTAB])
        fw.act(SIN, TAU, AF.Sin, [bTAB], [bSIN], scale=TWO_PI_S)
        fw.act(TAU, TAU, AF.Abs, [], [bTAB])
        fw.act(COS, TAU, AF.Sin, [], [bTAB], scale=-TWO_PI, bias=P.halfpi[:, 0:1])
        xr, xi = P.psum[2][:, 0:NCH], P.psum[3][:, 0:NCH]
        fw.tt(V_, TA, xr, COS, ALU.mult, [P.bps[2], bTAB], [bTA])
        fw.tt(V_, TB_, xi, SIN, ALU.mult, [P.bps[3], bSIN], [bTBb])
        fw.tt(V_, Vr, TA, TB_, ALU.add, [bTA, bTBb], [bVr])
        fw.tt(V_, TA, xi, COS, ALU.mult, [P.bps[3], bTAB], [bTA])
        fw.tt(V_, TB_, xr, SIN, ALU.mult, [P.bps[2], bSIN], [bTBb])
        fw.tt(V_, Vi, TA, TB_, ALU.subtract, [bTA, bTBb], [bVi])
        for (Vx, bVx, Gx, bGx) in ((Vr, bVr, Gr, bGr), (Vi, bVi, Gi, bGi)):
            segA = (lambda v: v[:, NL:NCH]) if d == 0 else (lambda v: rev(v[:, NL:NCH]))
            segB = (lambda v: v[:, 0:NL]) if d == 0 else (lambda v: rev(v[:, 0:NL]))
            lastA = NCH - 1 if d == 0 else NL
            rbA = AP(rcol, [[0, 32]])
            rbB = AP(rcol, [[0, NL]])
            fw.op(V_, lambda e, o=segA(Gx), r_=rbA, v=segA(Vx): e.tensor_tensor_scan(o, r_, v, 0.0, ALU.mult, ALU.add),
                  reads=[bVx] + B1, writes=[bGx])
            fw.op(V_, lambda e, o=segB(Gx), r_=rbB, v=segB(Vx), i_=Gx[:, lastA:lastA + 1]:
                  e.tensor_tensor_scan(o, r_, v, i_, ALU.mult, ALU.add), reads=[bVx] + B1, writes=[bGx])
        if d == 0:
            pieces = ((slice(1, NL), slice(0, NL - 1)), (slice(0, 1), slice(NCH - 1, NCH)))
        else:
            pieces = ((slice(0, NL - 1), slice(1, NL)), (slice(NL - 1, NL), slice(NL, NL + 1)))
        fw.tt(V_, TA, Gr, COS, ALU.mult, [bGr, bTAB], [bTA])
        fw.tt(V_, TB_, Gi, SIN, ALU.mult, [bGi, bSIN], [bTBb])
        for (do, so) in pieces:
            fw.tt(V_, Sp[0][:, do], TA[:, so], TB_[:, so], ALU.subtract, [bTA, bTBb], [bSp[0]])
        fw.tt(V_, TA, Gr, SIN, ALU.mult, [bGr, bSIN], [bTA])
        fw.tt(V_, TB_, Gi, COS, ALU.mult, [bGi, bTAB], [bTBb])
        for (do, so) in pieces:
            fw.tt(V_, Sp[1][:, do], TA[:, so], TB_[:, so], ALU.add, [bTA, bTBb], [bSp[1]])

    def ymm(it):
        gp, d = it // 2, it % 2
        st_ = it % 2
        g0 = 2 * gp
        for gl in range(2):
            g = g0 + gl
            bm = bMat[st_][gl]
            fw.mm(P.psum[gl][:, 0:NL], Mg[st_][gl], U8[:, g, 0:NL], d == 0, False, [bm, bU8[g]], [P.bps[gl]])
            fw.mm(P.psum[gl][:, 0:NL], DLre[st_][gl], Sp[0], False, False, [bm, bSp[0]], [P.bps[gl]])
            fw.mm(P.psum[gl][:, 0:NL], DLim[st_][gl], Sp[1], False, d == 1, [bm, bSp[1]], [P.bps[gl]])

    def epi(gp):
        g0 = 2 * gp
        oc = gp // 4
        for gl in range(2):
            g = g0 + gl
            fw.stt(gtmp, U8[:, g, 0:NL], dvec[:, g:g + 1], P.psum[gl][:, 0:NL], ALU.mult, ALU.add,
                   [P.bps[gl], bU8[g]] + B1, [bgt])
            fw.act(gt2, gtmp, AF.Square, [bgt], [bgt])
            fw.ts(V_, gt2, gt2, 0.044715, 1.0, ALU.mult, ALU.add, [bgt], [bgt])
            fw.tt(V_, gt2, gt2, gtmp, ALU.mult, [bgt], [bgt])
            fw.act(gt2, gt2, AF.Sigmoid, [bgt], [bgt], scale=2.0 * math.sqrt(2.0 / math.pi))
            fw.tt(V_, ybf[:, g % 8, :], gt2, gtmp, ALU.mult, [bgt], [bybf[g % 8]])
        if gp % 4 == 3:
            for t_ in range(8):
                pbo = 6 + out_i[0] % 2
                out_i[0] += 1
                for gg in range(8):
                    fw.mm(P.psum[pbo][:, 0:NL], Wsel[t_][:, 112 - 16 * gg:240 - 16 * gg], ybf[:, gg, :], gg == 0, gg == 7,
                          [bybf[gg]] + B1, [P.bps[pbo]])
                fw.copy("scalar", hT[:, oc, t_:L - 7 + t_:8], P.psum[pbo][:, 0:NL], [P.bps[pbo]],
                        [P.bh[0], P.bh[1], P.bh[2], P.bh[3]])

    def record(fn, *a):
        calls = []
        real = fw.op
        fw.op = lambda *aa, **kk: calls.append((aa, kk))
        try:
            fn(*a)
        finally:
            fw.op = real
        return calls

    def replay_interleaved(ca, cb):
        na, nb = len(ca), len(cb)
        ia = ib = 0
        while ia < na or ib < nb:
            if ib >= nb or (ia < na and ia * max(nb, 1) <= ib * max(na, 1)):
                aa, kk = ca[ia]; ia += 1
            else:
                aa, kk = cb[ib]; ib += 1
            fw.op(*aa, **kk)

    def merge(ca, cb):
        out = []
        na, nb = len(ca), len(cb)
        ia = ib = 0
        while ia < na or ib < nb:
            if ib >= nb or (ia < na and ia * max(nb, 1) <= ib * max(na, 1)):
                out.append(ca[ia]); ia += 1
            else:
                out.append(cb[ib]); ib += 1
        return out

    NIT = 64
    gen(0)
    pend_epi = []
    for it in range(NIT):
        xmm(it)
        cg = record(gen, it + 1) if it + 1 < NIT else []
        cs = record(scan, it)
        if globals().get("S5_INTERLEAVE", True):
            for aa, kk in merge(merge(cs, cg), pend_epi):
                fw.op(*aa, **kk)
        else:
            for aa, kk in pend_epi + cg + cs:
                fw.op(*aa, **kk)
        pend_epi = []
        ymm(it)
        if it % 2 == 1:
            if it + 1 < NIT and globals().get("S5_EPI_DEFER", True):
                pend_epi = record(epi, it // 2)
            else:
                epi(it // 2)
    fw.barrier()


def layer1(P):
    fw, A, I = P.fw, P.A, P.I
    l = 1
    PCE = "gpsimd" if globals().get("USE_POOL", False) else "vector"
    if not hasattr(P, "epsc"):
        P.epsc = A.f32(1)
        fw.memset("vector", P.epsc, EPS, [P.bconst])
    if P.xT is None:
        P.xT = A.f32(KC * T).rearrange("p (c t) -> p c t", c=KC)
        P.bx = [Buf(f"xT{i}") for i in range(5)]
        m_ = A.mark()
        P.xin = [A.f32(D), A.f32(D)]
        P.bxin = [Buf("xin0"), Buf("xin1")]
        P.xin_i = 0
        P.tps = [4, 5]
        P.tps_i = 0
        for tb in range(5):
            t0, tl = TBS[tb]
            load_xT_block(P, tb, P.xT[:, :, t0:t0 + tl], P.bx[tb])
        fw.barrier()
        A.release(m_)
    xT = P.xT
    mX = A.mark()
    hT = P.hT
    scr = norm_scratch(P)
    for tb in range(5):
        t0, tl = TBS[tb]
        norm_mod_block(P, l, 0, tb, xT[:, :, t0:t0 + tl], P.bx[tb], scr)
    fw.barrier()
    A.release(mX)
    if globals().get("S5_MODE", "chunk") == "chunk":
        s5_chunked(P)
        A.release(mX)
        if P.tap == "y1":
            tap_bf16(P, hT.rearrange("p c t -> p (c t)"), KC * T, P.bh)
            return True
        return layer1_tail(P, mX)
    wi = [A.bf16(8 * 512).rearrange("p (k n) -> p k n", k=8) for _ in range(2)]
    bwi = [Buf("wi0"), Buf("wi1")]
    wiv = I["ssm_w_in"].rearrange("(k p) n -> p k n", p=128)
    for i in range(2):
        fw.dma(wi[i], wiv[:, :, i * 512:(i + 1) * 512], writes=[bwi[i]], q="gpsimd")
    for tb in range(5):
        t0, tl = TBS[tb]
        for m in range(8):
            for k in range(KC):
                fw.mm(P.psum[m][:, 0:tl], wi[m // 4][:, k, (m % 4) * 128:(m % 4 + 1) * 128], hT[:, k, t0:t0 + tl],
                      k == 0, k == KC - 1, [bwi[m // 4], P.bh[tb]], [P.bps[m]])
        for m in range(8):
            fw.copy("scalar" if m % 2 else "vector", hT[:, m, t0:t0 + tl], P.psum[m][:, 0:tl], [P.bps[m]], [P.bh[tb]])
    fw.barrier()
    A.release(mX)
    uT = hT
    TWO_PI = 2.0 * math.pi
    TWO_PI_S = 6.2831845
    bprm = Buf("s5prm")
    prm = A.f32(128)
    prm4 = prm.rearrange("p (a d s) -> p a d s", a=2, d=2)
    ldt = A.f32(64).rearrange("p (d s) -> p d s", d=2)
    thp = A.f32(64).rearrange("p (d s) -> p d s", d=2)
    th2 = A.f32(64).rearrange("p (d s) -> p d s", d=2)
    rr_ = A.f32(64).rearrange("p (d s) -> p d s", d=2)
    kap = A.f32(128).rearrange("p (a d s) -> p a d s", a=2, d=2)
    dcol = A.f32(8)
    Bl = [A.bf16(16 * 128).rearrange("p (m c) -> p m c", m=16) for _ in range(2)]
    LT = [A.bf16(16 * 128).rearrange("p (m c) -> p m c", m=16) for _ in range(2)]
    LTm = [[A.bf16(128) for _ in range(4)] for _ in range(2)]
    bLTm = [[Buf(f"LTm{ri}{pr}") for pr in range(4)] for ri in range(2)]
    BlZ = [[A.bf16(128) for _ in range(4)] for _ in range(2)]
    bBlZ = [[Buf(f"BlZ{ri}{pr}") for pr in range(4)] for ri in range(2)]
    JH = 1152
    iotaJ = A.f32(JH)
    st2 = A.f32(2)
    mS = A.mark()
    rows = A.f32(128)
    for a_i, nm in enumerate(("ssm_a_re", "ssm_a_im")):
        for d in range(2):
            r0 = (a_i * 2 + d) * 32
            fw.dma(rows[r0:r0 + 32, :], I[nm][d].rearrange("g p -> (g p)").rearrange("(s q) -> s q", q=128), writes=[bprm])
    fw.tr(P.psum[0][:, 0:128], rows, P.ident, [bprm, P.bconst], [P.bps[0]])
    fw.copy("vector", prm, P.psum[0][:, 0:128], [P.bps[0]], [bprm])
    ld_t = I["ssm_log_dt"].tensor
    for gl in range(2):
        fw.dma(ldt[gl * 64:(gl + 1) * 64, :, :], bass.AP(ld_t, gl, [[0, 64], [64, 2], [2, 32]]), writes=[bprm], **SLOW)
    fw.dma(dcol, I["ssm_d"].rearrange("(c p) -> p c", p=128), writes=[bprm], **SLOW)
    fw.op("gpsimd", lambda e: e.iota(iotaJ, [[1, JH]], base=0, channel_multiplier=0,
                                     allow_small_or_imprecise_dtypes=True), writes=[bprm])
    dt_ = A.f32(64).rearrange("p (d s) -> p d s", d=2)
    xre = A.f32(64).rearrange("p (d s) -> p d s", d=2)
    th = A.f32(64).rearrange("p (d s) -> p d s", d=2)
    ki = A.i32(64).rearrange("p (d s) -> p d s", d=2)
    kf = A.f32(64).rearrange("p (d s) -> p d s", d=2)
    sn = A.f32(64).rearrange("p (d s) -> p d s", d=2)
    cs_ = A.f32(64).rearrange("p (d s) -> p d s", d=2)
    lr = A.f32(64).rearrange("p (d s) -> p d s", d=2)
    li = A.f32(64).rearrange("p (d s) -> p d s", d=2)
    t_a = A.f32(64).rearrange("p (d s) -> p d s", d=2)
    t_b = A.f32(64).rearrange("p (d s) -> p d s", d=2)
    nrm = A.f32(64).rearrange("p (d s) -> p d s", d=2)
    V_ = "vector"
    B1 = [bprm]
    fw.act(dt_, ldt, AF.Exp, B1, B1)
    fw.tt(V_, xre, prm4[:, 0], dt_, ALU.mult, B1, B1)
    fw.tt(V_, th, prm4[:, 1], dt_, ALU.mult, B1, B1)
    fw.act(rr_, xre, AF.Exp, B1, B1)
    fw.ts(V_, thp, th, 1.0 / TWO_PI, None, ALU.mult, None, B1, B1)
    fw.ts(V_, th2, thp, float(JH), None, ALU.mult, None, B1, B1)
    fw.copy(V_, ki, thp, B1, B1)
    fw.copy(V_, kf, ki, B1, B1)
    fw.tt(V_, kf, thp, kf, ALU.subtract, B1, B1)
    fw.act(sn, kf, AF.Sin, B1, B1, scale=TWO_PI_S)
    fw.act(kf, kf, AF.Abs, B1, B1)
    fw.act(cs_, kf, AF.Sin, B1, B1, scale=-TWO_PI, bias=P.halfpi[:, 0:1])
    fw.tt(V_, lr, rr_, cs_, ALU.mult, B1, B1)
    fw.tt(V_, li, rr_, sn, ALU.mult, B1, B1)
    fw.ts(V_, lr, lr, -1.0, None, ALU.add, None, B1, B1)
    fw.tt(V_, nrm, prm4[:, 0], prm4[:, 0], ALU.mult, B1, B1)
    fw.tt(V_, t_a, prm4[:, 1], prm4[:, 1], ALU.mult, B1, B1)
    fw.tt(V_, nrm, nrm, t_a, ALU.add, B1, B1)
    fw.op(V_, lambda e: e.reciprocal(nrm, nrm), reads=B1, writes=B1)
    fw.tt(V_, t_a, lr, prm4[:, 0], ALU.mult, B1, B1)
    fw.tt(V_, t_b, li, prm4[:, 1], ALU.mult, B1, B1)
    fw.tt(V_, t_a, t_a, t_b, ALU.add, B1, B1)
    fw.tt(V_, kap[:, 0], t_a, nrm, ALU.mult, B1, B1)
    fw.tt(V_, t_a, li, prm4[:, 0], ALU.mult, B1, B1)
    fw.tt(V_, t_b, lr, prm4[:, 1], ALU.mult, B1, B1)
    fw.tt(V_, t_a, t_a, t_b, ALU.subtract, B1, B1)
    fw.tt(V_, kap[:, 1], t_a, nrm, ALU.mult, B1, B1)
    bn = [A.f32(64 * 16).rearrange("p (m x) -> p m x", m=64) for _ in range(2)]
    for ri, nm in enumerate(("ssm_b_re", "ssm_b_im")):
        fw.dma(bn[ri], I[nm].rearrange("d g p x -> (d g p) x").rearrange("(m q) x -> q m x", q=128), writes=[bprm])
    kap_off = A.last_off
    Nn = [A.f32(64 * 16).rearrange("p (m x) -> p m x", m=64) for _ in range(2)]
    tN = A.f32(64 * 16).rearrange("p (m x) -> p m x", m=64)

    def kb(a):
        v = kap[:, a].rearrange("p d s -> p (d s)")
        dims = [list(d_) for d_ in v.ap]
        return bass.AP(v.tensor, v.offset, dims + [[0, 16]])
    fw.tt(V_, Nn[0], bn[0], kb(0), ALU.mult, B1, B1)
    fw.tt(V_, tN, bn[1], kb(1), ALU.mult, B1, B1)
    fw.tt(V_, Nn[0], Nn[0], tN, ALU.subtract, B1, B1)
    fw.tt(V_, Nn[1], bn[1], kb(0), ALU.mult, B1, B1)
    fw.tt(V_, tN, bn[0], kb(1), ALU.mult, B1, B1)
    fw.tt(V_, Nn[1], Nn[1], tN, ALU.add, B1, B1)
    Zb = A.f32(64 * 32).rearrange("p (m x) -> p m x", m=64)
    for ri in range(2):
        fw.memset(V_, Zb, 0.0, B1)
        fw.copy(V_, Zb[0:64, :, 0:16], Nn[ri][0:64, :, :], B1, B1)
        fw.copy(V_, Zb[64:128, :, 16:32], Nn[ri][64:128, :, :], B1, B1)
        Zf = Zb.rearrange("p m x -> p (m x)")
        for mm_ in range(16):
            pb = mm_ % 4
            fw.tr(P.psum[pb][:, 0:128], Zf[:, mm_ * 128:(mm_ + 1) * 128], P.ident, B1 + [P.bconst], [P.bps[pb]])
            fw.copy("scalar", Bl[ri][:, mm_, :], P.psum[pb][:, 0:128], [P.bps[pb]], B1)
    msk = A.f32(128)
    fw.memset(V_, msk, 0.0, B1)
    for gg in range(8):
        h_ = gg % 2
        fw.memset(V_, msk[h_ * 64:(h_ + 1) * 64, gg * 16:(gg + 1) * 16], 1.0, B1)
    Cn = A.f32(16 * 128).rearrange("p (m c) -> p m c", m=16)
    for ri, nm in enumerate(("ssm_c_re", "ssm_c_im")):
        src = I[nm].rearrange("d g q p -> (d g q) p").rearrange("(m r) p -> r m p", r=128)
        for dup in range(2):
            fw.dma(Cn[:, :, dup * 64:(dup + 1) * 64], src, writes=[bprm])
        for mm_ in range(16):
            pb = mm_ % 4
            fw.tr(P.psum[pb][:, 0:128], Cn[:, mm_, :], P.ident, B1 + [P.bconst], [P.bps[pb]])
            if ri == 0:
                fw.tt(V_, LT[ri][:, mm_, :], P.psum[pb][:, 0:128], msk, ALU.mult, [P.bps[pb]] + B1, B1)
            else:
                fw.stt(LT[ri][:, mm_, :], P.psum[pb][:, 0:128], -1.0, msk, ALU.mult, ALU.mult, [P.bps[pb]] + B1, B1)
    for ri in range(2):
        for pr in range(4):
            fw.memset(V_, LTm[ri][pr], 0.0, [bLTm[ri][pr]])
            fw.memset(V_, BlZ[ri][pr], 0.0, [bBlZ[ri][pr]])
    if P.tap == "s5lt":
        for ri in range(2):
            fw.copy(V_, Zb.rearrange("p m x -> p (m x)"), LT[ri].rearrange("p m c -> p (m c)"), B1, B1)
            fw.dma(P.dbg[:, ri * 2048:(ri + 1) * 2048], Zb.rearrange("p m x -> p (m x)"), reads=B1, writes=[P.bdbg])
            fw.copy(V_, Zb.rearrange("p m x -> p (m x)"), Bl[ri].rearrange("p m c -> p (m c)"), B1, B1)
            fw.dma(P.dbg[:, 4096 + ri * 2048:4096 + (ri + 1) * 2048], Zb.rearrange("p m x -> p (m x)"), reads=B1, writes=[P.bdbg])
        return True
    fw.barrier()
    A.release(mS)
    NB = JH
    TAU = A.f32(NB); KI = A.i32(NB); KF = A.f32(NB)
    TA = A.f32(NB); TB_ = A.f32(NB)
    Vr = A.f32(NB); Vi = A.f32(NB); Gr = A.f32(NB); Gi = A.f32(NB)
    Hh = [A.bf16(L), A.bf16(L)]
    gtmp = A.f32(512); gt2 = A.f32(512)
    yacc = A.f32(L)
    byacc = [Buf(f"yacc{i}") for i in range(4)]
    bTAB, bKI, bSIN, bTA, bTBb, bVr, bVi, bGr, bGi = (Buf(n) for n in ("tab", "ki", "sin", "ta", "tb", "vr", "vi", "gr", "gi"))
    bH = [Buf("Hre"), Buf("Him")]
    bst2 = Buf("st2")
    bgt = Buf("gtmp")
    COS, SIN = TAU, KF

    def segs(d, hf):
        out = []
        j0 = hf * JH
        if d == 0:
            whole = [(L, NCTX, False, False)] + [(0, L, False, True)]
        else:
            whole = [(L, NCTX, True, False)] + [(0, L, True, True)]
        pos = 0
        for (a, n, rv, lat) in whole:
            lo, hi = max(pos, j0), min(pos + n, j0 + JH)
            if lo < hi:
                cnt = hi - lo
                off = lo - pos
                while cnt > 0:
                    c_ = min(512, cnt)
                    if not rv:
                        out.append((a + off, c_, lo - j0, False, lat))
                    else:
                        out.append((a + n - off - c_, c_, lo - j0, True, lat))
                    off += c_
                    lo += c_
                    cnt -= c_
            pos += n
        return out

    bui = 0
    for oc in range(8):
        for d in range(2):
            for pr in range(4):
                st = oc * 4 + pr
                m16 = d * 8 + oc
                for ri in range(2):
                    if globals().get("SKIPCP", False) and (oc, d, pr) != (0, 0, 0):
                        continue
                    fw.copy(PCE, LTm[ri][pr][:, pr * 32:(pr + 1) * 32], LT[ri][:, m16, pr * 32:(pr + 1) * 32],
                            [bprm], [bLTm[ri][pr]])
                    fw.copy(PCE, BlZ[ri][pr][pr * 32:(pr + 1) * 32, :], Bl[ri][pr * 32:(pr + 1) * 32, m16, :],
                            [bprm], [bBlZ[ri][pr]])
                if P.tap == "s5dbg5" and (oc, d, pr) == P.dbgsel[:3]:
                    for ri in range(2):
                        for p4 in range(4):
                            fw.copy(V_, gtmp[:, 0:128], LTm[ri][p4], [bLTm[ri][p4]], [bgt])
                            fw.dma(P.dbg[:, (ri * 4 + p4) * 128:(ri * 4 + p4 + 1) * 128], gtmp[:, 0:128], reads=[bgt], writes=[P.bdbg])
                    return True
                thc = thp[:, d, st:st + 1]
                rcol = rr_[:, d, st:st + 1]
                rb = bass.AP(rcol.tensor, rcol.offset, [list(rcol.ap[0]), [0, NB]])
                for hf in range(2):
                    if globals().get("S5BAR2", False):
                        fw.barrier()
                    if hf == 0:
                        fw.ts(V_, TAU, iotaJ, thc, None, ALU.mult, None, [bprm], [bTAB])
                    else:
                        fw.ts(V_, TAU, iotaJ, thc, th2[:, d, st:st + 1], ALU.mult, ALU.add, [bprm], [bTAB])
                    fw.copy(V_, KI, TAU, [bTAB], [bKI])
                    fw.copy(V_, KF, KI, [bKI], [bSIN])
                    fw.tt(V_, TAU, TAU, KF, ALU.subtract, [bSIN], [bTAB])
                    fw.act(SIN, TAU, AF.Sin, [bTAB], [bSIN], scale=TWO_PI_S)
                    fw.act(TAU, TAU, AF.Abs, [], [bTAB])
                    fw.act(COS, TAU, AF.Sin, [], [bTAB], scale=-TWO_PI, bias=P.halfpi[:, 0:1])
                    sg_ = segs(d, hf)
                    for (a, n, jl, rv, lat) in sg_:
                        pre, pim = 4 + (bui % 2) * 2, 5 + (bui % 2) * 2
                        bui += 1
                        tb = min(a // 512, 4)
                        tb2 = min((a + n - 1) // 512, 4)
                        rd = [P.bh[tb]] + ([P.bh[tb2]] if tb2 != tb else [])
                        for ri, pb in ((0, pre), (1, pim)):
                            fw.mm(P.psum[pb][:, 0:n], BlZ[ri][pr], uT[:, oc, a:a + n], True, True,
                                  rd + [bBlZ[ri][pr]], [P.bps[pb]])
                        bre = P.psum[pre][:, 0:n]
                        bim = P.psum[pim][:, 0:n]
                        if rv:
                            bre, bim = rev(bre), rev(bim)
                        cs_s, sn_s = COS[:, jl:jl + n], SIN[:, jl:jl + n]
                        fw.tt(V_, TA[:, jl:jl + n], bre, cs_s, ALU.mult, [P.bps[pre], bTAB], [bTA])
                        fw.tt(V_, TB_[:, jl:jl + n], bim, sn_s, ALU.mult, [P.bps[pim], bSIN], [bTBb])
                        fw.tt(PCE, Vr[:, jl:jl + n], TA[:, jl:jl + n], TB_[:, jl:jl + n], ALU.add, [bTA, bTBb], [bVr])
                        fw.tt(V_, TA[:, jl:jl + n], bim, cs_s, ALU.mult, [P.bps[pim], bTAB], [bTA])
                        fw.tt(V_, TB_[:, jl:jl + n], bre, sn_s, ALU.mult, [P.bps[pre], bSIN], [bTBb])
                        fw.tt(PCE, Vi[:, jl:jl + n], TA[:, jl:jl + n], TB_[:, jl:jl + n], ALU.subtract, [bTA, bTBb], [bVi])
                    for (Vx, bVx, Gx, bGx, si) in ((Vr, bVr, Gr, bGr, 0), (Vi, bVi, Gi, bGi, 1)):
                        init = 0.0 if hf == 0 else st2[:, si:si + 1]
                        fw.op(V_, lambda e, Gx=Gx, Vx=Vx, init=init, rb=rb: e.tensor_tensor_scan(Gx, rb, Vx, init, ALU.mult, ALU.add),
                              reads=[bVx, bprm, bst2], writes=[bGx])
                        if hf == 0:
                            fw.copy(V_, st2[:, si:si + 1], Gx[:, NB - 1:NB], [bGx], [bst2])
                    if P.tap == "s5dbg" and (oc, d, pr, hf) == P.dbgsel:
                        for i_, (ap_, b_) in enumerate(((COS, bTAB), (SIN, bSIN), (Vr, bVr), (Vi, bVi), (Gr, bGr), (Gi, bGi))):
                            fw.dma(P.dbg[:, i_ * NB:(i_ + 1) * NB], ap_, reads=[b_], writes=[P.bdbg])
                        return True
                    for (a, n, jl, rv, lat) in sg_:
                        if not lat:
                            continue
                        cs_s, sn_s = COS[:, jl:jl + n], SIN[:, jl:jl + n]
                        hr = Hh[0][:, a:a + n]
                        hi_ = Hh[1][:, a:a + n]
                        if rv:
                            hr, hi_ = rev(hr), rev(hi_)
                        fw.tt(PCE, TA[:, jl:jl + n], Gr[:, jl:jl + n], cs_s, ALU.mult, [bGr, bTAB], [bTA])
                        fw.tt(PCE, TB_[:, jl:jl + n], Gi[:, jl:jl + n], sn_s, ALU.mult, [bGi, bSIN], [bTBb])
                        fw.tt(V_, hr, TA[:, jl:jl + n], TB_[:, jl:jl + n], ALU.subtract, [bTA, bTBb], [bH[0]])
                        fw.tt(PCE, TA[:, jl:jl + n], Gr[:, jl:jl + n], sn_s, ALU.mult, [bGr, bSIN], [bTA])
                        fw.tt(PCE, TB_[:, jl:jl + n], Gi[:, jl:jl + n], cs_s, ALU.mult, [bGi, bTAB], [bTBb])
                        fw.tt(V_, hi_, TA[:, jl:jl + n], TB_[:, jl:jl + n], ALU.add, [bTA, bTBb], [bH[1]])
                if P.tap == "s5dbg2" and (oc, d, pr) == P.dbgsel[:3]:
                    for i_ in range(2):
                        fw.copy(V_, Vr[:, 0:1024], Hh[i_][:, 0:1024], [bH[i_]], [bVr])
                        fw.dma(P.dbg[:, i_ * 2048:i_ * 2048 + 1024], Vr[:, 0:1024], reads=[bVr], writes=[P.bdbg])
                        fw.copy(V_, Vi[:, 0:1024], Hh[i_][:, 1024:2048], [bH[i_]], [bVi])
                        fw.dma(P.dbg[:, i_ * 2048 + 1024:i_ * 2048 + 2048], Vi[:, 0:1024], reads=[bVi], writes=[P.bdbg])
                    return True
                if globals().get("S5BAR", False):
                    fw.barrier()
                for tb in range(4):
                    for ri in range(2):
                        fw.mm(P.psum[tb][:, :], LTm[ri][pr], Hh[ri][:, tb * 512:(tb + 1) * 512], ri == 0, ri == 1,
                              [bLTm[ri][pr], bH[ri]], [P.bps[tb]])
                    ysl = yacc[:, tb * 512:(tb + 1) * 512]
                    if d == 0 and pr == 0:
                        fw.copy("scalar", ysl, P.psum[tb][:, :], [P.bps[tb]], [byacc[tb]])
                    else:
                        fw.tt(V_, ysl, P.psum[tb][:, :], ysl, ALU.add, [P.bps[tb]], [byacc[tb]])
                if P.tap == "s5dbg4" and (oc, d, pr) == (0, 0, 0) and P.dbgsel[:3] != (0, 0, 0):
                    fw.dma(P.dbg[:, 8192:8192 + 2048], yacc, reads=byacc, writes=[P.bdbg])
                    for i_ in range(2):
                        fw.copy(V_, Vr[:, 0:1024], Hh[i_][:, 0:1024], [bH[i_]], [bVr])
                        fw.dma(P.dbg[:, 10240 + i_ * 2048:10240 + i_ * 2048 + 1024], Vr[:, 0:1024], reads=[bVr], writes=[P.bdbg])
                        fw.copy(V_, Vi[:, 0:1024], Hh[i_][:, 1024:2048], [bH[i_]], [bVi])
                        fw.dma(P.dbg[:, 10240 + i_ * 2048 + 1024:10240 + i_ * 2048 + 2048], Vi[:, 0:1024], reads=[bVi], writes=[P.bdbg])
                        fw.copy(V_, gt2[:, 0:128], LTm[i_][0], [bLTm[i_][0]], [bgt])
                        fw.dma(P.dbg[:, 14336 + i_ * 128:14336 + (i_ + 1) * 128], gt2[:, 0:128], reads=[bgt], writes=[P.bdbg])
                if globals().get("S5BAR", False):
                    fw.barrier()
                if P.tap == "s5dbg4" and (oc, d, pr) == P.dbgsel[:3]:
                    for tb in range(4):
                        fw.dma(P.dbg[:, tb * 512:(tb + 1) * 512], yacc[:, tb * 512:(tb + 1) * 512], reads=[byacc[tb]], writes=[P.bdbg])
                    for tb in range(4):
                        fw.copy(V_, gtmp, P.psum[tb][:, :], [P.bps[tb]], [bgt])
                        fw.dma(P.dbg[:, 2048 + tb * 512:2048 + (tb + 1) * 512], gtmp, reads=[bgt], writes=[P.bdbg])
                    for ri in range(2):
                        fw.copy(V_, gtmp[:, 0:128], LTm[ri][pr], [bLTm[ri][pr]], [bgt])
                        fw.dma(P.dbg[:, 4096 + ri * 128:4096 + (ri + 1) * 128], gtmp[:, 0:128], reads=[bgt], writes=[P.bdbg])
                    return True
        if P.tap == "s5dbg3" and oc == P.dbgsel[0]:
            for tb in range(4):
                fw.copy(V_, gtmp, P.psum[tb][:, :], [P.bps[tb]], [bgt])
                fw.dma(P.dbg[:, tb * 512:(tb + 1) * 512], gtmp, reads=[bgt], writes=[P.bdbg])
            return True
        for tb in range(4):
            blk = hT[:, oc, tb * 512:(tb + 1) * 512]
            fw.stt(gtmp, blk, dcol[:, oc:oc + 1], yacc[:, tb * 512:(tb + 1) * 512], ALU.mult, ALU.add,
                   [byacc[tb], P.bh[tb], bprm], [bgt])
            fw.act(gt2, gtmp, AF.Square, [bgt], [bgt])
            fw.ts(V_, gt2, gt2, 0.044715, 1.0, ALU.mult, ALU.add, [bgt], [bgt])
            fw.tt(V_, gt2, gt2, gtmp, ALU.mult, [bgt], [bgt])
            fw.act(gt2, gt2, AF.Sigmoid, [bgt], [bgt], scale=2.0 * math.sqrt(2.0 / math.pi))
            fw.tt(V_, blk, gt2, gtmp, ALU.mult, [bgt], [P.bh[tb]])
    fw.barrier()
    A.release(mX)
    if P.tap == "y1":
        tap_bf16(P, hT.rearrange("p c t -> p (c t)"), KC * T, P.bh)
        return True
    return layer1_tail(P, mX)


def layer1_tail(P, mX):
    fw, A, I = P.fw, P.A, P.I
    l = 1
    xT, hT = P.xT, P.hT
    V_ = "vector"
    wo = [A.bf16(8 * 512).rearrange("p (k n) -> p k n", k=8) for _ in range(4)]
    bwo = [Buf(f"so{i}") for i in range(4)]
    wov = I["ssm_w_out"].rearrange("(k p) n -> p k n", p=128)
    sgm = [A.f32(512), A.f32(512)]
    bsgm = [Buf("sgm0"), Buf("sgm1")]
    for i in range(4):
        fw.dma(wo[i], wov[:, :, i * 512:(i + 1) * 512], writes=[bwo[i]], q="gpsimd")
    it = 0
    for tb in range(4):
        t0, tl = TBS[tb]
        for m in range(8):
            pa, pg = (it % 4) * 2, (it % 4) * 2 + 1
            it += 1
            for (pb, wi_) in ((pa, m // 4), (pg, 2 + m // 4)):
                for k in range(KC):
                    fw.mm(P.psum[pb][:, :], wo[wi_][:, k, (m % 4) * 128:(m % 4 + 1) * 128], hT[:, k, t0:t0 + tl],
                          k == 0, k == KC - 1, [bwo[wi_], P.bh[tb]], [P.bps[pb]])
            j = it % 2
            fw.act(sgm[j], P.psum[pg][:, :], AF.Sigmoid, [P.bps[pg]], [bsgm[j]])
            fw.tt(V_, sgm[j], P.psum[pa][:, :], sgm[j], ALU.mult, [P.bps[pa]], [bsgm[j]])
            fw.stt(xT[:, m, t0:t0 + tl], sgm[j], mod_cols(P, l, 2, m, 0), xT[:, m, t0:t0 + tl], ALU.mult, ALU.add,
                   [bsgm[j], P.bmod], [P.bx[tb]])
    fw.barrier()
    A.release(mX)
    if P.tap == "xmix1":
        tap_out(P, xT.rearrange("p c t -> p (c t)"), KC * T, P.bx)
        return True
    scr = norm_scratch(P)
    for tb in range(4):
        t0, tl = TBS[tb]
        norm_mod_block(P, l, 1, tb, xT[:, :, t0:t0 + tl], P.bx[tb], scr)
    fw.barrier()
    A.release(mX)
    wr = A.bf16(64).rearrange("p (k e) -> p k e", k=8)
    bwr = Buf("wr")
    fw.dma(wr, I["moe_router"].rearrange("(k p) e -> p k e", p=128), writes=[bwr], q="gpsimd", **SLOW)
    lg = A.f32(128).rearrange("p (t e) -> p t e", e=8)
    lg2 = A.f32(128).rearrange("p (t e) -> p t e", e=8)
    eq1 = A.f32(128).rearrange("p (t e) -> p t e", e=8)
    eq2 = A.f32(128).rearrange("p (t e) -> p t e", e=8)
    gate = A.f32(128).rearrange("p (t e) -> p t e", e=8)
    m1 = A.f32(16); m2 = A.f32(16); w1_ = A.f32(16); w2_ = A.f32(16)
    bg_ = Buf("gate")
    for tt in range(16):
        for k in range(KC):
            fw.mm(P.psum[0][:, tt * 8:(tt + 1) * 8], hT[:, k, tt * 128:(tt + 1) * 128], wr[:, k, :], k == 0, k == KC - 1,
                  [P.bh[tt // 4], bwr], [P.bps[0]])
    G1 = [bg_]
    fw.copy(V_, lg, P.psum[0][:, 0:128].rearrange("p (t e) -> p t e", e=8), [P.bps[0]], G1)

    def bc8(v):
        return bass.AP(v.tensor, v.offset, [list(d_) for d_ in v.ap] + [[0, 8]])
    fw.op(V_, lambda e: e.tensor_reduce(m1, lg, AX.X, ALU.max), reads=G1, writes=G1)
    fw.tt(V_, eq1, lg, bc8(m1), ALU.is_equal, G1, G1)
    fw.stt(lg2, eq1, -1.0e30, lg, ALU.mult, ALU.add, G1, G1)
    fw.op(V_, lambda e: e.tensor_reduce(m2, lg2, AX.X, ALU.max), reads=G1, writes=G1)
    fw.tt(V_, eq2, lg2, bc8(m2), ALU.is_equal, G1, G1)
    fw.tt(V_, w2_, m2, m1, ALU.subtract, G1, G1)
    fw.act(w2_, w2_, AF.Exp, G1, G1)
    fw.ts(V_, w1_, w2_, 1.0, None, ALU.add, None, G1, G1)
    fw.op(V_, lambda e: e.reciprocal(w1_, w1_), reads=G1, writes=G1)
    fw.tt(V_, w2_, w2_, w1_, ALU.mult, G1, G1)
    fw.tt(V_, gate, eq1, bc8(w1_), ALU.mult, G1, G1)
    fw.tt(V_, eq2, eq2, bc8(w2_), ALU.mult, G1, G1)
    fw.tt(V_, gate, gate, eq2, ALU.add, G1, G1)
    gbc = [A.bf16(L), A.bf16(L)]
    bgbc = [Buf("gbc0"), Buf("gbc1")]
    fb = ffn_buffers(P, L)
    halves = [(0, 1024), (1024, 1024)]
    for e_ in range(NEXP):
        gj = e_ % 2
        for tb in range(4):
            pb = 4 + (e_ * 4 + tb) % 4
            for t4 in range(4):
                tt = tb * 4 + t4
                gcol = gate[:, tt, e_:e_ + 1]
                gl_ = bass.AP(gcol.tensor, gcol.offset, [list(gcol.ap[0]), [0, 128]])
                fw.mm(P.psum[pb][:, t4 * 128:(t4 + 1) * 128], gl_, P.ident, True, True, G1 + [P.bconst], [P.bps[pb]])
            fw.copy("scalar", gbc[gj][:, tb * 512:(tb + 1) * 512], P.psum[pb][:, :], [P.bps[pb]], [bgbc[gj]])
        nxt = None
        if e_ + 1 < NEXP:
            nxt = (I["moe_w1"][e_ + 1], I["moe_w3"][e_ + 1], I["moe_w2"][e_ + 1])
        ffn(P, l, fb, I["moe_w1"][e_], I["moe_w3"][e_], I["moe_w2"][e_], EXPERT_DIM, halves, 4,
            gate=gbc[gj], bgate=bgbc[gj], first=(e_ == 0), nxt=nxt)
    fw.barrier()
    A.release(mX)
    if P.tap == "xffn1":
        tap_out(P, xT.rearrange("p c t -> p (c t)"), KC * T, P.bx)
        return True
    return False


def write_output(P):
    fw, A = P.fw, P.A
    m0 = A.mark()
    xo = [A.f32(D) for _ in range(2)]
    bxo = [Buf("xo0"), Buf("xo1")]
    ntt = 18 if 1 not in P.layers else 16
    bo = Buf("out_lat")
    boc = Buf("out_ctx")
    pi = 0
    for tt in range(ntt):
        j = tt % 2
        tb = min(tt // 4, 4)
        for half in range(2):
            pb = pi % 4
            pi += 1
            ps, bps = P.psum[pb], P.bps[pb]
            for cc in range(4):
                c = half * 4 + cc
                fw.tr(ps[:, cc * 128:(cc + 1) * 128], P.xT[:, c, tt * 128:(tt + 1) * 128], P.ident,
                      [P.bx[tb], P.bconst], [bps])
            fw.copy("vector" if half else "scalar", xo[j][:, half * 512:(half + 1) * 512], ps[:, :], [bps], [bxo[j]])
        if tt < 16:
            fw.dma(P.out_lat[tt * 128:(tt + 1) * 128, :], xo[j], reads=[bxo[j]], writes=[bo])
        else:
            fw.dma(P.out_ctx[(tt - 16) * 128:(tt - 15) * 128, :], xo[j], reads=[bxo[j]], writes=[boc])
    P.out_bufs.append(bo)
    if ntt == 18:
        P.out_bufs.append(boc)
    A.release(m0)


_CACHE = {}


def _get_prog(key, **kw):
    if key not in _CACHE:
        _CACHE[key] = build_program(**kw)
    return _CACHE[key]


def make_in_map(inputs, b, layers=(0, 1), x_override=None, ctx_override=None):
    f = lambda a: np.ascontiguousarray(np.asarray(a, dtype=np.float32))
    m = {}
    m["x"] = f(inputs["x"][b] if x_override is None else x_override)
    m["ctx"] = f(inputs["ctx"][b] if ctx_override is None else ctx_override)
    m["cc"] = f(np.stack([np.asarray(inputs["c"][b]), np.asarray(inputs["c_ctx"])]))
    for k in ("ada_w", "ada_b", "norm1_g", "norm2_g"):
        m[k] = f(inputs[k])
    if 0 in layers:
        for k in ("mix_w_in", "q_norm_g", "k_norm_g", "na_rpb", "conv_dw_w", "conv_dw_b", "conv_ln_g",
                  "conv_ln_b", "mix_w_out", "ffn_w1", "ffn_w3", "ffn_w2"):
            m[k] = f(inputs[k][0])
    if 1 in layers:
        for k in ("ssm_w_in", "ssm_a_re", "ssm_a_im", "ssm_log_dt", "ssm_b_re", "ssm_b_im", "ssm_c_re",
                  "ssm_c_im", "ssm_d", "ssm_w_out", "moe_router", "moe_w1", "moe_w3", "moe_w2"):
            m[k] = f(inputs[k][0])
    return m


MODE = "fused"


def kernel(**inputs):
    if MODE == "fused":
        nc, P = _get_prog("full", layers=(0, 1))
        in_maps = [make_in_map(inputs, b) for b in range(8)]
        res = run_bass_kernel_spmd(nc, in_maps, core_ids=list(range(8)))
        return np.stack([np.asarray(r["out_lat"], dtype=np.float32) for r in res.results], axis=0)
    ncA, PA = _get_prog("L0", layers=(0,))
    in_maps = [make_in_map(inputs, b, layers=(0,)) for b in range(8)]
    resA = run_bass_kernel_spmd(ncA, in_maps, core_ids=list(range(8)))
    ncB, PB = _get_prog("L1", layers=(1,))
    in_maps = [make_in_map(inputs, b, layers=(1,), x_override=np.asarray(resA.results[b]["out_lat"]),
                           ctx_override=np.asarray(resA.results[b]["out_ctx"])) for b in range(8)]
    resB = run_bass_kernel_spmd(ncB, in_maps, core_ids=list(range(8)))
    return np.stack([np.asarray(r["out_lat"], dtype=np.float32) for r in resB.results], axis=0)
```

```python
import contextlib
import math
import numpy as np
import concourse.bass as bass
import concourse.mybir as mybir
from concourse.bass_utils import run_bass_kernel_spmd

F32 = mybir.dt.float32
BF16 = mybir.dt.bfloat16
I32 = mybir.dt.int32
ALU = mybir.AluOpType
AF = mybir.ActivationFunctionType
AX = mybir.AxisListType

SEM_ROLL = 30000
D = 1024
KC = 8
L = 2048
NCTX = 256
T = L + NCTX
EPS = 1e-6
TBS = [(0, 512), (512, 512), (1024, 512), (1536, 512), (2048, 256)]
FFN_DIM = 2816
NEXP = 8
EXPERT_DIM = 3584
NEG = -30000.0
SLOW = dict(allow_slow_non_contiguous=True)


class Buf:
    __slots__ = ("name", "w", "r", "dsem", "dcnt")

    def __init__(self, name=""):
        self.name = name
        self.w = None
        self.r = []
        self.dsem = None
        self.dcnt = 0


class FW:
    ENGS = ("sync", "tensor", "vector", "scalar", "gpsimd")

    def __init__(self, nc):
        self.nc = nc
        self.es = contextlib.ExitStack()
        self.ops = {e: [] for e in self.ENGS}
        self.esem = {}
        self.ecnt = {e: 0 for e in self.ENGS}
        self.seen = {e: {} for e in self.ENGS}
        self.nsem = 0
        self.pending_dma = []
        for e in self.ENGS:
            self.esem[e] = self.new_sem("e_" + e)
        self.n_ops = 0

    def new_sem(self, name):
        self.nsem += 1
        return self.es.enter_context(self.nc.semaphore(f"{name}_{self.nsem}"))

    def sb(self, name, shape, dt):
        return self.es.enter_context(self.nc.sbuf_tensor(name, list(shape), dt))

    def ps(self, name, shape, dt):
        return self.es.enter_context(self.nc.psum_tensor(name, list(shape), dt))

    def _need(self, eng, tok, waits):
        if tok is None:
            return
        sem, val, weng = tok
        if weng == eng == "tensor":
            return
        k = id(sem)
        cur = self.seen[eng].get(k)
        if cur is not None and cur[1] >= val:
            return
        for i, (s, v) in enumerate(waits):
            if s is sem:
                if v < val:
                    waits[i] = (s, val)
                return
        waits.append((sem, val))

    def op(self, eng, fn, reads=(), writes=(), dma_dst=None):
        waits = []
        for b in reads:
            self._need(eng, b.w, waits)
        for b in writes:
            self._need(eng, b.w, waits)
            for t in b.r:
                self._need(eng, t, waits)
        for s, v in waits:
            self.seen[eng][id(s)] = (s, v)
        if dma_dst is not None:
            if dma_dst.dsem is None or dma_dst.dcnt + 16 > SEM_ROLL:
                dma_dst.dsem = self.new_sem("d_" + dma_dst.name)
                dma_dst.dcnt = 0
            dma_dst.dcnt += 16
            tok = (dma_dst.dsem, dma_dst.dcnt, "dma")
            inc = (dma_dst.dsem, 16)
            self.pending_dma.append(tok)
        else:
            if self.ecnt[eng] + 1 > SEM_ROLL:
                self.esem[eng] = self.new_sem("e_" + eng)
                self.ecnt[eng] = 0
            self.ecnt[eng] += 1
            tok = (self.esem[eng], self.ecnt[eng], eng)
            inc = (self.esem[eng], 1)
        for b in writes:
            b.w = tok
            b.r = []
        for b in reads:
            if b not in writes:
                b.r.append(tok)
        wl = list(waits)

        def emit(e, fn=fn, wl=wl, inc=inc):
            for s, v in wl:
                e.wait_ge(s, v)
            fn(e).then_inc(inc[0], inc[1])

        self.ops[eng].append(emit)
        self.n_ops += 1
        return tok

    def barrier(self):
        toks = [(self.esem[e], self.ecnt[e], e) for e in self.ENGS if self.ecnt[e] > 0]
        toks += self.pending_dma
        self.pending_dma = []
        for eng in self.ENGS:
            waits = []
            for t in toks:
                if t[2] == eng:
                    continue
                self._need(eng, t, waits)
            for s, v in waits:
                self.seen[eng][id(s)] = (s, v)
            wl = list(waits)

            def emit(e, wl=wl):
                for s, v in wl:
                    e.wait_ge(s, v)
            self.ops[eng].append(emit)

    def dma(self, out, in_, reads=(), writes=(), q="sync", **kw):
        return self.op(q, lambda e: e.dma_start(out=out, in_=in_, **kw),
                       reads=reads, writes=writes, dma_dst=writes[0])

    def mm(self, out, lhsT, rhs, start, stop, reads, writes, tp=None):
        if tp is not None:
            return self.op("tensor", lambda e: e.matmul(out, lhsT, rhs, start=start, stop=stop, tile_position=tp),
                           reads=reads, writes=writes)
        return self.op("tensor", lambda e: e.matmul(out, lhsT, rhs, start=start, stop=stop),
                       reads=reads, writes=writes)

    def tr(self, out, in_, ident, reads, writes):
        return self.op("tensor", lambda e: e.transpose(out, in_, ident), reads=reads, writes=writes)

    def act(self, out, in_, func, reads, writes, scale=1.0, bias=0.0):
        return self.op("scalar", lambda e: e.activation(out=out, in_=in_, func=func, scale=scale, bias=bias),
                       reads=reads, writes=writes)

    def tt(self, eng, out, in0, in1, op, reads, writes):
        return self.op(eng, lambda e: e.tensor_tensor(out, in0, in1, op), reads=reads, writes=writes)

    def ts(self, eng, out, in0, s1, s2, op0, op1, reads, writes):
        if s2 is None:
            return self.op(eng, lambda e: e.tensor_scalar(out, in0, s1, None, op0), reads=reads, writes=writes)
        return self.op(eng, lambda e: e.tensor_scalar(out, in0, s1, s2, op0, op1), reads=reads, writes=writes)

    def stt(self, out, in0, scalar, in1, op0, op1, reads, writes):
        return self.op("vector", lambda e: e.scalar_tensor_tensor(out, in0, scalar, in1, op0, op1),
                       reads=reads, writes=writes)

    def copy(self, eng, out, in_, reads, writes):
        if eng == "scalar":
            return self.act(out, in_, AF.Identity, reads, writes)
        return self.op(eng, lambda e: e.tensor_copy(out, in_), reads=reads, writes=writes)

    def memset(self, eng, ap, val, writes):
        return self.op(eng, lambda e: e.memset(ap, val), writes=writes)

    def finish(self, final_bufs):
        toks = [b.w for b in final_bufs if b.w is not None]
        ops = self.ops

        def fin(e):
            for s, v, _ in toks:
                e.wait_ge(s, v)
        ops["sync"].append(fin)
        with self.nc.Block() as block:
            @block.sync
            def _(e):
                for f in ops["sync"]:
                    f(e)

            @block.tensor
            def _(e):
                for f in ops["tensor"]:
                    f(e)

            @block.vector
            def _(e):
                for f in ops["vector"]:
                    f(e)

            @block.scalar
            def _(e):
                for f in ops["scalar"]:
                    f(e)

            @block.gpsimd
            def _(e):
                for f in ops["gpsimd"]:
                    f(e)
        self.es.close()


class Arena:
    def __init__(self, fw, nwords):
        self.t32 = fw.sb("arena", [128, nwords], F32)
        self.t16 = self.t32.bitcast(BF16)
        self.ti32 = self.t32.bitcast(I32)
        self.n = nwords
        self.top = 0
        self.peak = 0

    def ap32(self, off, dims):
        return bass.AP(self.t32, off, [[self.n, 128]] + [list(d) for d in dims])

    def ap16(self, off, dims):
        return bass.AP(self.t16, off, [[2 * self.n, 128]] + [list(d) for d in dims])

    def _take(self, nwords):
        self.last_off = self.top
        a = self.top
        self.top += nwords
        self.peak = max(self.peak, self.top)
        assert self.top <= self.n, f"arena overflow {self.top} > {self.n}"
        return a

    def f32(self, n):
        a = self._take(n)
        return self.t32[:, a:a + n]

    def i32(self, n):
        a = self._take(n)
        return self.ti32[:, a:a + n]

    def bf16(self, n):
        nw = (n + 1) // 2
        a = self._take(nw)
        return self.t16[:, 2 * a:2 * a + n]

    def mark(self):
        return self.top

    def release(self, m):
        self.top = m


class Prog:
    pass


def build_program(layers=(0, 1), tap=None, in_feature_major=False):
    nc = bass.Bass("TRN2", target_bir_lowering=False)
    fw = FW(nc)
    P = Prog()
    P.nc, P.fw = nc, fw

    def din(name, shape, dt=F32):
        return nc.dram_tensor(name, list(shape), dt, kind="ExternalInput").ap()

    I = {}
    I["x"] = din("x", [L, D])
    I["ctx"] = din("ctx", [NCTX, D])
    I["cc"] = din("cc", [2, D])
    I["ada_w"] = din("ada_w", [2, D, 6 * D])
    I["ada_b"] = din("ada_b", [2, 6 * D])
    I["norm1_g"] = din("norm1_g", [2, D])
    I["norm2_g"] = din("norm2_g", [2, D])
    if 0 in layers:
        I["mix_w_in"] = din("mix_w_in", [D, 2560])
        I["q_norm_g"] = din("q_norm_g", [64])
        I["k_norm_g"] = din("k_norm_g", [64])
        I["na_rpb"] = din("na_rpb", [8, 15, 31])
        I["conv_dw_w"] = din("conv_dw_w", [31, 512])
        I["conv_dw_b"] = din("conv_dw_b", [512])
        I["conv_ln_g"] = din("conv_ln_g", [512])
        I["conv_ln_b"] = din("conv_ln_b", [512])
        I["mix_w_out"] = din("mix_w_out", [D, D])
        I["ffn_w1"] = din("ffn_w1", [D, FFN_DIM])
        I["ffn_w3"] = din("ffn_w3", [D, FFN_DIM])
        I["ffn_w2"] = din("ffn_w2", [FFN_DIM, D])
    if 1 in layers:
        I["ssm_w_in"] = din("ssm_w_in", [D, D])
        I["ssm_a_re"] = din("ssm_a_re", [2, 64, 64])
        I["ssm_a_im"] = din("ssm_a_im", [2, 64, 64])
        I["ssm_log_dt"] = din("ssm_log_dt", [2, 64])
        I["ssm_b_re"] = din("ssm_b_re", [2, 64, 64, 16])
        I["ssm_b_im"] = din("ssm_b_im", [2, 64, 64, 16])
        I["ssm_c_re"] = din("ssm_c_re", [2, 64, 16, 64])
        I["ssm_c_im"] = din("ssm_c_im", [2, 64, 16, 64])
        I["ssm_d"] = din("ssm_d", [D])
        I["ssm_w_out"] = din("ssm_w_out", [D, 2 * D])
        I["moe_router"] = din("moe_router", [D, NEXP])
        I["moe_w1"] = din("moe_w1", [NEXP, D, EXPERT_DIM])
        I["moe_w3"] = din("moe_w3", [NEXP, D, EXPERT_DIM])
        I["moe_w2"] = din("moe_w2", [NEXP, EXPERT_DIM, D])
    P.I = I
    P.out_lat = nc.dram_tensor("out_lat", [L, D], F32, kind="ExternalOutput").ap()
    P.out_bufs = []
    if 1 not in layers:
        P.out_ctx = nc.dram_tensor("out_ctx", [NCTX, D], F32, kind="ExternalOutput").ap()
    P.tap = tap
    P.dbgsel = globals().get("DBGSEL", (0, 0, 0, 0))
    if tap is not None:
        P.dbg = nc.dram_tensor("dbg", [128, 8 * T], F32, kind="ExternalOutput").ap()
        P.bdbg = Buf("dbg")

    A = Arena(fw, 53150)
    P.A = A
    P.psum = [fw.ps(f"ps{i}", [128, 512], F32) for i in range(8)]
    P.bps = [Buf(f"ps{i}") for i in range(8)]

    setup_consts(P)
    P.hT = A.bf16(KC * T).rearrange("p (c t) -> p c t", c=KC)
    P.hT_off = A.last_off
    P.bh = [Buf(f"hT{i}") for i in range(5)]
    P.xT = None
    P.layers = layers
    done = False
    for l in layers:
        if l not in P.adaln_done and not (l == 0 and globals().get("ADA0_INTERLEAVE", False)):
            adaln(P, l)
            P.adaln_done.add(l)
        if l == 0:
            done = layer0(P)
        else:
            done = layer1(P)
        if done:
            break
    if not done:
        write_output(P)
    fw.finish(P.out_bufs + ([P.bdbg] if tap is not None else []))
    P.peak = A.peak
    return nc, P


def setup_consts(P):
    fw, A = P.fw, P.A
    P.bconst = Buf("const")
    bc = P.bconst
    io = A.f32(128)
    pio = A.f32(1)
    P.ident = A.f32(128)
    P.identb = A.bf16(128)
    P.onesD = A.f32(128)
    P.ones512 = A.f32(128)
    P.ones64 = A.f32(128)
    P.ones1 = A.bf16(128)
    P.iota_f = io
    P.pio = pio
    P.halfpi = A.f32(1)
    fw.memset("vector", P.halfpi, math.pi / 2.0, [bc])
    fw.op("gpsimd", lambda e: e.iota(io, [[1, 128]], base=0, channel_multiplier=0,
                                     allow_small_or_imprecise_dtypes=True), writes=[bc])
    fw.op("gpsimd", lambda e: e.iota(pio, [[0, 1]], base=0, channel_multiplier=1,
                                     allow_small_or_imprecise_dtypes=True), writes=[bc])
    fw.ts("vector", P.ident, io, pio[:, 0:1], None, ALU.is_equal, None, [bc], [bc])
    fw.copy("vector", P.identb, P.ident, [bc], [bc])
    fw.memset("vector", P.onesD, 1.0 / D, [bc])
    fw.memset("vector", P.ones512, 1.0 / 512, [bc])
    fw.memset("vector", P.ones1, 1.0 / D, [bc])
    fw.memset("vector", P.ones64, 0.0, [bc])
    fw.memset("vector", P.ones64[0:64, 0:64], 1.0 / 64, [bc])
    fw.memset("vector", P.ones64[64:128, 64:128], 1.0 / 64, [bc])
    P.mod = [A.f32(96).rearrange("p (m s) -> p m s", s=2) for _ in range(2)]
    P.modA = [A.f32(32).rearrange("p (n c s) -> p n c s", n=2, s=2) for _ in range(2)]
    P.gn = A.f32(32).rearrange("p (l n c) -> p l n c", l=2, n=2)
    P.bmod = Buf("mod")
    P.adaln_done = set()
    m_g = A.mark()
    gnat = A.f32(128)
    for n_, nm in enumerate(("norm1_g", "norm2_g")):
        fw.dma(gnat[n_ * 16:(n_ + 1) * 16, :], P.I[nm].rearrange("l (c p) -> (l c) p", p=128), writes=[P.bmod])
    fw.tr(P.psum[0][:, 0:32], gnat[0:32, :], P.ident[0:32, 0:32], [P.bmod, bc], [P.bps[0]])
    for n_ in range(2):
        fw.copy("vector", P.gn[:, :, n_, :], P.psum[0][:, n_ * 16:(n_ + 1) * 16].rearrange("p (l c) -> p l c", l=2),
                [P.bps[0]], [P.bmod])
    fw.barrier()
    A.release(m_g)


def adaln(P, l, hold=False, banks=(7, 6)):
    fw, A, I = P.fw, P.A, P.I
    m0 = A.mark()
    ccT = A.f32(16).rearrange("p (c s) -> p c s", s=2)
    scT = A.f32(16).rearrange("p (c s) -> p c s", s=2)
    adab = A.f32(48)
    bcc = Buf("cc")
    pan = [A.bf16(8 * 512).rearrange("p (k n) -> p k n", k=8) for _ in range(3)]
    bpan = [Buf("adapan0"), Buf("adapan1"), Buf("adapan2")]
    scTb = A.bf16(16).rearrange("p (c s) -> p c s", s=2)
    nat = A.f32(128)
    fw.dma(nat[0:16, :], I["cc"].rearrange("s (c p) -> (s c) p", p=128), writes=[bcc])
    fw.dma(nat[16:64, :], I["ada_b"][l].rearrange("(m p) -> m p", p=128), writes=[bcc])
    fw.tr(P.psum[banks[1]][:, 0:64], nat[0:64, :], P.ident[0:64, 0:64], [bcc, P.bconst], [P.bps[banks[1]]])
    fw.copy("vector", ccT, P.psum[banks[1]][:, 0:16].rearrange("p (s c) -> p c s", s=2), [P.bps[banks[1]]], [bcc])
    fw.copy("vector", adab, P.psum[banks[1]][:, 16:64], [P.bps[banks[1]]], [bcc])
    fw.act(scT, ccT, AF.Silu, [bcc], [bcc])
    fw.copy("vector", scTb, scT, [bcc], [bcc])
    wv = I["ada_w"][l].rearrange("(k p) n -> p k n", p=128)
    ps = P.psum[banks[0]]
    bps = P.bps[banks[0]]
    for pi in range(12):
        j = pi % 3
        fw.dma(pan[j], wv[:, :, pi * 512:(pi + 1) * 512], writes=[bpan[j]], q="gpsimd")
        for mi in range(4):
            m = pi * 4 + mi
            for k in range(8):
                fw.mm(ps[:, 2 * m:2 * m + 2], pan[j][:, k, mi * 128:(mi + 1) * 128], scTb[:, k, :],
                      k == 0, k == 7, [bpan[j], bcc], [bps])
    mod = P.mod[l]
    for s in range(2):
        fw.tt("vector", mod[:, :, s], ps[:, 0:96].rearrange("p (m s) -> p m s", s=2)[:, :, s], adab, ALU.add,
              [bps, bcc], [P.bmod])
    for n in range(2):
        for s in range(2):
            sc = mod[:, (3 * n + 1) * 8:(3 * n + 2) * 8, s]
            fw.stt(P.modA[l][:, n, :, s], sc, 1.0, P.gn[:, l, n, :], ALU.add, ALU.mult, [P.bmod], [P.bmod])
    if hold:
        return
    fw.barrier()
    A.release(m0)


def mod_cols(P, l, j, c, s):
    return P.mod[l][:, j * 8 + c, s:s + 1]


def load_xT_block(P, tb, dst, bdst, ev=0):
    fw, A = P.fw, P.A
    t0, tl = TBS[tb]
    for tt in range(tl // 128):
        j = P.xin_i % 2
        P.xin_i += 1
        tok = t0 + tt * 128
        src = P.I["x"][tok:tok + 128, :] if tok < L else P.I["ctx"][tok - L:tok - L + 128, :]
        fw.dma(P.xin[j], src, writes=[P.bxin[j]])
        for half in range(2):
            pb = P.tps[P.tps_i % 2]
            ps, bps = P.psum[pb], P.bps[pb]
            P.tps_i += 1
            for cc in range(4):
                c = half * 4 + cc
                fw.tr(ps[:, cc * 128:(cc + 1) * 128], P.xin[j][:, c * 128:(c + 1) * 128], P.ident,
                      [P.bxin[j], P.bconst], [bps])
            eng = "vector" if (P.tps_i % 2) else "scalar"
            fw.copy(eng, dst[:, half * 4:half * 4 + 4, tt * 128:(tt + 1) * 128],
                    ps[:, 0:512].rearrange("p (c t) -> p c t", c=4), [bps], [bdst])


def norm_mod_block(P, l, n, tb, xsrc, bx, scr):
    fw = P.fw
    t0, tl = TBS[tb]
    s = 1 if tb == 4 else 0
    sq, sd, tmp, bsq, bsd, btmp = scr
    pb = 6
    ps, bps = P.psum[pb], P.bps[pb]
    for c in range(KC):
        fw.act(sq[c % 2][:, 0:tl], xsrc[:, c, 0:tl], AF.Square, [bx], [bsq[c % 2]])
        fw.mm(ps[:, 0:tl], P.ones1, sq[c % 2][:, 0:tl], c == 0, c == KC - 1, [bsq[c % 2], P.bconst], [bps])
    fw.act(sd[:, 0:tl], ps[:, 0:tl], AF.Sqrt, [bps], [bsd], bias=P.epsc[:, 0:1])
    fw.op("vector", lambda e: e.reciprocal(sd[:, 0:tl], sd[:, 0:tl]), reads=[bsd], writes=[bsd])
    for c in range(KC):
        j = c % 2
        fw.stt(tmp[j][:, 0:tl], xsrc[:, c, 0:tl], P.modA[l][:, n, c, s:s + 1], sd[:, 0:tl], ALU.mult, ALU.mult,
               [bx, bsd, P.bmod], [btmp[j]])
        fw.act(P.hT[:, c, t0:t0 + tl], tmp[j][:, 0:tl], AF.Identity, [btmp[j], P.bmod], [P.bh[tb]],
               bias=mod_cols(P, l, 3 * n, c, s))


def norm_scratch(P):
    A = P.A
    sq = [A.bf16(512), A.bf16(512)]
    sd = A.f32(512)
    tmp = [A.f32(512), A.f32(512)]
    return (sq, sd, tmp, [Buf("sq0"), Buf("sq1")], Buf("sd"), [Buf("tmp0"), Buf("tmp1")])


def tap_out(P, ap2d, ncols, bufs):
    P.fw.dma(P.dbg[:, 0:ncols], ap2d, reads=bufs, writes=[P.bdbg])


def tap_bf16(P, src3, n, bufs):
    fw, A = P.fw, P.A
    m = A.mark()
    t = A.f32(2304)
    bt = Buf("tapt")
    for c in range(n // 2304 if n >= 2304 else 1):
        w = min(n, 2304)
        fw.copy("vector", t[:, 0:w], src3[:, c * 2304:c * 2304 + w], bufs, [bt])
        fw.dma(P.dbg[:, c * 2304:c * 2304 + w], t[:, 0:w], reads=[bt], writes=[P.bdbg])
    A.release(m)


def layer0(P):
    fw, A, I = P.fw, P.A, P.I
    l = 0
    if not hasattr(P, "epsc"):
        P.epsc = A.f32(1)
        fw.memset("vector", P.epsc, EPS, [P.bconst])
    mM = A.mark()
    P.xin = [A.f32(D), A.f32(D)]
    P.bxin = [Buf("xin0"), Buf("xin1")]
    P.xin_i = 0
    P.tps = [4, 5]
    P.tps_i = 0
    xblk = [A.f32(KC * 512).rearrange("p (c t) -> p c t", c=KC) for _ in range(2)]
    bxb = [Buf("xblk0"), Buf("xblk1")]
    scr = norm_scratch(P)

    def phase0():
        for tb in range(5):
            j = tb % 2
            load_xT_block(P, tb, xblk[j], bxb[j])
            norm_mod_block(P, l, 0, tb, xblk[j], bxb[j], scr)
    if 0 not in P.adaln_done:
        def rec0(fn, *a_, **k_):
            calls = []
            real = fw.op
            fw.op = lambda *aa, **kk: calls.append((aa, kk))
            try:
                fn(*a_, **k_)
            finally:
                fw.op = real
            return calls
        cb = rec0(adaln, P, 0, hold=True, banks=(7, 3))
        P.adaln_done.add(0)
        ca = rec0(phase0)
        na, nb = len(ca), len(cb)
        ia = ib = 0
        while ia < na or ib < nb:
            if ia >= na or (ib < nb and ib * max(na, 1) <= ia * max(nb, 1) * 3):
                aa, kk = cb[ib]; ib += 1
            else:
                aa, kk = ca[ia]; ia += 1
            fw.op(*aa, **kk)
    else:
        phase0()
    if P.tap == "h1_0":
        tap_bf16(P, P.hT.rearrange("p c t -> p (c t)"), KC * T, P.bh)
        return True
    fw.barrier()
    A.release(mM)
    qkT = A.bf16(8 * T).rearrange("p (c t) -> p c t", c=8)
    bqk = [[Buf(f"qk{c}_{tb}") for tb in range(5)] for c in range(8)]
    V = A.bf16(18 * 8 * 65).rearrange("p (j h d) -> p j h d", j=18, h=8)
    bV = [Buf(f"V{j}") for j in range(18)]
    mB3 = A.mark()
    ULEN = 2078 + 286
    u = A.bf16(4 * ULEN).rearrange("p (c t) -> p c t", c=4)
    bu = [[Buf(f"u{c}_{tb}") for tb in range(5)] for c in range(4)]
    mB2 = A.mark()
    pans = [A.bf16(8 * 512).rearrange("p (k n) -> p k n", k=8) for _ in range(4)]
    bpan = [Buf(f"pan{i}") for i in range(4)]
    sq = [A.bf16(512) for _ in range(2)]
    bsq = [Buf("qsq0"), Buf("qsq1")]
    ones64b = A.bf16(128)
    fw.copy("vector", ones64b, P.ones64, [P.bconst], [P.bconst])
    sd = [A.f32(512) for _ in range(2)]
    bsd = [Buf("qsd0"), Buf("qsd1")]
    gq = A.f32(1)
    gk = A.f32(1)
    bg = Buf("gqk")
    for hh in range(2):
        fw.dma(gq[hh * 64:(hh + 1) * 64, :], I["q_norm_g"].rearrange("(d o) -> d o", o=1), writes=[bg], **SLOW)
        fw.dma(gk[hh * 64:(hh + 1) * 64, :], I["k_norm_g"].rearrange("(d o) -> d o", o=1), writes=[bg], **SLOW)
    fw.ts("vector", gq, gq, 0.125, None, ALU.mult, None, [bg], [bg])
    fw.memset("vector", V[:, :, :, 64:65], 1.0, bV)
    fw.memset("vector", u.rearrange("p c t -> p (c t)"), 0.0, [b for r in bu for b in r])
    wv = I["mix_w_in"].rearrange("(k p) n -> p k n", p=128)
    for pi in range(5):
        fw.dma(pans[pi % 4], wv[:, :, pi * 512:(pi + 1) * 512], writes=[bpan[pi % 4]], q="gpsimd")
        if pi == 3:
            break
    prj = [0, 1, 2, 3]
    prj_i = [0]
    st_i = [0]
    pending = [None]

    def qk_tail(args):
        c, tb, pb = args
        t0, tl = TBS[tb]
        ps, bps = P.psum[pb], P.bps[pb]
        sb_ = 4 + st_i[0] % 2
        j = st_i[0] % 2
        st_i[0] += 1
        ps2, bps2 = P.psum[sb_], P.bps[sb_]
        fw.mm(ps2[:, 0:tl], ones64b, sq[j][:, 0:tl], True, True, [bsq[j], P.bconst], [bps2])
        fw.act(sd[j][:, 0:tl], ps2[:, 0:tl], AF.Sqrt, [bps2], [bsd[j]], bias=P.epsc[:, 0:1])
        fw.op("vector", lambda e: e.reciprocal(sd[j][:, 0:tl], sd[j][:, 0:tl]), reads=[bsd[j]], writes=[bsd[j]])
        g = gq if c < 4 else gk
        fw.stt(qkT[:, c, t0:t0 + tl], ps[:, 0:tl], g[:, 0:1], sd[j][:, 0:tl], ALU.mult, ALU.mult,
               [bps, bsd[j], bg], [bqk[c][tb]])

    def b1_qk():
        for pi in range(2):
            pan, bp = pans[pi], bpan[pi]
            for tb in range(5):
                t0, tl = TBS[tb]
                for mc in range(4):
                    c = pi * 4 + mc
                    pb = prj[prj_i[0] % 4]
                    prj_i[0] += 1
                    ps, bps = P.psum[pb], P.bps[pb]
                    for k in range(KC):
                        fw.mm(ps[:, 0:tl], pan[:, k, mc * 128:(mc + 1) * 128], P.hT[:, k, t0:t0 + tl],
                              k == 0, k == KC - 1, [bp, P.bh[tb]], [bps])
                    j = (st_i[0] + (1 if pending[0] is not None else 0)) % 2
                    fw.act(sq[j][:, 0:tl], ps[:, 0:tl], AF.Square, [bps], [bsq[j]])
                    if pending[0] is not None:
                        qk_tail(pending[0])
                    pending[0] = (c, tb, pb)
        qk_tail(pending[0])
        pending[0] = None

    if 1 in P.layers and 1 not in P.adaln_done:
        def record_(fn, *a_, **k_):
            calls = []
            real = fw.op
            fw.op = lambda *aa, **kk: calls.append((aa, kk))
            try:
                fn(*a_, **k_)
            finally:
                fw.op = real
            return calls
        ca = record_(b1_qk)
        cb = record_(adaln, P, 1, hold=True, banks=(7, 6))
        P.adaln_done.add(1)
        na, nb = len(ca), len(cb)
        ia = ib = 0
        while ia < na or ib < nb:
            if ib >= nb or (ia < na and ia * max(nb, 1) <= ib * max(na, 1)):
                aa, kk = ca[ia]; ia += 1
            else:
                aa, kk = cb[ib]; ib += 1
            fw.op(*aa, **kk)
    else:
        b1_qk()
    pan, bp = pans[2], bpan[2]
    fw.dma(pans[0], wv[:, :, 4 * 512:5 * 512], writes=[bpan[0]], q="gpsimd")
    for tt in range(18):
        pb = prj[prj_i[0] % 4]
        prj_i[0] += 1
        ps, bps = P.psum[pb], P.bps[pb]
        tb = min(tt // 4, 4)
        for k in range(KC):
            fw.mm(ps[:, :], P.hT[:, k, tt * 128:(tt + 1) * 128], pan[:, k, :], k == 0, k == KC - 1,
                  [bp, P.bh[tb]], [bps])
        fw.copy("scalar" if tt % 2 else "vector", V[:, tt, :, 0:64], ps[:, :].rearrange("p (h d) -> p h d", h=8),
                [bps], [bV[tt]])
    sg = [A.f32(512) for _ in range(2)]
    bsg = [Buf("sg0"), Buf("sg1")]
    pa, bpa, pg, bpg = pans[3], bpan[3], pans[0], bpan[0]

    def uoff(tb):
        t0, tl = TBS[tb]
        return (15 + t0) if tb < 4 else (2078 + 15)
    for tb in range(5):
        t0, tl = TBS[tb]
        for mc in range(4):
            pba = prj[prj_i[0] % 4]
            pbg = prj[(prj_i[0] + 1) % 4]
            prj_i[0] += 2
            for (pn, bpn, pb) in ((pa, bpa, pba), (pg, bpg, pbg)):
                for k in range(KC):
                    fw.mm(P.psum[pb][:, 0:tl], pn[:, k, mc * 128:(mc + 1) * 128], P.hT[:, k, t0:t0 + tl],
                          k == 0, k == KC - 1, [bpn, P.bh[tb]], [P.bps[pb]])
            j = (tb * 4 + mc) % 2
            fw.act(sg[j][:, 0:tl], P.psum[pbg][:, 0:tl], AF.Sigmoid, [P.bps[pbg]], [bsg[j]])
            o = uoff(tb)
            fw.tt("vector", u[:, mc, o:o + tl], P.psum[pba][:, 0:tl], sg[j][:, 0:tl], ALU.mult,
                  [P.bps[pba], bsg[j]], [bu[mc][tb]])
    fw.barrier()
    A.release(mB2)
    catT = P.hT
    bcat = P.bh
    wrow = A.f32(512)
    cw = A.f32(4 * 31).rearrange("p (c k) -> p c k", c=4)
    cvec = A.f32(12).rearrange("p (v c) -> p v c", v=3)
    bcw = Buf("convw")
    fw.dma(wrow[0:31, :], I["conv_dw_w"], writes=[bcw])
    for vi, nm in enumerate(("conv_dw_b", "conv_ln_g", "conv_ln_b")):
        fw.dma(cvec[:, vi, :], I[nm].rearrange("(c p) -> p c", p=128), writes=[bcw], **SLOW)
    for c in range(4):
        fw.tr(P.psum[0][:, c * 32:c * 32 + 31], wrow[0:31, c * 128:(c + 1) * 128], P.ident[0:31, 0:31],
              [bcw, P.bconst], [P.bps[0]])
    fw.copy("vector", cw, P.psum[0][:, 0:128].rearrange("p (c k) -> p c k", c=4)[:, :, 0:31], [P.bps[0]], [bcw])
    acc = [A.f32(512) for _ in range(4)]
    bacc = [Buf(f"acc{c}") for c in range(4)]
    Dg = [[A.bf16(128) for _ in range(31)] for _ in range(4)]
    bDg = Buf("Dg")
    for c in range(4):
        for k in range(31):
            fw.ts("vector", Dg[c][k], P.identb, cw[:, c, k:k + 1], None, ALU.mult, None, [bcw, P.bconst], [bDg])
    csq = [A.f32(512) for _ in range(2)]
    bcsq = [Buf("csq0"), Buf("csq1")]
    mean = A.f32(512)
    msq = A.f32(512)
    rs = A.f32(512)
    bst = Buf("lnstat")
    t1 = [A.f32(512) for _ in range(2)]
    bt1 = [Buf("lt0"), Buf("lt1")]
    for tb in range(5):
        t0, tl = TBS[tb]
        ub = t0 if tb < 4 else 2078
        pm, bpm = P.psum[1 + 2 * (tb % 2)], P.bps[1 + 2 * (tb % 2)]
        pe2, bpe2 = P.psum[2 + 2 * (tb % 2)], P.bps[2 + 2 * (tb % 2)]
        for c in range(4):
            pcv = 5 + (tb * 4 + c) % 3
            for k in range(31):
                fw.mm(P.psum[pcv][:, 0:tl], Dg[c][k], u[:, c, ub + k:ub + k + tl], k == 0, k == 30,
                      [bu[c][tbb] for tbb in range(5)] + [bDg], [P.bps[pcv]])
            fw.act(acc[c][:, 0:tl], P.psum[pcv][:, 0:tl], AF.Identity, [P.bps[pcv], bcw], [bacc[c]],
                   bias=cvec[:, 0, c:c + 1])
            fw.mm(pm[:, 0:tl], P.ones512, acc[c][:, 0:tl], c == 0, c == 3, [bacc[c], P.bconst], [bpm])
            fw.act(csq[c % 2][:, 0:tl], acc[c][:, 0:tl], AF.Square, [bacc[c]], [bcsq[c % 2]])
            fw.mm(pe2[:, 0:tl], P.ones512, csq[c % 2][:, 0:tl], c == 0, c == 3, [bcsq[c % 2], P.bconst], [bpe2])
        fw.act(mean[:, 0:tl], pm[:, 0:tl], AF.Identity, [bpm], [bst])
        fw.act(msq[:, 0:tl], pm[:, 0:tl], AF.Square, [bpm], [bst])
        fw.tt("vector", rs[:, 0:tl], pe2[:, 0:tl], msq[:, 0:tl], ALU.subtract, [bpe2, bst], [bst])
        fw.ts("vector", rs[:, 0:tl], rs[:, 0:tl], 0.0, None, ALU.max, None, [bst], [bst])
        fw.act(rs[:, 0:tl], rs[:, 0:tl], AF.Sqrt, [bst], [bst], bias=P.epsc[:, 0:1])
        fw.op("vector", lambda e, tl=tl: e.reciprocal(rs[:, 0:tl], rs[:, 0:tl]), reads=[bst], writes=[bst])
        for c in range(4):
            j = c % 2
            fw.tt("vector", t1[j][:, 0:tl], acc[c][:, 0:tl], mean[:, 0:tl], ALU.subtract, [bacc[c], bst], [bt1[j]])
            fw.tt("vector", t1[j][:, 0:tl], t1[j][:, 0:tl], rs[:, 0:tl], ALU.mult, [bst], [bt1[j]])
            fw.act(catT[:, 4 + c, t0:t0 + tl], t1[j][:, 0:tl], AF.Silu, [bt1[j], bcw], [bcat[tb]],
                   scale=cvec[:, 1, c:c + 1], bias=cvec[:, 2, c:c + 1])
    fw.barrier()
    A.release(mB3)
    Fs = A.f32(127)
    bF = Buf("Fs")
    Fd = P.nc.dram_tensor("Fd_scratch", [120, 64, 127], F32).ap()
    Fs_off = A.last_off
    bFd = Buf("Fd")
    fw.memset("vector", Fs, 0.0, [bF])
    rp = I["na_rpb"]
    for h_ in range(8):
        fw.dma(Fs[h_ * 15:(h_ + 1) * 15, 48:79], bass.AP(rp.tensor, h_ * 15 * 31 + 30, [[31, 15], [-1, 31]]),
               writes=[bF], **SLOW)
    for h_ in range(8):
        fw.dma(Fd[h_ * 15:(h_ + 1) * 15], bass.AP(A.t32, Fs_off + h_ * 15 * A.n, [[A.n, 15], [0, 64], [1, 127]]),
               reads=[bF], writes=[bFd])
    TT = A.f32(8 * 14 * 64)
    TT_off = A.last_off
    TT4 = TT.rearrange("p (h e q) -> p h e q", h=8, e=14)
    bTT = Buf("TT")
    for h in range(8):
        for krl in range(2):
            src = bass.AP(Fd.tensor, (h * 15 + krl) * 64 * 127 + 63, [[126, 64], [64 * 127, 14], [1, 64]])
            fw.dma(TT4[krl * 64:(krl + 1) * 64, h, :, :], src, reads=[bFd], writes=[bTT])
    cm = A.f32(64)
    cm_off = A.last_off
    cs = A.f32(64)
    kcol = A.f32(1)
    bcm = Buf("cm")
    fw.op("gpsimd", lambda e: e.iota(kcol[0:64, :], [[0, 1]], base=0, channel_multiplier=1,
                                     allow_small_or_imprecise_dtypes=True), writes=[bcm])
    fw.op("gpsimd", lambda e: e.iota(kcol[64:128, :], [[0, 1]], base=0, channel_multiplier=1,
                                     allow_small_or_imprecise_dtypes=True), writes=[bcm])
    fw.ts("vector", cs, P.iota_f[:, 0:64], -8.0, 0.0, ALU.add, ALU.max, [P.bconst], [bcm])
    fw.ts("vector", cs, cs, 48.0, kcol[:, 0:1], ALU.min, ALU.subtract, [bcm], [bcm])
    fw.ts("vector", cm, cs, 0.0, None, ALU.is_le, None, [bcm], [bcm])
    fw.ts("vector", cs, cs, -15.0, None, ALU.is_ge, None, [bcm], [bcm])
    fw.tt("vector", cm, cm, cs, ALU.mult, [bcm], [bcm])
    fw.ts("vector", cm, cm, -NEG, NEG, ALU.mult, ALU.add, [bcm], [bcm])
    fw.tt("vector", TT.rearrange("p (m q) -> p m q", q=64), TT.rearrange("p (m q) -> p m q", q=64),
          A.ap32(cm_off, [[0, 112], [1, 64]]), ALU.add, [bcm, bTT], [bTT])
    ssb = [A.f32(320) for _ in range(2)]
    bssb = [Buf("ssb0"), Buf("ssb1")]
    pT = [A.bf16(448) for _ in range(3)]
    bpT = [Buf(f"pT{i}") for i in range(3)]
    rec = A.f32(8)
    A_rec_off = [A.last_off]
    brec = Buf("rec")
    osb = [A.bf16(512) for _ in range(2)]
    bosb = [Buf("osb0"), Buf("osb1")]
    its = []
    for qr in range(36):
        if qr < 32:
            ws = min(max(qr - 4, 0), 24)
            j0, j1 = ws // 2, (ws + 7) // 2
            nw = j1 - j0 + 1
            qtok = qr * 64
            e0 = 2 * j0 - qr + 7
            qtb = qr // 8
        else:
            ws, j0, nw, e0 = 0, 0, 0, 0
            qtok = L + (qr - 32) * 64
            qtb = 4
        for h in range(8):
            its.append((qr, h, ws, j0, nw, qtok, e0, qtb))

    def att_S(it):
        qr, h, ws, j0, nw, qtok, e0, qtb = its[it]
        pb = (h % 2) * 64
        cq, ck = h // 2, 4 + h // 2
        ps, bps = P.psum[it % 3], P.bps[it % 3]
        for i in range(nw + 2):
            ktok = (j0 + i) * 128 if i < nw else L + (i - nw) * 128
            ktb = min(ktok // 512, 4)
            fw.mm(ps[:, i * 64:(i + 1) * 64], qkT[pb:pb + 64, ck, ktok:ktok + 128],
                  qkT[pb:pb + 64, cq, qtok:qtok + 64], True, True, [bqk[ck][ktb], bqk[cq][qtb]], [bps])

    def att_SM(it):
        qr, h, ws, j0, nw, qtok, e0, qtb = its[it]
        ps, bps = P.psum[it % 3], P.bps[it % 3]
        ntile = nw + 2
        p_, bp_ = pT[it % 3], bpT[it % 3]
        if nw > 0:
            s_, bs_ = ssb[it % 2], bssb[it % 2]
            fw.tt("vector", s_[:, 0:nw * 64].rearrange("p (i q) -> p i q", q=64),
                  ps[:, 0:nw * 64].rearrange("p (i q) -> p i q", q=64),
                  A.ap32(TT_off + (h * 14 + e0) * 64, [[128, nw], [1, 64]]), ALU.add, [bps, bTT], [bs_])
            fw.act(p_[:, 0:nw * 64], s_[:, 0:nw * 64], AF.Exp, [bs_], [bp_])
        fw.act(p_[:, nw * 64:ntile * 64], ps[:, nw * 64:ntile * 64], AF.Exp, [bps], [bp_])

    def att_PV(it):
        qr, h, ws, j0, nw, qtok, e0, qtb = its[it]
        ntile = nw + 2
        p_, bp_ = pT[it % 3], bpT[it % 3]
        pso = [3 + 2 * (qr % 2), 4 + 2 * (qr % 2)]
        po, bpo = P.psum[pso[h // 4]], P.bps[pso[h // 4]]
        oc = (h % 4) * 65
        for i in range(ntile):
            if i < nw:
                vt = j0 + i
                r0, r1 = 0, 128
                if ws % 2 == 1 and i == 0:
                    r0 = 64
                if ws % 2 == 1 and i == nw - 1:
                    r1 = 64
            else:
                vt = 16 + (i - nw)
                r0, r1 = 0, 128
            fw.mm(po[0:64, oc:oc + 65], p_[r0:r1, i * 64:(i + 1) * 64], V[r0:r1, vt, h, :],
                  i == 0, i == ntile - 1, [bp_, bV[vt]], [bpo])

    def att_FIN(qr, qtok, qtb):
        pso = [3 + 2 * (qr % 2), 4 + 2 * (qr % 2)]
        ob, bob = osb[qr % 2], bosb[qr % 2]
        for half in range(2):
            po, bpo = P.psum[pso[half]], P.bps[pso[half]]
            pv = po[0:64, 0:260].rearrange("p (h d) -> p h d", h=4)
            fw.op("vector", lambda e, pv=pv, half=half: e.reciprocal(rec[0:64, half * 4:half * 4 + 4], pv[:, :, 64]),
                  reads=[bpo], writes=[brec])
            rb = bass.AP(A.t32, A_rec_off[0] + half * 4, [[A.n, 64], [1, 4], [0, 64]])
            fw.tt("vector", ob[0:64, half * 256:(half + 1) * 256].rearrange("p (h d) -> p h d", h=4),
                  pv[:, :, 0:64], rb, ALU.mult, [bpo, brec], [bob])
        pt, bpt = P.psum[7], P.bps[7]
        for c in range(4):
            fw.mm(pt[:, c * 64:(c + 1) * 64], ob[0:64, c * 128:(c + 1) * 128], P.identb[0:64, 0:64], True, True,
                  [bob, P.bconst], [bpt])
        fw.copy("scalar", catT[:, 0:4, qtok:qtok + 64], pt[:, 0:256].rearrange("p (c t) -> p c t", c=4),
                [bpt], [bcat[qtb]])

    NA = len(its)
    LOOK = 2
    for i in range(min(LOOK, NA)):
        att_S(i)
    pend_fin = None
    for it in range(NA):
        if it + LOOK < NA:
            att_S(it + LOOK)
        att_SM(it)
        att_PV(it)
        if pend_fin is not None:
            att_FIN(*pend_fin)
            pend_fin = None
        if its[it][1] == 7:
            pend_fin = (its[it][0], its[it][5], its[it][7])
    if pend_fin is not None:
        att_FIN(*pend_fin)
    fw.barrier()
    A.release(mM)
    xT = A.f32(KC * T).rearrange("p (c t) -> p c t", c=KC)
    P.xT = xT
    P.bx = [Buf(f"xT{i}") for i in range(5)]
    mX = A.mark()
    P.xin = [A.f32(D), A.f32(D)]
    P.bxin = [Buf("xin0"), Buf("xin1")]
    P.xin_i = 0
    P.tps = [4, 5]
    P.tps_i = 0
    wo = [A.bf16(8 * 512).rearrange("p (k n) -> p k n", k=8) for _ in range(2)]
    bwo = [Buf("wo0"), Buf("wo1")]
    wov = I["mix_w_out"].rearrange("(k p) n -> p k n", p=128)
    for i in range(2):
        fw.dma(wo[i], wov[:, :, i * 512:(i + 1) * 512], writes=[bwo[i]], q="gpsimd")
    for tb in range(5):
        t0, tl = TBS[tb]
        s_ = 1 if tb == 4 else 0
        load_xT_block(P, tb, xT[:, :, t0:t0 + tl], P.bx[tb])
        for m in range(8):
            pb = m % 4
            ps, bps = P.psum[pb], P.bps[pb]
            for k in range(KC):
                fw.mm(ps[:, 0:tl], wo[m // 4][:, k, (m % 4) * 128:(m % 4 + 1) * 128], catT[:, k, t0:t0 + tl],
                      k == 0, k == KC - 1, [bwo[m // 4], bcat[tb]], [bps])
            fw.stt(xT[:, m, t0:t0 + tl], ps[:, 0:tl], mod_cols(P, l, 2, m, s_), xT[:, m, t0:t0 + tl], ALU.mult, ALU.add,
                   [bps, P.bmod], [P.bx[tb]])
    fw.barrier()
    A.release(mX)
    if P.tap == "xmix0":
        tap_out(P, xT.rearrange("p c t -> p (c t)"), KC * T, P.bx)
        return True
    scr = norm_scratch(P)
    for tb in range(5):
        t0, tl = TBS[tb]
        norm_mod_block(P, l, 1, tb, xT[:, :, t0:t0 + tl], P.bx[tb], scr)
    fw.barrier()
    A.release(mX)
    fb = ffn_buffers(P, T)
    ffn(P, l, fb, I["ffn_w1"], I["ffn_w3"], I["ffn_w2"], FFN_DIM, [(0, 1024), (1024, 1024), (2048, 256)], 5)
    fw.barrier()
    A.release(mX)
    if P.tap == "xffn0":
        tap_out(P, xT.rearrange("p c t -> p (c t)"), KC * T, P.bx)
        return True
    return False


def ffn_buffers(P, ntok):
    A = P.A
    fb = Prog()
    fb.w1p = [A.bf16(8 * 512).rearrange("p (k n) -> p k n", k=8) for _ in range(2)]
    fb.w3p = [A.bf16(8 * 512).rearrange("p (k n) -> p k n", k=8) for _ in range(2)]
    fb.w2p = [A.bf16(4 * D).rearrange("p (f d) -> p f d", f=4) for _ in range(2)]
    fb.bw = [[Buf(f"w{n}p{i}") for i in range(2)] for n in range(3)]
    fb.act = A.bf16(4 * ntok).rearrange("p (f t) -> p f t", f=4)
    fb.bact = [[Buf(f"act{f}_{h}") for h in range(3)] for f in range(4)]
    fb.s = [A.bf16(1024) for _ in range(2)]
    fb.bs = [Buf("s0"), Buf("s1")]
    fb.a = [A.bf16(1024) for _ in range(2)]
    fb.ba = [Buf("a0"), Buf("a1")]
    fb.yi = 0
    fb.si = 0
    return fb


def ffn(P, l, fb, w1, w3, w2, F, halves, ntb, gate=None, bgate=None, first=True, nxt=None):
    fw = P.fw
    nfg = (F + 511) // 512
    w1v = w1.rearrange("(k p) n -> p k n", p=128)
    w3v = w3.rearrange("(k p) n -> p k n", p=128)

    def load(fg, wset):
        w1v_, w3v_, w2_ = wset
        j = fb.ldi % 2
        fb.ldi += 1
        nc_ = min(512, w2_.shape[0] - fg * 512)
        fw.dma(fb.w1p[j][:, :, 0:nc_], w1v_[:, :, fg * 512:fg * 512 + nc_], writes=[fb.bw[0][j]], q="gpsimd")
        fw.dma(fb.w3p[j][:, :, 0:nc_], w3v_[:, :, fg * 512:fg * 512 + nc_], writes=[fb.bw[1][j]], q="gpsimd")
        fw.dma(fb.w2p[j][:, 0:nc_ // 128, :], w2_[fg * 512:fg * 512 + nc_, :].rearrange("(f p) d -> p f d", p=128),
               writes=[fb.bw[2][j]], q="gpsimd")
    me = (w1v, w3v, w2)
    if first:
        fb.ldi = 0
        fb.usei = 0
        load(0, me)
    for fg in range(nfg):
        if fg + 1 < nfg:
            load(fg + 1, me)
        elif nxt is not None:
            n1, n3, n2 = nxt
            load(0, (n1.rearrange("(k p) n -> p k n", p=128), n3.rearrange("(k p) n -> p k n", p=128), n2))
        j = fb.usei % 2
        fb.usei += 1
        ncol = min(512, F - fg * 512)
        nfc = ncol // 128
        for hi, (t0, tl) in enumerate(halves):
            nb = (tl + 511) // 512
            for fc in range(nfc):
                for (wp, bw, banks) in ((fb.w1p[j], fb.bw[0][j], (0, 1)), (fb.w3p[j], fb.bw[1][j], (2, 3))):
                    for k in range(KC):
                        for b in range(nb):
                            bl = min(512, tl - b * 512)
                            tb = min((t0 + b * 512) // 512, 4)
                            fw.mm(P.psum[banks[b]][:, 0:bl], wp[:, k, fc * 128:(fc + 1) * 128],
                                  P.hT[:, k, t0 + b * 512:t0 + b * 512 + bl], k == 0, k == KC - 1,
                                  [bw, P.bh[tb]], [P.bps[banks[b]]])
                sj = fb.si % 2
                fb.si += 1
                for b in range(nb):
                    bl = min(512, tl - b * 512)
                    fw.act(fb.s[sj][:, b * 512:b * 512 + bl], P.psum[b][:, 0:bl], AF.Silu, [P.bps[b]], [fb.bs[sj]])
                for b in range(nb):
                    bl = min(512, tl - b * 512)
                    a0 = t0 + b * 512
                    if gate is None:
                        fw.tt("vector", fb.act[:, fc, a0:a0 + bl], P.psum[2 + b][:, 0:bl], fb.s[sj][:, b * 512:b * 512 + bl],
                              ALU.mult, [P.bps[2 + b], fb.bs[sj]], [fb.bact[fc][hi]])
                    else:
                        fw.tt("vector", fb.a[sj][:, b * 512:b * 512 + bl], P.psum[2 + b][:, 0:bl],
                              fb.s[sj][:, b * 512:b * 512 + bl], ALU.mult, [P.bps[2 + b], fb.bs[sj]], [fb.ba[sj]])
                if gate is not None:
                    fw.tt("vector", fb.act[:, fc, t0:t0 + tl], fb.a[sj][:, 0:tl], gate[:, t0:t0 + tl], ALU.mult,
                          [fb.ba[sj], bgate], [fb.bact[fc][hi]])
        for m in range(8):
            for tb in range(ntb):
                t0, tl = TBS[tb]
                s_ = 1 if tb == 4 else 0
                hi = [i for i, (h0, hl) in enumerate(halves) if h0 <= t0 < h0 + hl][0]
                pb = 4 + fb.yi % 4
                fb.yi += 1
                ps, bps = P.psum[pb], P.bps[pb]
                for fc in range(nfc):
                    fw.mm(ps[:, 0:tl], fb.w2p[j][:, fc, m * 128:(m + 1) * 128], fb.act[:, fc, t0:t0 + tl],
                          fc == 0, fc == nfc - 1, [fb.bw[2][j], fb.bact[fc][hi]], [bps])
                fw.stt(P.xT[:, m, t0:t0 + tl], ps[:, 0:tl], mod_cols(P, l, 5, m, s_), P.xT[:, m, t0:t0 + tl],
                       ALU.mult, ALU.add, [bps, P.bmod], [P.bx[tb]])


def rev(ap):
    dims = [list(d) for d in ap.ap]
    st, n = dims[-1]
    dims[-1] = [-st, n]
    return bass.AP(ap.tensor, ap.offset + st * (n - 1), dims)


def s5_chunked(P):
    fw, A, I = P.fw, P.A, P.I
    hT = P.hT
    V_ = "vector"
    TWO_PI = 2.0 * math.pi
    TWO_PI_S = 6.2831845
    NCH = T // 8
    NL = L // 8
    bprm = Buf("s5prm")
    B1 = [bprm]
    ident, identb = P.ident, P.identb

    def AP(v, dims, off=0):
        return bass.AP(v.tensor, v.offset + off, [list(v.ap[0])] + [list(d_) for d_ in dims])

    U8 = A.bf16(64 * NCH).rearrange("p (g c) -> p g c", g=64)
    bU8 = [Buf(f"U8_{g}") for g in range(64)]
    mW = A.mark()
    wi = [A.bf16(8 * 512).rearrange("p (k n) -> p k n", k=8) for _ in range(2)]
    bwi = [Buf("wi0"), Buf("wi1")]
    wiv = I["ssm_w_in"].rearrange("(k p) n -> p k n", p=128)
    for i in range(2):
        fw.dma(wi[i], wiv[:, :, i * 512:(i + 1) * 512], writes=[bwi[i]], q="gpsimd")
    utok = A.bf16(8 * D).rearrange("p (g s x) -> p g s x", g=64, s=8)
    butok = Buf("utok")
    pi = 0
    for (c0, M) in ((0, 128), (128, 128), (256, 32)):
        for s_ in range(8):
            for half in range(2):
                pb = pi % 4
                pi += 1
                t_lo = c0 * 8 + s_
                tbs = sorted(set(min(tt // 512, 4) for tt in (t_lo, t_lo + 8 * (M - 1))))
                for k in range(KC):
                    fw.mm(P.psum[pb][0:M, :], hT[:, k, t_lo:t_lo + 8 * (M - 1) + 1:8], wi[half][:, k, :], k == 0, k == KC - 1,
                          [P.bh[tb] for tb in range(tbs[0], tbs[-1] + 1)] + [bwi[half]], [P.bps[pb]])
                fw.copy("scalar" if pi % 2 else "vector", utok[0:M, half * 32:(half + 1) * 32, s_, :],
                        P.psum[pb][0:M, :].rearrange("p (g x) -> p g x", x=16), [P.bps[pb]], [butok])
        for g in range(64):
            pb = 4 + g % 4
            fw.mm(P.psum[pb][:, 0:M], utok[0:M, g, :, :].rearrange("p s x -> p (s x)"), identb[0:M, 0:M], True, True,
                  [butok, P.bconst], [P.bps[pb]])
            fw.copy("scalar" if g % 2 else "vector", U8[:, g, c0:c0 + M], P.psum[pb][:, 0:M], [P.bps[pb]], [bU8[g]])
    fw.barrier()
    A.release(mW)
    pw = [A.bf16(2048).rearrange("p (m e) -> p m e", e=16) for _ in range(2)]
    bb = [A.bf16(2048).rearrange("p (m x) -> p m x", x=16) for _ in range(2)]
    ctm = [A.bf16(2048).rearrange("p (m x) -> p m x", x=16) for _ in range(3)]
    thp8 = A.f32(64).rearrange("p (d s) -> p d s", d=2)
    r8 = A.f32(64).rearrange("p (d s) -> p d s", d=2)
    dvec = A.f32(64)
    Wsel = [A.t16[:, 2 * (P.hT_off + t_ * 1152 + 1024):2 * (P.hT_off + t_ * 1152 + 1024) + 240] for t_ in range(8)]
    mask = [A.f32(128), A.f32(128)]
    iotaC = [A.f32(NCH), A.f32(NCH)]
    XLre = [[A.bf16(128) for _ in range(2)] for _ in range(2)]
    XLim = [[A.bf16(128) for _ in range(2)] for _ in range(2)]
    DLre = [[A.bf16(128) for _ in range(2)] for _ in range(2)]
    DLim = [[A.bf16(128) for _ in range(2)] for _ in range(2)]
    Mg = [[A.bf16(128) for _ in range(2)] for _ in range(2)]
    bMat = [[Buf(f"mat{s_}{g_}") for g_ in range(2)] for s_ in range(2)]
    ybf = A.bf16(8 * NL).rearrange("p (g c) -> p g c", g=8)
    bybf = [Buf(f"ybf{g}") for g in range(8)]
    mW = A.mark()
    a_nat = A.f32(512).rearrange("p (i c) -> p i c", i=4)
    aT = A.f32(256).rearrange("p (i g) -> p i g", i=4)
    for ai, nm in enumerate(("ssm_a_re", "ssm_a_im")):
        for d in range(2):
            for rep in range(2):
                fw.dma(a_nat[0:64, ai * 2 + d, rep * 64:(rep + 1) * 64], I[nm][d], writes=[bprm])
    for idx in range(4):
        fw.tr(P.psum[idx][:, 0:64], a_nat[0:64, idx, :], ident[0:64, 0:64], B1 + [P.bconst], [P.bps[idx]])
        fw.copy(V_, aT[:, idx, :], P.psum[idx][:, 0:64], [P.bps[idx]], B1)
    are = aT[:, 0:2, :].rearrange("p d g -> p (d g)")
    aim = aT[:, 2:4, :].rearrange("p d g -> p (d g)")
    ldtb = A.f32(128)
    fw.dma(ldtb, bass.AP(I["ssm_log_dt"].tensor, 0, [[0, 128], [1, 128]]), writes=[bprm])
    dtb = A.f32(128); xre = A.f32(128); thp = A.f32(128)
    fw.act(dtb, ldtb, AF.Exp, B1, B1)
    fw.tt(V_, xre, are, dtb, ALU.mult, B1, B1)
    fw.tt(V_, thp, aim, dtb, ALU.mult, B1, B1)
    fw.ts(V_, thp, thp, 1.0 / TWO_PI, None, ALU.mult, None, B1, B1)
    evec = A.f32(16)
    fw.op("gpsimd", lambda e: e.iota(evec, [[1, 16]], base=-7, channel_multiplier=0,
                                     allow_small_or_imprecise_dtypes=True), writes=B1)
    ho = P.hT_off
    ang = A.t32[:, ho:ho + 2048]; ki = A.ti32[:, ho + 2048:ho + 4096]
    kf = A.t32[:, ho + 4096:ho + 6144]; mexp = A.t32[:, ho + 6144:ho + 8192]
    ang3 = ang.rearrange("p (m e) -> p m e", e=16)
    mexp3 = mexp.rearrange("p (m e) -> p m e", e=16)
    fw.tt(V_, ang3, AP(thp, [[1, 128], [0, 16]]), AP(evec, [[0, 128], [1, 16]]), ALU.mult, B1, B1)
    fw.tt(V_, mexp3, AP(xre, [[1, 128], [0, 16]]), AP(evec, [[0, 128], [1, 16]]), ALU.mult, B1, B1)
    fw.copy(V_, ki, ang, B1, B1)
    fw.copy(V_, kf, ki, B1, B1)
    fw.tt(V_, ang, ang, kf, ALU.subtract, B1, B1)
    fw.act(kf, ang, AF.Sin, B1, B1, scale=TWO_PI_S)
    fw.act(ang, ang, AF.Abs, B1, B1)
    fw.act(ang, ang, AF.Sin, B1, B1, scale=-TWO_PI, bias=P.halfpi[:, 0:1])
    fw.act(mexp, mexp, AF.Exp, B1, B1)
    fw.tt(V_, pw[0].rearrange("p m e -> p (m e)"), mexp, ang, ALU.mult, B1, B1)
    fw.tt(V_, pw[1].rearrange("p m e -> p (m e)"), mexp, kf, ALU.mult, B1, B1)
    lr1 = A.f32(128); li1 = A.f32(128); nrm = A.f32(128); ta = A.f32(128); tb_ = A.f32(128)
    kre = A.f32(128); kim = A.f32(128)
    kf3 = kf.rearrange("p (m e) -> p m e", e=16)
    fw.tt(V_, lr1, mexp3[:, :, 8], ang3[:, :, 8], ALU.mult, B1, B1)
    fw.ts(V_, lr1, lr1, -1.0, None, ALU.add, None, B1, B1)
    fw.tt(V_, li1, mexp3[:, :, 8], kf3[:, :, 8], ALU.mult, B1, B1)
    fw.tt(V_, nrm, are, are, ALU.mult, B1, B1)
    fw.tt(V_, ta, aim, aim, ALU.mult, B1, B1)
    fw.tt(V_, nrm, nrm, ta, ALU.add, B1, B1)
    fw.op(V_, lambda e: e.reciprocal(nrm, nrm), reads=B1, writes=B1)
    fw.tt(V_, ta, lr1, are, ALU.mult, B1, B1)
    fw.tt(V_, tb_, li1, aim, ALU.mult, B1, B1)
    fw.tt(V_, ta, ta, tb_, ALU.add, B1, B1)
    fw.tt(V_, kre, ta, nrm, ALU.mult, B1, B1)
    fw.tt(V_, ta, li1, are, ALU.mult, B1, B1)
    fw.tt(V_, tb_, lr1, aim, ALU.mult, B1, B1)
    fw.tt(V_, ta, ta, tb_, ALU.subtract, B1, B1)
    fw.tt(V_, kim, ta, nrm, ALU.mult, B1, B1)
    bn = [A.t32[:, ho + 2048:ho + 4096].rearrange("p (m x) -> p m x", x=16),
          A.t32[:, ho + 4096:ho + 6144].rearrange("p (m x) -> p m x", x=16)]
    for ri, nm in enumerate(("ssm_b_re", "ssm_b_im")):
        srcb = I[nm].rearrange("d g p x -> p (d g) x")
        for rep in range(2):
            for q8 in range(8):
                fw.dma(bn[ri][rep * 64:(rep + 1) * 64, q8 * 16:(q8 + 1) * 16, :], srcb[:, q8 * 16:(q8 + 1) * 16, :], writes=[bprm])
    t3 = ang3
    t4 = mexp3
    kreb = AP(kre, [[1, 128], [0, 16]])
    kimb = AP(kim, [[1, 128], [0, 16]])
    fw.tt(V_, t3, bn[0], kreb, ALU.mult, B1, B1)
    fw.tt(V_, t4, bn[1], kimb, ALU.mult, B1, B1)
    fw.tt(V_, t3, t3, t4, ALU.subtract, B1, B1)
    fw.copy(V_, bb[0][0:64], t3[0:64], B1, B1)
    fw.copy(V_, bb[1][64:128], t3[64:128], B1, B1)
    fw.tt(V_, t3, bn[1], kreb, ALU.mult, B1, B1)
    fw.tt(V_, t4, bn[0], kimb, ALU.mult, B1, B1)
    fw.tt(V_, t3, t3, t4, ALU.add, B1, B1)
    fw.copy(V_, bb[0][64:128], t3[64:128], B1, B1)
    fw.ts(V_, bb[1][0:64], t3[0:64], -1.0, None, ALU.mult, None, B1, B1)
    Cn = A.t32[:, ho:ho + 2048].rearrange("p (m c) -> p m c", m=16)
    for ri, nm in enumerate(("ssm_c_re", "ssm_c_im")):
        src = I[nm].rearrange("d g q p -> (d g q) p").rearrange("(m r) p -> r m p", r=128)
        for dup in range(2):
            for hm in range(2):
                fw.dma(Cn[:, hm * 8:(hm + 1) * 8, dup * 64:(dup + 1) * 64], src[:, hm * 8:(hm + 1) * 8, :], writes=[bprm])
        cv = A.t16[:, 2 * (ho + 4096 + 1024 * ri):2 * (ho + 4096 + 1024 * ri) + 2048]
        for mm_ in range(16):
            pb = mm_ % 4
            fw.tr(P.psum[pb][:, 0:128], Cn[:, mm_, :], ident, B1 + [P.bconst], [P.bps[pb]])
            fw.copy("scalar", cv[:, mm_ * 128:(mm_ + 1) * 128], P.psum[pb][:, 0:128], [P.bps[pb]], B1)
    cre_ = A.t16[:, 2 * (ho + 4096):2 * (ho + 4096) + 2048]
    cim_ = A.t16[:, 2 * (ho + 5120):2 * (ho + 5120) + 2048]
    cA, cB, cC = (ctm[i].rearrange("p m x -> p (m x)") for i in range(3))
    fw.copy(V_, cA[0:64], cre_[0:64], B1, B1)
    fw.ts(V_, cA[64:128], cim_[64:128], -1.0, None, ALU.mult, None, B1, B1)
    fw.ts(V_, cB[0:64], cim_[0:64], -1.0, None, ALU.mult, None, B1, B1)
    fw.ts(V_, cB[64:128], cre_[64:128], -1.0, None, ALU.mult, None, B1, B1)
    fw.ts(V_, cC[0:64], cre_[0:64], -1.0, None, ALU.mult, None, B1, B1)
    fw.copy(V_, cC[64:128], cre_[64:128], B1, B1)
    rows = A.f32(128)
    prm = A.f32(128)
    prm4 = prm.rearrange("p (a d s) -> p a d s", a=2, d=2)
    ldt = A.f32(64).rearrange("p (d s) -> p d s", d=2)
    for a_i, nm in enumerate(("ssm_a_re", "ssm_a_im")):
        for d in range(2):
            r0 = (a_i * 2 + d) * 32
            fw.dma(rows[r0:r0 + 32, :], I[nm][d].rearrange("g p -> (g p)").rearrange("(s q) -> s q", q=128), writes=[bprm])
    fw.tr(P.psum[4][:, 0:128], rows, ident, B1 + [P.bconst], [P.bps[4]])
    fw.copy(V_, prm, P.psum[4][:, 0:128], [P.bps[4]], B1)
    dt2 = A.f32(64).rearrange("p (d s) -> p d s", d=2)
    ldv = ldtb.rearrange("p (d s two) -> p d s two", d=2, two=2)
    for gl in range(2):
        fw.act(dt2[gl * 64:(gl + 1) * 64], ldv[gl * 64:(gl + 1) * 64, :, :, gl], AF.Exp, B1, B1)
    fw.tt(V_, r8, prm4[:, 0], dt2, ALU.mult, B1, B1)
    fw.act(r8, r8, AF.Exp, B1, B1, scale=8.0)
    fw.tt(V_, thp8, prm4[:, 1], dt2, ALU.mult, B1, B1)
    fw.ts(V_, thp8, thp8, 8.0 / TWO_PI, None, ALU.mult, None, B1, B1)
    dnat = A.f32(16)
    dT = A.f32(64)
    fw.dma(dnat[0:64, :], I["ssm_d"].rearrange("(g q) -> g q", q=16), writes=[bprm])
    fw.tr(P.psum[5][0:16, 0:64], dnat[0:64, :], ident[0:64, 0:64], B1 + [P.bconst], [P.bps[5]])
    fw.copy(V_, dT[0:16, :], P.psum[5][0:16, 0:64], [P.bps[5]], B1)
    for t_ in range(8):
        fw.dma(dvec[16 * t_:16 * t_ + 16, :], dT[0:16, :], reads=B1, writes=[bprm])
    for t_ in range(8):
        fw.memset(V_, Wsel[t_], 0.0, B1)
        fw.copy(V_, Wsel[t_][:, 112:128], identb[:, 16 * t_:16 * t_ + 16], B1 + [P.bconst], B1)
    rbi = A.i32(1); cbi = A.i32(128); rbf = A.f32(1); cbf = A.f32(128)
    fw.op("gpsimd", lambda e: e.iota(rbi, [[0, 1]], base=0, channel_multiplier=1), writes=B1)
    fw.op("gpsimd", lambda e: e.iota(cbi, [[1, 128]], base=0, channel_multiplier=0), writes=B1)
    fw.ts(V_, rbi, rbi, 4, None, ALU.arith_shift_right, None, B1, B1)
    fw.ts(V_, cbi, cbi, 4, None, ALU.arith_shift_right, None, B1, B1)
    fw.copy(V_, rbf, rbi, B1, B1)
    fw.copy(V_, cbf, cbi, B1, B1)
    fw.ts(V_, mask[0], cbf, rbf[:, 0:1], None, ALU.is_ge, None, B1, B1)
    fw.ts(V_, mask[1], cbf, rbf[:, 0:1], None, ALU.is_le, None, B1, B1)
    iop = dict(channel_multiplier=0, allow_small_or_imprecise_dtypes=True)
    fw.op("gpsimd", lambda e: e.iota(iotaC[0][:, 0:NL], [[1, NL]], base=32, **iop), writes=B1)
    fw.op("gpsimd", lambda e: e.iota(iotaC[0][:, NL:NCH], [[1, 32]], base=0, **iop), writes=B1)
    fw.op("gpsimd", lambda e: e.iota(iotaC[1][:, 0:NL], [[-1, NL]], base=NCH - 1, **iop), writes=B1)
    fw.op("gpsimd", lambda e: e.iota(iotaC[1][:, NL:NCH], [[-1, 32]], base=31, **iop), writes=B1)
    for bufl in (XLre, XLim, DLre, DLim):
        for s_ in range(2):
            for gl in range(2):
                fw.memset(V_, bufl[s_][gl], 0.0, [bMat[s_][gl]])
    fw.barrier()
    A.release(mW)
    LRP = [(A.f32(128), A.f32(128), A.f32(128)) for _ in range(2)]
    bLRP = [(Buf("Lm0"), Buf("Rm0"), Buf("Pm0")), (Buf("Lm1"), Buf("Rm1"), Buf("Pm1"))]
    c1 = A.f32(128); c2 = A.f32(128)
    bc12 = Buf("c12")
    TAU = A.f32(NCH); KI = A.i32(NCH); KF = A.f32(NCH); TA = A.f32(NCH); TB_ = A.f32(NCH)
    Vr = A.f32(NCH); Vi = A.f32(NCH); Gr = A.f32(NCH); Gi = A.f32(NCH)
    Sp = [A.bf16(NL), A.bf16(NL)]
    gtmp = A.f32(NL); gt2 = A.f32(NL)
    bTAB, bKI, bSIN, bTA, bTBb, bVr, bVi, bGr, bGi = (Buf(n) for n in ("tab", "ki", "sin", "ta", "tb", "vr", "vi", "gr", "gi"))
    bSp = [Buf("spre"), Buf("spim")]
    bgt = Buf("gtmp")
    COS, SIN = TAU, KF
    gen_i = [0]
    out_i = [0]

    c3 = A.f32(128); c4 = A.f32(128)
    bc34 = Buf("c34")
    TOEP = globals().get("S5_TOEP", True)
    Esh = c3.tensor[:, 0:1]
    if TOEP:
        cbase = A.last_off - 128
        Esh = A.t16[:, 2 * cbase:2 * cbase + 352]
        cst32 = A.t32[:, cbase + 176:cbase + 192]
        kst = [A.t16[:, 2 * (cbase + 192):2 * (cbase + 192) + 16], A.t16[:, 2 * (cbase + 200):2 * (cbase + 200) + 16]]
        bE = Buf("Esh"); bcst = Buf("cst32"); bkst = [Buf("kst0"), Buf("kst1")]
        fw.memset(V_, Esh, 0.0, [bE])
        fw.copy(V_, Esh[:, 112:240], identb, [P.bconst], [bE])

    def cmul(eng, out, rows, mg, tab, e0, es, VA, VB):
        r0, r1 = rows
        pr_ = pw[0][r0:r1, mg, :]
        pi_ = pw[1][r0:r1, mg, :]
        va_ = VA[r0:r1, mg, :]
        vb_ = VB[r0:r1, mg, :]
        pe = lambda v: AP(v, [[es, 8], [0, 16]], off=e0)
        vb = lambda v: AP(v, [[0, 8], [1, 16]])
        o3 = out[r0:r1, :].rearrange("p (i x) -> p i x", x=16)
        s1, s2, bs_ = (c1, c2, bc12) if eng == "vector" else (LRP[0][0], LRP[0][1], bc34)
        a1 = s1[r0:r1, :].rearrange("p (i x) -> p i x", x=16)
        a2 = s2[r0:r1, :].rearrange("p (i x) -> p i x", x=16)
        fw.tt(eng, a1, pe(pr_), vb(va_), ALU.mult, B1, [bs_])
        fw.tt(eng, a2, pe(pi_), vb(vb_), ALU.mult, B1, [bs_])
        fw.tt(eng, o3, a1, a2, ALU.add, [bs_], tab)

    def gen(it):
        gp, d = it // 2, it % 2
        st_ = it % 2
        g0 = 2 * gp
        eL = (7, -1) if d == 0 else (0, 1)
        eR = (7, 1) if d == 0 else (14, -1)
        eD = (8, 1) if d == 0 else (15, -1)
        eP = (14, -1) if d == 0 else (7, 1)
        for gl in range(2):
            g = g0 + gl
            mg = d * 64 + g
            q_ = gen_i[0] % 2
            Lm_, Rm_, Pm_ = LRP[q_]
            bL_, bR_, bP_ = bLRP[q_]
            ALLR = (0, 128)
            if not TOEP:
                cmul(V_, Lm_, ALLR, mg, [bL_], eL[0], eL[1], bb[0], bb[1])
                cmul(V_, Rm_, ALLR, mg, [bR_], eR[0], eR[1], ctm[0], ctm[1])
            cmul("gpsimd" if globals().get("S5_POOL_D", True) else V_, Pm_, ALLR, mg, [bP_], eP[0], eP[1], bb[0], bb[1])
            rws = (gl * 64, gl * 64 + 64)
            bm = bMat[st_][gl]
            DE = "gpsimd" if globals().get("S5_POOL_D", True) else V_
            if gl == 0:
                cmul(DE, DLre[st_][gl], rws, mg, [bm], eD[0], eD[1], ctm[0], ctm[1])
                cmul(DE, DLim[st_][gl], rws, mg, [bm], eD[0], eD[1], ctm[1], ctm[2])
            else:
                cmul(DE, DLre[st_][gl], rws, mg, [bm], eD[0], eD[1], ctm[2], ctm[0])
                cmul(DE, DLim[st_][gl], rws, mg, [bm], eD[0], eD[1], ctm[0], ctm[1])
            pbm = 4 + gen_i[0] % 2
            gen_i[0] += 1
            if not TOEP:
                fw.mm(P.psum[pbm][:, 0:128], Lm_, Rm_, True, True, [bL_, bR_], [P.bps[pbm]])
                fw.tr(P.psum[pbm][:, 128:256], Pm_, ident, [bP_, P.bconst], [P.bps[pbm]])
                fw.tt(V_, Mg[st_][gl], P.psum[pbm][:, 0:128], mask[d], ALU.mult, [P.bps[pbm]] + B1, [bm])
            else:
                kq = gen_i[0] % 2
                fw.act(cst32, ctm[0][:, mg, :], AF.Identity, B1, [bcst])
                fw.mm(P.psum[pbm][:, 256:272], Pm_, cst32, True, True, [bP_, bcst], [P.bps[pbm]])
                fw.tr(P.psum[pbm][:, 128:256], Pm_, ident, [bP_, P.bconst], [P.bps[pbm]])
                fw.act(kst[kq], P.psum[pbm][:, 256:272], AF.Identity, [P.bps[pbm]], [bkst[kq]])
                for t_ in range(8):
                    off = (112 + 16 * (7 - t_)) if d == 0 else (112 - 16 * t_)
                    fw.mm(P.psum[pbm][:, t_ * 16:(t_ + 1) * 16], Esh[:, off:off + 128], kst[kq], True, True,
                          [bE, bkst[kq]], [P.bps[pbm]])
                fw.act(Mg[st_][gl], P.psum[pbm][:, 0:128], AF.Identity, [P.bps[pbm]], [bm])
            fw.copy("scalar", XLre[st_][gl][:, gl * 64:gl * 64 + 64], P.psum[pbm][:, 128:192], [P.bps[pbm]], [bm])
            fw.copy("scalar", XLim[st_][gl][:, gl * 64:gl * 64 + 64], P.psum[pbm][:, 192:256], [P.bps[pbm]], [bm])

    def xmm(it):
        gp, d = it // 2, it % 2
        st_ = it % 2
        g0 = 2 * gp
        for gl in range(2):
            fw.mm(P.psum[2][:, 0:NCH], XLre[st_][gl], U8[:, g0 + gl, :], gl == 0, gl == 1,
                  [bMat[st_][gl], bU8[g0 + gl]], [P.bps[2]])
        for gl in range(2):
            fw.mm(P.psum[3][:, 0:NCH], XLim[st_][gl], U8[:, g0 + gl, :], gl == 0, gl == 1,
                  [bMat[st_][gl], bU8[g0 + gl]], [P.bps[3]])

    def scan(it):
        gp, d = it // 2, it % 2
        thc = thp8[:, d, gp:gp + 1]
        rcol = r8[:, d, gp:gp + 1]
        fw.ts(V_, TAU, iotaC[d], thc, None, ALU.mult, None, B1, [bTAB])
        fw.copy(V_, KI, TAU, [bTAB], [bKI])
        fw.copy(V_, KF, KI, [bKI], [bSIN])
        fw.tt(V_, TAU, TAU, KF, ALU.subtract, [bSIN], [bTAB])
        fw.act(SIN, TAU, AF.Sin, [bTAB], [bSIN], scale=TWO_PI_S)
        fw.act(TAU, TAU, AF.Abs, [], [bTAB])
        fw.act(COS, TAU, AF.Sin, [], [bTAB], scale=-TWO_PI, bias=P.halfpi[:, 0:1])
        xr, xi = P.psum[2][:, 0:NCH], P.psum[3][:, 0:NCH]
        fw.tt(V_, TA, xr, COS, ALU.mult, [P.bps[2], bTAB], [bTA])
        fw.tt(V_, TB_, xi, SIN, ALU.mult, [P.bps[3], bSIN], [bTBb])
        fw.tt(V_, Vr, TA, TB_, ALU.add, [bTA, bTBb], [bVr])
        fw.tt(V_, TA, xi, COS, ALU.mult, [P.bps[3], bTAB], [bTA])
        fw.tt(V_, TB_, xr, SIN, ALU.mult, [P.bps[2], bSIN], [bTBb])
        fw.tt(V_, Vi, TA, TB_, ALU.subtract, [bTA, bTBb], [bVi])
        for (Vx, bVx, Gx, bGx) in ((Vr, bVr, Gr, bGr), (Vi, bVi, Gi, bGi)):
            segA = (lambda v: v[:, NL:NCH]) if d == 0 else (lambda v: rev(v[:, NL:NCH]))
            segB = (lambda v: v[:, 0:NL]) if d == 0 else (lambda v: rev(v[:, 0:NL]))
            lastA = NCH - 1 if d == 0 else NL
            rbA = AP(rcol, [[0, 32]])
            rbB = AP(rcol, [[0, NL]])
            fw.op(V_, lambda e, o=segA(Gx), r_=rbA, v=segA(Vx): e.tensor_tensor_scan(o, r_, v, 0.0, ALU.mult, ALU.add),
                  reads=[bVx] + B1, writes=[bGx])
            fw.op(V_, lambda e, o=segB(Gx), r_=rbB, v=segB(Vx), i_=Gx[:, lastA:lastA + 1]:
                  e.tensor_tensor_scan(o, r_, v, i_, ALU.mult, ALU.add), reads=[bVx] + B1, writes=[bGx])
        if d == 0:
            pieces = ((slice(1, NL), slice(0, NL - 1)), (slice(0, 1), slice(NCH - 1, NCH)))
        else:
            pieces = ((slice(0, NL - 1), slice(1, NL)), (slice(NL - 1, NL), slice(NL, NL + 1)))
        fw.tt(V_, TA, Gr, COS, ALU.mult, [bGr, bTAB], [bTA])
        fw.tt(V_, TB_, Gi, SIN, ALU.mult, [bGi, bSIN], [bTBb])
        for (do, so) in pieces:
            fw.tt(V_, Sp[0][:, do], TA[:, so], TB_[:, so], ALU.subtract, [bTA, bTBb], [bSp[0]])
        fw.tt(V_, TA, Gr, SIN, ALU.mult, [bGr, bSIN], [bTA])
        fw.tt(V_, TB_, Gi, COS, ALU.mult, [bGi, bTAB], [bTBb])
        for (do, so) in pieces:
            fw.tt(V_, Sp[1][:, do], TA[:, so], TB_[:, so], ALU.add, [bTA, bTBb], [bSp[1]])

    def ymm(it):
        gp, d = it // 2, it % 2
        st_ = it % 2
        g0 = 2 * gp
        for gl in range(2):
            g = g0 + gl
            bm = bMat[st_][gl]
            fw.mm(P.psum[gl][:, 0:NL], Mg[st_][gl], U8[:, g, 0:NL], d == 0, False, [bm, bU8[g]], [P.bps[gl]])
            fw.mm(P.psum[gl][:, 0:NL], DLre[st_][gl], Sp[0], False, False, [bm, bSp[0]], [P.bps[gl]])
            fw.mm(P.psum[gl][:, 0:NL], DLim[st_][gl], Sp[1], False, d == 1, [bm, bSp[1]], [P.bps[gl]])

    def epi(gp):
        g0 = 2 * gp
        oc = gp // 4
        for gl in range(2):
            g = g0 + gl
            fw.stt(gtmp, U8[:, g, 0:NL], dvec[:, g:g + 1], P.psum[gl][:, 0:NL], ALU.mult, ALU.add,
                   [P.bps[gl], bU8[g]] + B1, [bgt])
            fw.act(gt2, gtmp, AF.Square, [bgt], [bgt])
            fw.ts(V_, gt2, gt2, 0.044715, 1.0, ALU.mult, ALU.add, [bgt], [bgt])
            fw.tt(V_, gt2, gt2, gtmp, ALU.mult, [bgt], [bgt])
            fw.act(gt2, gt2, AF.Sigmoid, [bgt], [bgt], scale=2.0 * math.sqrt(2.0 / math.pi))
            fw.tt(V_, ybf[:, g % 8, :], gt2, gtmp, ALU.mult, [bgt], [bybf[g % 8]])
        if gp % 4 == 3:
            for t_ in range(8):
                pbo = 6 + out_i[0] % 2
                out_i[0] += 1
                for gg in range(8):
                    fw.mm(P.psum[pbo][:, 0:NL], Wsel[t_][:, 112 - 16 * gg:240 - 16 * gg], ybf[:, gg, :], gg == 0, gg == 7,
                          [bybf[gg]] + B1, [P.bps[pbo]])
                fw.copy("scalar", hT[:, oc, t_:L - 7 + t_:8], P.psum[pbo][:, 0:NL], [P.bps[pbo]],
                        [P.bh[0], P.bh[1], P.bh[2], P.bh[3]])

    def record(fn, *a):
        calls = []
        real = fw.op
        fw.op = lambda *aa, **kk: calls.append((aa, kk))
        try:
            fn(*a)
        finally:
            fw.op = real
        return calls

    def replay_interleaved(ca, cb):
        na, nb = len(ca), len(cb)
        ia = ib = 0
        while ia < na or ib < nb:
            if ib >= nb or (ia < na and ia * max(nb, 1) <= ib * max(na, 1)):
                aa, kk = ca[ia]; ia += 1
            else:
                aa, kk = cb[ib]; ib += 1
            fw.op(*aa, **kk)

    def merge(ca, cb):
        out = []
        na, nb = len(ca), len(cb)
        ia = ib = 0
        while ia < na or ib < nb:
            if ib >= nb or (ia < na and ia * max(nb, 1) <= ib * max(na, 1)):
                out.append(ca[ia]); ia += 1
            else:
                out.append(cb[ib]); ib += 1
        return out

    NIT = 64
    gen(0)
    pend_epi = []
    for it in range(NIT):
        xmm(it)
        cg = record(gen, it + 1) if it + 1 < NIT else []
        cs = record(scan, it)
        if globals().get("S5_INTERLEAVE", True):
            for aa, kk in merge(merge(cs, cg), pend_epi):
                fw.op(*aa, **kk)
        else:
            for aa, kk in pend_epi + cg + cs:
                fw.op(*aa, **kk)
        pend_epi = []
        ymm(it)
        if it % 2 == 1:
            if it + 1 < NIT and globals().get("S5_EPI_DEFER", True):
                pend_epi = record(epi, it // 2)
            else:
                epi(it // 2)
    fw.barrier()


def layer1(P):
    fw, A, I = P.fw, P.A, P.I
    l = 1
    PCE = "gpsimd" if globals().get("USE_POOL", False) else "vector"
    if not hasattr(P, "epsc"):
        P.epsc = A.f32(1)
        fw.memset("vector", P.epsc, EPS, [P.bconst])
    if P.xT is None:
        P.xT = A.f32(KC * T).rearrange("p (c t) -> p c t", c=KC)
        P.bx = [Buf(f"xT{i}") for i in range(5)]
        m_ = A.mark()
        P.xin = [A.f32(D), A.f32(D)]
        P.bxin = [Buf("xin0"), Buf("xin1")]
        P.xin_i = 0
        P.tps = [4, 5]
        P.tps_i = 0
        for tb in range(5):
            t0, tl = TBS[tb]
            load_xT_block(P, tb, P.xT[:, :, t0:t0 + tl], P.bx[tb])
        fw.barrier()
        A.release(m_)
    xT = P.xT
    mX = A.mark()
    hT = P.hT
    scr = norm_scratch(P)
    for tb in range(5):
        t0, tl = TBS[tb]
        norm_mod_block(P, l, 0, tb, xT[:, :, t0:t0 + tl], P.bx[tb], scr)
    fw.barrier()
    A.release(mX)
    if globals().get("S5_MODE", "chunk") == "chunk":
        s5_chunked(P)
        A.release(mX)
        if P.tap == "y1":
            tap_bf16(P, hT.rearrange("p c t -> p (c t)"), KC * T, P.bh)
            return True
        return layer1_tail(P, mX)
    wi = [A.bf16(8 * 512).rearrange("p (k n) -> p k n", k=8) for _ in range(2)]
    bwi = [Buf("wi0"), Buf("wi1")]
    wiv = I["ssm_w_in"].rearrange("(k p) n -> p k n", p=128)
    for i in range(2):
        fw.dma(wi[i], wiv[:, :, i * 512:(i + 1) * 512], writes=[bwi[i]], q="gpsimd")
    for tb in range(5):
        t0, tl = TBS[tb]
        for m in range(8):
            for k in range(KC):
                fw.mm(P.psum[m][:, 0:tl], wi[m // 4][:, k, (m % 4) * 128:(m % 4 + 1) * 128], hT[:, k, t0:t0 + tl],
                      k == 0, k == KC - 1, [bwi[m // 4], P.bh[tb]], [P.bps[m]])
        for m in range(8):
            fw.copy("scalar" if m % 2 else "vector", hT[:, m, t0:t0 + tl], P.psum[m][:, 0:tl], [P.bps[m]], [P.bh[tb]])
    fw.barrier()
    A.release(mX)
    uT = hT
    TWO_PI = 2.0 * math.pi
    TWO_PI_S = 6.2831845
    bprm = Buf("s5prm")
    prm = A.f32(128)
    prm4 = prm.rearrange("p (a d s) -> p a d s", a=2, d=2)
    ldt = A.f32(64).rearrange("p (d s) -> p d s", d=2)
    thp = A.f32(64).rearrange("p (d s) -> p d s", d=2)
    th2 = A.f32(64).rearrange("p (d s) -> p d s", d=2)
    rr_ = A.f32(64).rearrange("p (d s) -> p d s", d=2)
    kap = A.f32(128).rearrange("p (a d s) -> p a d s", a=2, d=2)
    dcol = A.f32(8)
    Bl = [A.bf16(16 * 128).rearrange("p (m c) -> p m c", m=16) for _ in range(2)]
    LT = [A.bf16(16 * 128).rearrange("p (m c) -> p m c", m=16) for _ in range(2)]
    LTm = [[A.bf16(128) for _ in range(4)] for _ in range(2)]
    bLTm = [[Buf(f"LTm{ri}{pr}") for pr in range(4)] for ri in range(2)]
    BlZ = [[A.bf16(128) for _ in range(4)] for _ in range(2)]
    bBlZ = [[Buf(f"BlZ{ri}{pr}") for pr in range(4)] for ri in range(2)]
    JH = 1152
    iotaJ = A.f32(JH)
    st2 = A.f32(2)
    mS = A.mark()
    rows = A.f32(128)
    for a_i, nm in enumerate(("ssm_a_re", "ssm_a_im")):
        for d in range(2):
            r0 = (a_i * 2 + d) * 32
            fw.dma(rows[r0:r0 + 32, :], I[nm][d].rearrange("g p -> (g p)").rearrange("(s q) -> s q", q=128), writes=[bprm])
    fw.tr(P.psum[0][:, 0:128], rows, P.ident, [bprm, P.bconst], [P.bps[0]])
    fw.copy("vector", prm, P.psum[0][:, 0:128], [P.bps[0]], [bprm])
    ld_t = I["ssm_log_dt"].tensor
    for gl in range(2):
        fw.dma(ldt[gl * 64:(gl + 1) * 64, :, :], bass.AP(ld_t, gl, [[0, 64], [64, 2], [2, 32]]), writes=[bprm], **SLOW)
    fw.dma(dcol, I["ssm_d"].rearrange("(c p) -> p c", p=128), writes=[bprm], **SLOW)
    fw.op("gpsimd", lambda e: e.iota(iotaJ, [[1, JH]], base=0, channel_multiplier=0,
                                     allow_small_or_imprecise_dtypes=True), writes=[bprm])
    dt_ = A.f32(64).rearrange("p (d s) -> p d s", d=2)
    xre = A.f32(64).rearrange("p (d s) -> p d s", d=2)
    th = A.f32(64).rearrange("p (d s) -> p d s", d=2)
    ki = A.i32(64).rearrange("p (d s) -> p d s", d=2)
    kf = A.f32(64).rearrange("p (d s) -> p d s", d=2)
    sn = A.f32(64).rearrange("p (d s) -> p d s", d=2)
    cs_ = A.f32(64).rearrange("p (d s) -> p d s", d=2)
    lr = A.f32(64).rearrange("p (d s) -> p d s", d=2)
    li = A.f32(64).rearrange("p (d s) -> p d s", d=2)
    t_a = A.f32(64).rearrange("p (d s) -> p d s", d=2)
    t_b = A.f32(64).rearrange("p (d s) -> p d s", d=2)
    nrm = A.f32(64).rearrange("p (d s) -> p d s", d=2)
    V_ = "vector"
    B1 = [bprm]
    fw.act(dt_, ldt, AF.Exp, B1, B1)
    fw.tt(V_, xre, prm4[:, 0], dt_, ALU.mult, B1, B1)
    fw.tt(V_, th, prm4[:, 1], dt_, ALU.mult, B1, B1)
    fw.act(rr_, xre, AF.Exp, B1, B1)
    fw.ts(V_, thp, th, 1.0 / TWO_PI, None, ALU.mult, None, B1, B1)
    fw.ts(V_, th2, thp, float(JH), None, ALU.mult, None, B1, B1)
    fw.copy(V_, ki, thp, B1, B1)
    fw.copy(V_, kf, ki, B1, B1)
    fw.tt(V_, kf, thp, kf, ALU.subtract, B1, B1)
    fw.act(sn, kf, AF.Sin, B1, B1, scale=TWO_PI_S)
    fw.act(kf, kf, AF.Abs, B1, B1)
    fw.act(cs_, kf, AF.Sin, B1, B1, scale=-TWO_PI, bias=P.halfpi[:, 0:1])
    fw.tt(V_, lr, rr_, cs_, ALU.mult, B1, B1)
    fw.tt(V_, li, rr_, sn, ALU.mult, B1, B1)
    fw.ts(V_, lr, lr, -1.0, None, ALU.add, None, B1, B1)
    fw.tt(V_, nrm, prm4[:, 0], prm4[:, 0], ALU.mult, B1, B1)
    fw.tt(V_, t_a, prm4[:, 1], prm4[:, 1], ALU.mult, B1, B1)
    fw.tt(V_, nrm, nrm, t_a, ALU.add, B1, B1)
    fw.op(V_, lambda e: e.reciprocal(nrm, nrm), reads=B1, writes=B1)
    fw.tt(V_, t_a, lr, prm4[:, 0], ALU.mult, B1, B1)
    fw.tt(V_, t_b, li, prm4[:, 1], ALU.mult, B1, B1)
    fw.tt(V_, t_a, t_a, t_b, ALU.add, B1, B1)
    fw.tt(V_, kap[:, 0], t_a, nrm, ALU.mult, B1, B1)
    fw.tt(V_, t_a, li, prm4[:, 0], ALU.mult, B1, B1)
    fw.tt(V_, t_b, lr, prm4[:, 1], ALU.mult, B1, B1)
    fw.tt(V_, t_a, t_a, t_b, ALU.subtract, B1, B1)
    fw.tt(V_, kap[:, 1], t_a, nrm, ALU.mult, B1, B1)
    bn = [A.f32(64 * 16).rearrange("p (m x) -> p m x", m=64) for _ in range(2)]
    for ri, nm in enumerate(("ssm_b_re", "ssm_b_im")):
        fw.dma(bn[ri], I[nm].rearrange("d g p x -> (d g p) x").rearrange("(m q) x -> q m x", q=128), writes=[bprm])
    kap_off = A.last_off
    Nn = [A.f32(64 * 16).rearrange("p (m x) -> p m x", m=64) for _ in range(2)]
    tN = A.f32(64 * 16).rearrange("p (m x) -> p m x", m=64)

    def kb(a):
        v = kap[:, a].rearrange("p d s -> p (d s)")
        dims = [list(d_) for d_ in v.ap]
        return bass.AP(v.tensor, v.offset, dims + [[0, 16]])
    fw.tt(V_, Nn[0], bn[0], kb(0), ALU.mult, B1, B1)
    fw.tt(V_, tN, bn[1], kb(1), ALU.mult, B1, B1)
    fw.tt(V_, Nn[0], Nn[0], tN, ALU.subtract, B1, B1)
    fw.tt(V_, Nn[1], bn[1], kb(0), ALU.mult, B1, B1)
    fw.tt(V_, tN, bn[0], kb(1), ALU.mult, B1, B1)
    fw.tt(V_, Nn[1], Nn[1], tN, ALU.add, B1, B1)
    Zb = A.f32(64 * 32).rearrange("p (m x) -> p m x", m=64)
    for ri in range(2):
        fw.memset(V_, Zb, 0.0, B1)
        fw.copy(V_, Zb[0:64, :, 0:16], Nn[ri][0:64, :, :], B1, B1)
        fw.copy(V_, Zb[64:128, :, 16:32], Nn[ri][64:128, :, :], B1, B1)
        Zf = Zb.rearrange("p m x -> p (m x)")
        for mm_ in range(16):
            pb = mm_ % 4
            fw.tr(P.psum[pb][:, 0:128], Zf[:, mm_ * 128:(mm_ + 1) * 128], P.ident, B1 + [P.bconst], [P.bps[pb]])
            fw.copy("scalar", Bl[ri][:, mm_, :], P.psum[pb][:, 0:128], [P.bps[pb]], B1)
    msk = A.f32(128)
    fw.memset(V_, msk, 0.0, B1)
    for gg in range(8):
        h_ = gg % 2
        fw.memset(V_, msk[h_ * 64:(h_ + 1) * 64, gg * 16:(gg + 1) * 16], 1.0, B1)
    Cn = A.f32(16 * 128).rearrange("p (m c) -> p m c", m=16)
    for ri, nm in enumerate(("ssm_c_re", "ssm_c_im")):
        src = I[nm].rearrange("d g q p -> (d g q) p").rearrange("(m r) p -> r m p", r=128)
        for dup in range(2):
            fw.dma(Cn[:, :, dup * 64:(dup + 1) * 64], src, writes=[bprm])
        for mm_ in range(16):
            pb = mm_ % 4
            fw.tr(P.psum[pb][:, 0:128], Cn[:, mm_, :], P.ident, B1 + [P.bconst], [P.bps[pb]])
            if ri == 0:
                fw.tt(V_, LT[ri][:, mm_, :], P.psum[pb][:, 0:128], msk, ALU.mult, [P.bps[pb]] + B1, B1)
            else:
                fw.stt(LT[ri][:, mm_, :], P.psum[pb][:, 0:128], -1.0, msk, ALU.mult, ALU.mult, [P.bps[pb]] + B1, B1)
    for ri in range(2):
        for pr in range(4):
            fw.memset(V_, LTm[ri][pr], 0.0, [bLTm[ri][pr]])
            fw.memset(V_, BlZ[ri][pr], 0.0, [bBlZ[ri][pr]])
    if P.tap == "s5lt":
        for ri in range(2):
            fw.copy(V_, Zb.rearrange("p m x -> p (m x)"), LT[ri].rearrange("p m c -> p (m c)"), B1, B1)
            fw.dma(P.dbg[:, ri * 2048:(ri + 1) * 2048], Zb.rearrange("p m x -> p (m x)"), reads=B1, writes=[P.bdbg])
            fw.copy(V_, Zb.rearrange("p m x -> p (m x)"), Bl[ri].rearrange("p m c -> p (m c)"), B1, B1)
            fw.dma(P.dbg[:, 4096 + ri * 2048:4096 + (ri + 1) * 2048], Zb.rearrange("p m x -> p (m x)"), reads=B1, writes=[P.bdbg])
        return True
    fw.barrier()
    A.release(mS)
    NB = JH
    TAU = A.f32(NB); KI = A.i32(NB); KF = A.f32(NB)
    TA = A.f32(NB); TB_ = A.f32(NB)
    Vr = A.f32(NB); Vi = A.f32(NB); Gr = A.f32(NB); Gi = A.f32(NB)
    Hh = [A.bf16(L), A.bf16(L)]
    gtmp = A.f32(512); gt2 = A.f32(512)
    yacc = A.f32(L)
    byacc = [Buf(f"yacc{i}") for i in range(4)]
    bTAB, bKI, bSIN, bTA, bTBb, bVr, bVi, bGr, bGi = (Buf(n) for n in ("tab", "ki", "sin", "ta", "tb", "vr", "vi", "gr", "gi"))
    bH = [Buf("Hre"), Buf("Him")]
    bst2 = Buf("st2")
    bgt = Buf("gtmp")
    COS, SIN = TAU, KF

    def segs(d, hf):
        out = []
        j0 = hf * JH
        if d == 0:
            whole = [(L, NCTX, False, False)] + [(0, L, False, True)]
        else:
            whole = [(L, NCTX, True, False)] + [(0, L, True, True)]
        pos = 0
        for (a, n, rv, lat) in whole:
            lo, hi = max(pos, j0), min(pos + n, j0 + JH)
            if lo < hi:
                cnt = hi - lo
                off = lo - pos
                while cnt > 0:
                    c_ = min(512, cnt)
                    if not rv:
                        out.append((a + off, c_, lo - j0, False, lat))
                    else:
                        out.append((a + n - off - c_, c_, lo - j0, True, lat))
                    off += c_
                    lo += c_
                    cnt -= c_
            pos += n
        return out

    bui = 0
    for oc in range(8):
        for d in range(2):
            for pr in range(4):
                st = oc * 4 + pr
                m16 = d * 8 + oc
                for ri in range(2):
                    if globals().get("SKIPCP", False) and (oc, d, pr) != (0, 0, 0):
                        continue
                    fw.copy(PCE, LTm[ri][pr][:, pr * 32:(pr + 1) * 32], LT[ri][:, m16, pr * 32:(pr + 1) * 32],
                            [bprm], [bLTm[ri][pr]])
                    fw.copy(PCE, BlZ[ri][pr][pr * 32:(pr + 1) * 32, :], Bl[ri][pr * 32:(pr + 1) * 32, m16, :],
                            [bprm], [bBlZ[ri][pr]])
                if P.tap == "s5dbg5" and (oc, d, pr) == P.dbgsel[:3]:
                    for ri in range(2):
                        for p4 in range(4):
                            fw.copy(V_, gtmp[:, 0:128], LTm[ri][p4], [bLTm[ri][p4]], [bgt])
                            fw.dma(P.dbg[:, (ri * 4 + p4) * 128:(ri * 4 + p4 + 1) * 128], gtmp[:, 0:128], reads=[bgt], writes=[P.bdbg])
                    return True
                thc = thp[:, d, st:st + 1]
                rcol = rr_[:, d, st:st + 1]
                rb = bass.AP(rcol.tensor, rcol.offset, [list(rcol.ap[0]), [0, NB]])
                for hf in range(2):
                    if globals().get("S5BAR2", False):
                        fw.barrier()
                    if hf == 0:
                        fw.ts(V_, TAU, iotaJ, thc, None, ALU.mult, None, [bprm], [bTAB])
                    else:
                        fw.ts(V_, TAU, iotaJ, thc, th2[:, d, st:st + 1], ALU.mult, ALU.add, [bprm], [bTAB])
                    fw.copy(V_, KI, TAU, [bTAB], [bKI])
                    fw.copy(V_, KF, KI, [bKI], [bSIN])
                    fw.tt(V_, TAU, TAU, KF, ALU.subtract, [bSIN], [bTAB])
                    fw.act(SIN, TAU, AF.Sin, [bTAB], [bSIN], scale=TWO_PI_S)
                    fw.act(TAU, TAU, AF.Abs, [], [bTAB])
                    fw.act(COS, TAU, AF.Sin, [], [bTAB], scale=-TWO_PI, bias=P.halfpi[:, 0:1])
                    sg_ = segs(d, hf)
                    for (a, n, jl, rv, lat) in sg_:
                        pre, pim = 4 + (bui % 2) * 2, 5 + (bui % 2) * 2
                        bui += 1
                        tb = min(a // 512, 4)
                        tb2 = min((a + n - 1) // 512, 4)
                        rd = [P.bh[tb]] + ([P.bh[tb2]] if tb2 != tb else [])
                        for ri, pb in ((0, pre), (1, pim)):
                            fw.mm(P.psum[pb][:, 0:n], BlZ[ri][pr], uT[:, oc, a:a + n], True, True,
                                  rd + [bBlZ[ri][pr]], [P.bps[pb]])
                        bre = P.psum[pre][:, 0:n]
                        bim = P.psum[pim][:, 0:n]
                        if rv:
                            bre, bim = rev(bre), rev(bim)
                        cs_s, sn_s = COS[:, jl:jl + n], SIN[:, jl:jl + n]
                        fw.tt(V_, TA[:, jl:jl + n], bre, cs_s, ALU.mult, [P.bps[pre], bTAB], [bTA])
                        fw.tt(V_, TB_[:, jl:jl + n], bim, sn_s, ALU.mult, [P.bps[pim], bSIN], [bTBb])
                        fw.tt(PCE, Vr[:, jl:jl + n], TA[:, jl:jl + n], TB_[:, jl:jl + n], ALU.add, [bTA, bTBb], [bVr])
                        fw.tt(V_, TA[:, jl:jl + n], bim, cs_s, ALU.mult, [P.bps[pim], bTAB], [bTA])
                        fw.tt(V_, TB_[:, jl:jl + n], bre, sn_s, ALU.mult, [P.bps[pre], bSIN], [bTBb])
                        fw.tt(PCE, Vi[:, jl:jl + n], TA[:, jl:jl + n], TB_[:, jl:jl + n], ALU.subtract, [bTA, bTBb], [bVi])
                    for (Vx, bVx, Gx, bGx, si) in ((Vr, bVr, Gr, bGr, 0), (Vi, bVi, Gi, bGi, 1)):
                        init = 0.0 if hf == 0 else st2[:, si:si + 1]
                        fw.op(V_, lambda e, Gx=Gx, Vx=Vx, init=init, rb=rb: e.tensor_tensor_scan(Gx, rb, Vx, init, ALU.mult, ALU.add),
                              reads=[bVx, bprm, bst2], writes=[bGx])
                        if hf == 0:
                            fw.copy(V_, st2[:, si:si + 1], Gx[:, NB - 1:NB], [bGx], [bst2])
                    if P.tap == "s5dbg" and (oc, d, pr, hf) == P.dbgsel:
                        for i_, (ap_, b_) in enumerate(((COS, bTAB), (SIN, bSIN), (Vr, bVr), (Vi, bVi), (Gr, bGr), (Gi, bGi))):
                            fw.dma(P.dbg[:, i_ * NB:(i_ + 1) * NB], ap_, reads=[b_], writes=[P.bdbg])
                        return True
                    for (a, n, jl, rv, lat) in sg_:
                        if not lat:
                            continue
                        cs_s, sn_s = COS[:, jl:jl + n], SIN[:, jl:jl + n]
                        hr = Hh[0][:, a:a + n]
                        hi_ = Hh[1][:, a:a + n]
                        if rv:
                            hr, hi_ = rev(hr), rev(hi_)
                        fw.tt(PCE, TA[:, jl:jl + n], Gr[:, jl:jl + n], cs_s, ALU.mult, [bGr, bTAB], [bTA])
                        fw.tt(PCE, TB_[:, jl:jl + n], Gi[:, jl:jl + n], sn_s, ALU.mult, [bGi, bSIN], [bTBb])
                        fw.tt(V_, hr, TA[:, jl:jl + n], TB_[:, jl:jl + n], ALU.subtract, [bTA, bTBb], [bH[0]])
                        fw.tt(PCE, TA[:, jl:jl + n], Gr[:, jl:jl + n], sn_s, ALU.mult, [bGr, bSIN], [bTA])
                        fw.tt(PCE, TB_[:, jl:jl + n], Gi[:, jl:jl + n], cs_s, ALU.mult, [bGi, bTAB], [bTBb])
                        fw.tt(V_, hi_, TA[:, jl:jl + n], TB_[:, jl:jl + n], ALU.add, [bTA, bTBb], [bH[1]])
                if P.tap == "s5dbg2" and (oc, d, pr) == P.dbgsel[:3]:
                    for i_ in range(2):
                        fw.copy(V_, Vr[:, 0:1024], Hh[i_][:, 0:1024], [bH[i_]], [bVr])
                        fw.dma(P.dbg[:, i_ * 2048:i_ * 2048 + 1024], Vr[:, 0:1024], reads=[bVr], writes=[P.bdbg])
                        fw.copy(V_, Vi[:, 0:1024], Hh[i_][:, 1024:2048], [bH[i_]], [bVi])
                        fw.dma(P.dbg[:, i_ * 2048 + 1024:i_ * 2048 + 2048], Vi[:, 0:1024], reads=[bVi], writes=[P.bdbg])
                    return True
                if globals().get("S5BAR", False):
                    fw.barrier()
                for tb in range(4):
                    for ri in range(2):
                        fw.mm(P.psum[tb][:, :], LTm[ri][pr], Hh[ri][:, tb * 512:(tb + 1) * 512], ri == 0, ri == 1,
                              [bLTm[ri][pr], bH[ri]], [P.bps[tb]])
                    ysl = yacc[:, tb * 512:(tb + 1) * 512]
                    if d == 0 and pr == 0:
                        fw.copy("scalar", ysl, P.psum[tb][:, :], [P.bps[tb]], [byacc[tb]])
                    else:
                        fw.tt(V_, ysl, P.psum[tb][:, :], ysl, ALU.add, [P.bps[tb]], [byacc[tb]])
                if P.tap == "s5dbg4" and (oc, d, pr) == (0, 0, 0) and P.dbgsel[:3] != (0, 0, 0):
                    fw.dma(P.dbg[:, 8192:8192 + 2048], yacc, reads=byacc, writes=[P.bdbg])
                    for i_ in range(2):
                        fw.copy(V_, Vr[:, 0:1024], Hh[i_][:, 0:1024], [bH[i_]], [bVr])
                        fw.dma(P.dbg[:, 10240 + i_ * 2048:10240 + i_ * 2048 + 1024], Vr[:, 0:1024], reads=[bVr], writes=[P.bdbg])
                        fw.copy(V_, Vi[:, 0:1024], Hh[i_][:, 1024:2048], [bH[i_]], [bVi])
                        fw.dma(P.dbg[:, 10240 + i_ * 2048 + 1024:10240 + i_ * 2048 + 2048], Vi[:, 0:1024], reads=[bVi], writes=[P.bdbg])
                        fw.copy(V_, gt2[:, 0:128], LTm[i_][0], [bLTm[i_][0]], [bgt])
                        fw.dma(P.dbg[:, 14336 + i_ * 128:14336 + (i_ + 1) * 128], gt2[:, 0:128], reads=[bgt], writes=[P.bdbg])
                if globals().get("S5BAR", False):
                    fw.barrier()
                if P.tap == "s5dbg4" and (oc, d, pr) == P.dbgsel[:3]:
                    for tb in range(4):
                        fw.dma(P.dbg[:, tb * 512:(tb + 1) * 512], yacc[:, tb * 512:(tb + 1) * 512], reads=[byacc[tb]], writes=[P.bdbg])
                    for tb in range(4):
                        fw.copy(V_, gtmp, P.psum[tb][:, :], [P.bps[tb]], [bgt])
                        fw.dma(P.dbg[:, 2048 + tb * 512:2048 + (tb + 1) * 512], gtmp, reads=[bgt], writes=[P.bdbg])
                    for ri in range(2):
                        fw.copy(V_, gtmp[:, 0:128], LTm[ri][pr], [bLTm[ri][pr]], [bgt])
                        fw.dma(P.dbg[:, 4096 + ri * 128:4096 + (ri + 1) * 128], gtmp[:, 0:128], reads=[bgt], writes=[P.bdbg])
                    return True
        if P.tap == "s5dbg3" and oc == P.dbgsel[0]:
            for tb in range(4):
                fw.copy(V_, gtmp, P.psum[tb][:, :], [P.bps[tb]], [bgt])
                fw.dma(P.dbg[:, tb * 512:(tb + 1) * 512], gtmp, reads=[bgt], writes=[P.bdbg])
            return True
        for tb in range(4):
            blk = hT[:, oc, tb * 512:(tb + 1) * 512]
            fw.stt(gtmp, blk, dcol[:, oc:oc + 1], yacc[:, tb * 512:(tb + 1) * 512], ALU.mult, ALU.add,
                   [byacc[tb], P.bh[tb], bprm], [bgt])
            fw.act(gt2, gtmp, AF.Square, [bgt], [bgt])
            fw.ts(V_, gt2, gt2, 0.044715, 1.0, ALU.mult, ALU.add, [bgt], [bgt])
            fw.tt(V_, gt2, gt2, gtmp, ALU.mult, [bgt], [bgt])
            fw.act(gt2, gt2, AF.Sigmoid, [bgt], [bgt], scale=2.0 * math.sqrt(2.0 / math.pi))
            fw.tt(V_, blk, gt2, gtmp, ALU.mult, [bgt], [P.bh[tb]])
    fw.barrier()
    A.release(mX)
    if P.tap == "y1":
        tap_bf16(P, hT.rearrange("p c t -> p (c t)"), KC * T, P.bh)
        return True
    return layer1_tail(P, mX)


def layer1_tail(P, mX):
    fw, A, I = P.fw, P.A, P.I
    l = 1
    xT, hT = P.xT, P.hT
    V_ = "vector"
    wo = [A.bf16(8 * 512).rearrange("p (k n) -> p k n", k=8) for _ in range(4)]
    bwo = [Buf(f"so{i}") for i in range(4)]
    wov = I["ssm_w_out"].rearrange("(k p) n -> p k n", p=128)
    sgm = [A.f32(512), A.f32(512)]
    bsgm = [Buf("sgm0"), Buf("sgm1")]
    for i in range(4):
        fw.dma(wo[i], wov[:, :, i * 512:(i + 1) * 512], writes=[bwo[i]], q="gpsimd")
    it = 0
    for tb in range(4):
        t0, tl = TBS[tb]
        for m in range(8):
            pa, pg = (it % 4) * 2, (it % 4) * 2 + 1
            it += 1
            for (pb, wi_) in ((pa, m // 4), (pg, 2 + m // 4)):
                for k in range(KC):
                    fw.mm(P.psum[pb][:, :], wo[wi_][:, k, (m % 4) * 128:(m % 4 + 1) * 128], hT[:, k, t0:t0 + tl],
                          k == 0, k == KC - 1, [bwo[wi_], P.bh[tb]], [P.bps[pb]])
            j = it % 2
            fw.act(sgm[j], P.psum[pg][:, :], AF.Sigmoid, [P.bps[pg]], [bsgm[j]])
            fw.tt(V_, sgm[j], P.psum[pa][:, :], sgm[j], ALU.mult, [P.bps[pa]], [bsgm[j]])
            fw.stt(xT[:, m, t0:t0 + tl], sgm[j], mod_cols(P, l, 2, m, 0), xT[:, m, t0:t0 + tl], ALU.mult, ALU.add,
                   [bsgm[j], P.bmod], [P.bx[tb]])
    fw.barrier()
    A.release(mX)
    if P.tap == "xmix1":
        tap_out(P, xT.rearrange("p c t -> p (c t)"), KC * T, P.bx)
        return True
    scr = norm_scratch(P)
    for tb in range(4):
        t0, tl = TBS[tb]
        norm_mod_block(P, l, 1, tb, xT[:, :, t0:t0 + tl], P.bx[tb], scr)
    fw.barrier()
    A.release(mX)
    wr = A.bf16(64).rearrange("p (k e) -> p k e", k=8)
    bwr = Buf("wr")
    fw.dma(wr, I["moe_router"].rearrange("(k p) e -> p k e", p=128), writes=[bwr], q="gpsimd", **SLOW)
    lg = A.f32(128).rearrange("p (t e) -> p t e", e=8)
    lg2 = A.f32(128).rearrange("p (t e) -> p t e", e=8)
    eq1 = A.f32(128).rearrange("p (t e) -> p t e", e=8)
    eq2 = A.f32(128).rearrange("p (t e) -> p t e", e=8)
    gate = A.f32(128).rearrange("p (t e) -> p t e", e=8)
    m1 = A.f32(16); m2 = A.f32(16); w1_ = A.f32(16); w2_ = A.f32(16)
    bg_ = Buf("gate")
    for tt in range(16):
        for k in range(KC):
            fw.mm(P.psum[0][:, tt * 8:(tt + 1) * 8], hT[:, k, tt * 128:(tt + 1) * 128], wr[:, k, :], k == 0, k == KC - 1,
                  [P.bh[tt // 4], bwr], [P.bps[0]])
    G1 = [bg_]
    fw.copy(V_, lg, P.psum[0][:, 0:128].rearrange("p (t e) -> p t e", e=8), [P.bps[0]], G1)

    def bc8(v):
        return bass.AP(v.tensor, v.offset, [list(d_) for d_ in v.ap] + [[0, 8]])
    fw.op(V_, lambda e: e.tensor_reduce(m1, lg, AX.X, ALU.max), reads=G1, writes=G1)
    fw.tt(V_, eq1, lg, bc8(m1), ALU.is_equal, G1, G1)
    fw.stt(lg2, eq1, -1.0e30, lg, ALU.mult, ALU.add, G1, G1)
    fw.op(V_, lambda e: e.tensor_reduce(m2, lg2, AX.X, ALU.max), reads=G1, writes=G1)
    fw.tt(V_, eq2, lg2, bc8(m2), ALU.is_equal, G1, G1)
    fw.tt(V_, w2_, m2, m1, ALU.subtract, G1, G1)
    fw.act(w2_, w2_, AF.Exp, G1, G1)
    fw.ts(V_, w1_, w2_, 1.0, None, ALU.add, None, G1, G1)
    fw.op(V_, lambda e: e.reciprocal(w1_, w1_), reads=G1, writes=G1)
    fw.tt(V_, w2_, w2_, w1_, ALU.mult, G1, G1)
    fw.tt(V_, gate, eq1, bc8(w1_), ALU.mult, G1, G1)
    fw.tt(V_, eq2, eq2, bc8(w2_), ALU.mult, G1, G1)
    fw.tt(V_, gate, gate, eq2, ALU.add, G1, G1)
    gbc = [A.bf16(L), A.bf16(L)]
    bgbc = [Buf("gbc0"), Buf("gbc1")]
    fb = ffn_buffers(P, L)
    halves = [(0, 1024), (1024, 1024)]
    for e_ in range(NEXP):
        gj = e_ % 2
        for tb in range(4):
            pb = 4 + (e_ * 4 + tb) % 4
            for t4 in range(4):
                tt = tb * 4 + t4
                gcol = gate[:, tt, e_:e_ + 1]
                gl_ = bass.AP(gcol.tensor, gcol.offset, [list(gcol.ap[0]), [0, 128]])
                fw.mm(P.psum[pb][:, t4 * 128:(t4 + 1) * 128], gl_, P.ident, True, True, G1 + [P.bconst], [P.bps[pb]])
            fw.copy("scalar", gbc[gj][:, tb * 512:(tb + 1) * 512], P.psum[pb][:, :], [P.bps[pb]], [bgbc[gj]])
        nxt = None
        if e_ + 1 < NEXP:
            nxt = (I["moe_w1"][e_ + 1], I["moe_w3"][e_ + 1], I["moe_w2"][e_ + 1])
        ffn(P, l, fb, I["moe_w1"][e_], I["moe_w3"][e_], I["moe_w2"][e_], EXPERT_DIM, halves, 4,
            gate=gbc[gj], bgate=bgbc[gj], first=(e_ == 0), nxt=nxt)
    fw.barrier()
    A.release(mX)
    if P.tap == "xffn1":
        tap_out(P, xT.rearrange("p c t -> p (c t)"), KC * T, P.bx)
        return True
    return False


def write_output(P):
    fw, A = P.fw, P.A
    m0 = A.mark()
    xo = [A.f32(D) for _ in range(2)]
    bxo = [Buf("xo0"), Buf("xo1")]
    ntt = 18 if 1 not in P.layers else 16
    bo = Buf("out_lat")
    boc = Buf("out_ctx")
    pi = 0
    for tt in range(ntt):
        j = tt % 2
        tb = min(tt // 4, 4)
        for half in range(2):
            pb = pi % 4
            pi += 1
            ps, bps = P.psum[pb], P.bps[pb]
            for cc in range(4):
                c = half * 4 + cc
                fw.tr(ps[:, cc * 128:(cc + 1) * 128], P.xT[:, c, tt * 128:(tt + 1) * 128], P.ident,
                      [P.bx[tb], P.bconst], [bps])
            fw.copy("vector" if half else "scalar", xo[j][:, half * 512:(half + 1) * 512], ps[:, :], [bps], [bxo[j]])
        if tt < 16:
            fw.dma(P.out_lat[tt * 128:(tt + 1) * 128, :], xo[j], reads=[bxo[j]], writes=[bo])
        else:
            fw.dma(P.out_ctx[(tt - 16) * 128:(tt - 15) * 128, :], xo[j], reads=[bxo[j]], writes=[boc])
    P.out_bufs.append(bo)
    if ntt == 18:
        P.out_bufs.append(boc)
    A.release(m0)


_CACHE = {}


def _get_prog(key, **kw):
    if key not in _CACHE:
        _CACHE[key] = build_program(**kw)
    return _CACHE[key]


def make_in_map(inputs, b, layers=(0, 1), x_override=None, ctx_override=None):
    f = lambda a: np.ascontiguousarray(np.asarray(a, dtype=np.float32))
    m = {}
    m["x"] = f(inputs["x"][b] if x_override is None else x_override)
    m["ctx"] = f(inputs["ctx"][b] if ctx_override is None else ctx_override)
    m["cc"] = f(np.stack([np.asarray(inputs["c"][b]), np.asarray(inputs["c_ctx"])]))
    for k in ("ada_w", "ada_b", "norm1_g", "norm2_g"):
        m[k] = f(inputs[k])
    if 0 in layers:
        for k in ("mix_w_in", "q_norm_g", "k_norm_g", "na_rpb", "conv_dw_w", "conv_dw_b", "conv_ln_g",
                  "conv_ln_b", "mix_w_out", "ffn_w1", "ffn_w3", "ffn_w2"):
            m[k] = f(inputs[k][0])
    if 1 in layers:
        for k in ("ssm_w_in", "ssm_a_re", "ssm_a_im", "ssm_log_dt", "ssm_b_re", "ssm_b_im", "ssm_c_re",
                  "ssm_c_im", "ssm_d", "ssm_w_out", "moe_router", "moe_w1", "moe_w3", "moe_w2"):
            m[k] = f(inputs[k][0])
    return m


MODE = "fused"


def kernel(**inputs):
    if MODE == "fused":
        nc, P = _get_prog("full", layers=(0, 1))
        in_maps = [make_in_map(inputs, b) for b in range(8)]
        res = run_bass_kernel_spmd(nc, in_maps, core_ids=list(range(8)))
        return np.stack([np.asarray(r["out_lat"], dtype=np.float32) for r in res.results], axis=0)
    ncA, PA = _get_prog("L0", layers=(0,))
    in_maps = [make_in_map(inputs, b, layers=(0,)) for b in range(8)]
    resA = run_bass_kernel_spmd(ncA, in_maps, core_ids=list(range(8)))
    ncB, PB = _get_prog("L1", layers=(1,))
    in_maps = [make_in_map(inputs, b, layers=(1,), x_override=np.asarray(resA.results[b]["out_lat"]),
                           ctx_override=np.asarray(resA.results[b]["out_ctx"])) for b in range(8)]
    resB = run_bass_kernel_spmd(ncB, in_maps, core_ids=list(range(8)))
    return np.stack([np.asarray(r["out_lat"], dtype=np.float32) for r in resB.results], axis=0)
```
